# Optimizing a Trainium2 kernel written in Bass

```python
import math
import jax, jax.numpy as jnp
from jax import lax
import numpy as np

D_MODEL = 2048
BATCH = 4
SEQ = 2048
DEPTH = 1

Q_BLOCK = 128
DA_HEADS = 8
DA_HALF_DIM = 64
DA_V_DIM = 2 * DA_HALF_DIM
DA_WIDTH = DA_HEADS * DA_V_DIM
SB_HEADS = 8
SB_DIM = 128
SB_WIDTH = SB_HEADS * SB_DIM
REL_BUCKETS = 32
REL_MAX_DIST = 128
IN_COLS = 3 * DA_WIDTH + 3 * SB_WIDTH + 2 * D_MODEL
N_GROUPS = 8
EXPERTS_PER_GROUP = 8
N_EXPERTS = N_GROUPS * EXPERTS_PER_GROUP
TOP_K = 2
D_EXPERT = 1024
MOE_BLOCK = 128
N_MOD = 6
EPS = 1e-6

kernel_name = "hybrid_diffattn_stickbreak_hmoe_block"


def rms_norm(x, g):
    xf = x.astype(jnp.float32)
    y = xf * lax.rsqrt(jnp.mean(xf * xf, axis=-1, keepdims=True) + EPS)
    return (y * g.astype(jnp.float32)).astype(x.dtype)


def rel_bucket(q_pos, k_pos):
    n = jnp.maximum(q_pos[:, None] - k_pos[None, :], 0)
    max_exact = REL_BUCKETS // 2
    nf = jnp.maximum(n, 1).astype(jnp.float32)
    large = max_exact + (jnp.log(nf / max_exact) / math.log(REL_MAX_DIST / max_exact)
                         * (REL_BUCKETS - max_exact)).astype(jnp.int32)
    large = jnp.minimum(large, REL_BUCKETS - 1)
    return jnp.where(n < max_exact, n, large)


def split_heads(t, n_heads, dim):
    b, s, _ = t.shape
    return t.reshape(b, s, n_heads, dim).transpose(0, 2, 1, 3)


def merge_heads(t):
    b, h, s, d = t.shape
    return t.transpose(0, 2, 1, 3).reshape(b, s, h * d)


def to_blocks(t):
    b, h, s, d = t.shape
    return t.reshape(b, h, s // Q_BLOCK, Q_BLOCK, d).transpose(2, 0, 1, 3, 4)


def from_blocks(t):
    nqb, b, h, qb, d = t.shape
    return t.transpose(1, 2, 0, 3, 4).reshape(b, h, nqb * qb, d)


def diff_attention(q1, q2, k1, k2, v, lam, rel_table):
    s_len = q1.shape[2]
    k_pos = jnp.arange(s_len)
    scale = DA_HALF_DIM ** -0.5

    def block(args):
        q1b, q2b, blk = args
        q_pos = blk * Q_BLOCK + jnp.arange(Q_BLOCK)
        bias = rel_table[rel_bucket(q_pos, k_pos)].astype(jnp.float32).transpose(2, 0, 1)
        causal = k_pos[None, :] <= q_pos[:, None]

        def probs(qb, k):
            s = jnp.einsum('bhqd,bhkd->bhqk', qb, k).astype(jnp.float32) * scale + bias
            return jax.nn.softmax(jnp.where(causal, s, -jnp.inf), axis=-1)

        a = probs(q1b, k1) - lam * probs(q2b, k2)
        return jnp.einsum('bhqk,bhkd->bhqd', a.astype(v.dtype), v)

    out = lax.map(block, (to_blocks(q1), to_blocks(q2), jnp.arange(s_len // Q_BLOCK)))
    return from_blocks(out)


def stick_breaking_attention(q, k, v):
    s_len = q.shape[2]
    k_pos = jnp.arange(s_len)
    scale = SB_DIM ** -0.5

    def block(args):
        qb, blk = args
        q_pos = blk * Q_BLOCK + jnp.arange(Q_BLOCK)
        past = k_pos[None, :] < q_pos[:, None]
        z = jnp.einsum('bhqd,bhkd->bhqk', qb, k).astype(jnp.float32) * scale
        log_beta = jax.nn.log_sigmoid(z)
        log_keep = jnp.where(past, jax.nn.log_sigmoid(-z), 0.0)
        later = lax.cumsum(log_keep, axis=3, reverse=True) - log_keep
        w = jnp.where(past, jnp.exp(log_beta + later), 0.0)
        return jnp.einsum('bhqk,bhkd->bhqd', w.astype(v.dtype), v)

    out = lax.map(block, (to_blocks(q), jnp.arange(s_len // Q_BLOCK)))
    return from_blocks(out)


def hierarchical_moe(h, w_rg, b_rg, w_re, b_re, w_gate, w_up, w_down):
    bsz, slen, d = h.shape
    n_tok = bsz * slen
    hf = h.reshape(n_tok, d)
    g_logits = (hf @ w_rg).astype(jnp.float32) + b_rg.astype(jnp.float32)
    g_prob = jax.nn.softmax(g_logits, axis=-1)
    g_idx = jnp.argmax(g_logits, axis=-1)
    p_g = jnp.take_along_axis(g_prob, g_idx[:, None], axis=1)
    e_logits = ((hf @ w_re).astype(jnp.float32) + b_re.astype(jnp.float32)
                ).reshape(n_tok, N_GROUPS, EXPERTS_PER_GROUP)
    e_sel = jnp.take_along_axis(e_logits, g_idx[:, None, None], axis=1)[:, 0]
    top_p, top_i = lax.top_k(jax.nn.softmax(e_sel, axis=-1), TOP_K)
    top_p = top_p / jnp.sum(top_p, axis=-1, keepdims=True)
    gate = (p_g * top_p).reshape(-1)
    flat_e = (g_idx[:, None] * EXPERTS_PER_GROUP + top_i).reshape(-1)
    flat_tok = jnp.repeat(jnp.arange(n_tok, dtype=jnp.int32), TOP_K)
    m = n_tok * TOP_K
    n_blocks = -(-m // MOE_BLOCK) + N_EXPERTS
    rows = n_blocks * MOE_BLOCK
    onehot = jax.nn.one_hot(flat_e, N_EXPERTS, dtype=jnp.int32)
    rank = jnp.take_along_axis(jnp.cumsum(onehot, axis=0), flat_e[:, None], axis=1)[:, 0] - 1
    counts = jnp.sum(onehot, axis=0)
    padded = ((counts + MOE_BLOCK - 1) // MOE_BLOCK) * MOE_BLOCK
    pend = jnp.cumsum(padded)
    dest = (pend - padded)[flat_e] + rank
    buf_tok = jnp.zeros((rows,), jnp.int32).at[dest].set(flat_tok)
    buf_w = jnp.zeros((rows,), jnp.float32).at[dest].set(gate)
    starts = jnp.arange(n_blocks) * MOE_BLOCK
    block_e = jnp.minimum(jnp.sum(pend[None, :] <= starts[:, None], axis=1), N_EXPERTS - 1)
    xs = hf[buf_tok].reshape(n_blocks, MOE_BLOCK, d)

    def expert_block(args):
        xb, e = args
        a = xb @ w_gate[e]
        u = xb @ w_up[e]
        return (jax.nn.silu(a) * u) @ w_down[e]

    ys = lax.map(expert_block, (xs, block_e)).reshape(rows, d)
    out = jnp.zeros((n_tok, d), h.dtype).at[buf_tok].add(ys * buf_w[:, None].astype(h.dtype))
    return out.reshape(bsz, slen, d)


def setup_inputs(seed: int = 0) -> dict:
    key = jax.random.key(seed)
    ks = jax.random.split(key, 24)
    f32 = jnp.float32
    nrm = lambda k, shape, s: jax.random.normal(k, shape, f32) * s
    D = D_MODEL
    return {
        "x": nrm(ks[0], (BATCH, SEQ, D), 1.0),
        "c": nrm(ks[1], (BATCH, D), 1.0),
        "rel_bias_table": nrm(ks[2], (REL_BUCKETS, DA_HEADS), 0.5),
        "w_ada": nrm(ks[3], (DEPTH, D, N_MOD * D), D ** -0.5),
        "b_ada": nrm(ks[4], (DEPTH, N_MOD * D), 0.02),
        "g_mix": 1.0 + nrm(ks[5], (DEPTH, D), 0.02),
        "w_in": nrm(ks[6], (DEPTH, D, IN_COLS), D ** -0.5),
        "lambda_q1": nrm(ks[7], (DEPTH, DA_HALF_DIM), 0.1),
        "lambda_k1": nrm(ks[8], (DEPTH, DA_HALF_DIM), 0.1),
        "lambda_q2": nrm(ks[9], (DEPTH, DA_HALF_DIM), 0.1),
        "lambda_k2": nrm(ks[10], (DEPTH, DA_HALF_DIM), 0.1),
        "g_subln": 1.0 + nrm(ks[11], (DEPTH, DA_V_DIM), 0.02),
        "w_proj_a": nrm(ks[12], (DEPTH, DA_WIDTH, D), DA_WIDTH ** -0.5),
        "w_proj_b": nrm(ks[13], (DEPTH, SB_WIDTH, D), SB_WIDTH ** -0.5),
        "w_out": nrm(ks[14], (DEPTH, D, D), D ** -0.5),
        "g_ffn": 1.0 + nrm(ks[15], (DEPTH, D), 0.02),
        "w_router_group": nrm(ks[16], (DEPTH, D, N_GROUPS), D ** -0.5),
        "b_router_group": nrm(ks[17], (DEPTH, N_GROUPS), 0.01),
        "w_router_expert": nrm(ks[18], (DEPTH, D, N_EXPERTS), D ** -0.5),
        "b_router_expert": nrm(ks[19], (DEPTH, N_EXPERTS), 0.01),
        "w_expert_gate": nrm(ks[20], (DEPTH, N_EXPERTS, D, D_EXPERT), D ** -0.5),
        "w_expert_up": nrm(ks[21], (DEPTH, N_EXPERTS, D, D_EXPERT), D ** -0.5),
        "w_expert_down": nrm(ks[22], (DEPTH, N_EXPERTS, D_EXPERT, D), D_EXPERT ** -0.5),
        "g_final": 1.0 + nrm(ks[23], (D,), 0.02),
    }


def reference(x, c, rel_bias_table, w_ada, b_ada, g_mix, w_in, lambda_q1, lambda_k1,
              lambda_q2, lambda_k2, g_subln, w_proj_a, w_proj_b, w_out, g_ffn,
              w_router_group, b_router_group, w_router_expert, b_router_expert,
              w_expert_gate, w_expert_up, w_expert_down, g_final):
    D = D_MODEL
    cut = np.cumsum([DA_WIDTH, DA_WIDTH, DA_WIDTH, SB_WIDTH, SB_WIDTH, SB_WIDTH, D]).tolist()
    for l in range(DEPTH):
        lam_init = 0.8 - 0.6 * math.exp(-0.3 * l)
        mod = jax.nn.silu(c) @ w_ada[l] + b_ada[l]
        shift_m, scale_m, gate_m, shift_f, scale_f, gate_f = jnp.split(mod[:, None, :], N_MOD, axis=-1)

        h = rms_norm(x, g_mix[l]) * (1.0 + scale_m) + shift_m
        proj = h @ w_in[l]
        qa, ka, va, qb, kb, vb, gate_a, gate_b = jnp.split(proj, cut, axis=-1)
        qa = split_heads(qa, DA_HEADS, 2 * DA_HALF_DIM)
        ka = split_heads(ka, DA_HEADS, 2 * DA_HALF_DIM)
        va = split_heads(va, DA_HEADS, DA_V_DIM)
        lam = (jnp.exp(jnp.sum(lambda_q1[l].astype(jnp.float32) * lambda_k1[l].astype(jnp.float32)))
               - jnp.exp(jnp.sum(lambda_q2[l].astype(jnp.float32) * lambda_k2[l].astype(jnp.float32)))
               + lam_init)
        oa = diff_attention(qa[..., :DA_HALF_DIM], qa[..., DA_HALF_DIM:],
                            ka[..., :DA_HALF_DIM], ka[..., DA_HALF_DIM:], va, lam, rel_bias_table)
        oa = merge_heads(rms_norm(oa, g_subln[l]) * (1.0 - lam_init))
        ob = merge_heads(stick_breaking_attention(split_heads(qb, SB_HEADS, SB_DIM),
                                                  split_heads(kb, SB_HEADS, SB_DIM),
                                                  split_heads(vb, SB_HEADS, SB_DIM)))
        merged = (jax.nn.sigmoid(gate_a) * (oa @ w_proj_a[l])
                  + jax.nn.sigmoid(gate_b) * (ob @ w_proj_b[l]))
        x = x + gate_m * (merged @ w_out[l])

        h2 = rms_norm(x, g_ffn[l]) * (1.0 + scale_f) + shift_f
        x = x + gate_f * hierarchical_moe(h2, w_router_group[l], b_router_group[l],
                                          w_router_expert[l], b_router_expert[l],
                                          w_expert_gate[l], w_expert_up[l], w_expert_down[l])
    return rms_norm(x, g_final)
```

```python
import math
from contextlib import ExitStack

import numpy as np
import concourse.bass as bass
import concourse.mybir as mybir
from concourse.bass_utils import run_bass_kernel_spmd

F32 = mybir.dt.float32
BF16 = mybir.dt.bfloat16
I32 = mybir.dt.int32
U32 = mybir.dt.uint32
AF = mybir.ActivationFunctionType
ALU = mybir.AluOpType
AX = mybir.AxisListType

D = 2048
SEQ = 2048
NOWN = 1024
NH = 8
NBLK = 80
EPS = 1e-6
LAM_INIT = 0.8 - 0.6 * math.exp(0.0)
NEG = -30000.0
SBUF_LOG = []


class Sched:
    def __init__(self, nc, stack, n_dma_sems=32):
        self.nc = nc
        self.eng = {"pe": nc.tensor, "act": nc.scalar, "dve": nc.vector,
                    "pool": nc.gpsimd, "sp": nc.sync}
        self.sem = {e: stack.enter_context(nc.semaphore("s_" + e)) for e in self.eng}
        self.cnt = {e: 0 for e in self.eng}
        self.dsem = [stack.enter_context(nc.semaphore("d%d" % i)) for i in range(n_dma_sems)]
        self.dcnt = [0] * n_dma_sems
        self.dnext = 0
        self.seen = {e: {} for e in self.eng}
        self.lastw = {}
        self.reads = {}
        self.semobj = {}
        for e, s in self.sem.items():
            self.semobj[("e", e)] = s
        for i, s in enumerate(self.dsem):
            self.semobj[("d", i)] = s

    def _wait(self, e, ev):
        sid, val, src = ev
        if src == "pe" and e == "pe":
            return
        if self.seen[e].get(sid, 0) >= val:
            return
        self.seen[e][sid] = val
        self.eng[e].wait_ge(self.semobj[sid], val)

    def _deps(self, e, reads, writes, extra):
        evs = []
        for k in reads:
            if k in self.lastw:
                evs.append(self.lastw[k])
        for k in writes:
            if k in self.lastw:
                evs.append(self.lastw[k])
            evs.extend(self.reads.get(k, []))
        evs.extend(extra)
        best = {}
        for ev in evs:
            sid, val, src = ev
            if src == "pe" and e == "pe":
                continue
            if sid not in best or best[sid][1] < val:
                best[sid] = ev
        for ev in best.values():
            self._wait(e, ev)

    def _record(self, ev, reads, writes):
        for k in reads:
            lst = self.reads.setdefault(k, [])
            lst.append(ev)
        for k in writes:
            self.lastw[k] = ev
            self.reads[k] = []

    def op(self, e, fn, reads=(), writes=(), extra=()):
        self._deps(e, reads, writes, extra)
        ins = fn()
        self.cnt[e] += 1
        ins.then_inc(self.sem[e], 1)
        ev = (("e", e), self.cnt[e], e)
        self._record(ev, reads, writes)
        return ev

    def dma(self, q, fn, reads=(), writes=(), extra=()):
        i = self.dnext
        self.dnext = (self.dnext + 1) % len(self.dsem)
        sid = ("d", i)
        if self.dcnt[i] > 0:
            self._wait(q, (sid, self.dcnt[i], "dma"))
        self._deps(q, reads, writes, extra)
        ins = fn()
        self.dcnt[i] += 16
        ins.then_inc(self.dsem[i], 16)
        ev = (sid, self.dcnt[i], "dma")
        self._record(ev, reads, writes)
        return ev

    def all_events(self):
        evs = []
        for i, c in enumerate(self.dcnt):
            if c:
                evs.append((("d", i), c, "dma"))
        for en in self.eng:
            if self.cnt[en]:
                evs.append((("e", en), self.cnt[en], "x"))
        return evs

    def barrier(self):
        evs = self.all_events()
        for e in self.eng:
            for ev in evs:
                self._wait(e, ev)
        self.lastw = {}
        self.reads = {}

    def finish(self, e="sp"):
        for ev in self.all_events():
            self._wait(e, ev)


def bcast_rows(ap, nparts=128):
    n = ap.shape[-1]
    return bass.AP(ap.tensor, ap.offset, [[0, nparts], [1, n]])


def rel_bucket_np(n):
    n = np.maximum(n, 0)
    nf = np.maximum(n, 1).astype(np.float32)
    large = 16 + (np.log(nf / np.float32(16)) / np.float32(math.log(128 / 16)) * np.float32(16)).astype(np.int32)
    large = np.minimum(large, 31)
    return np.where(n < 16, n, large)


def host_consts():
    i = np.arange(128)
    c = {}
    c["ident"] = np.eye(128, dtype=np.float32)
    c["antiI"] = np.eye(128, dtype=np.float32)[::-1].copy()
    c["tri"] = (i[:, None] < i[None, :]).astype(np.float32)
    c["ugt"] = (i[:, None] > i[None, :]).astype(np.float32)
    c["ones"] = np.ones((128, 128), np.float32)
    n = np.arange(640) - 256
    oh = np.zeros((32, 640), np.float32)
    valid = (n >= 0) & (n < 256)
    bk = rel_bucket_np(np.clip(n, 0, None))
    oh[bk[valid], np.nonzero(valid)[0]] = 1.0
    oh[31, valid] -= 1.0
    c["ohb"] = oh
    c["negrow"] = np.where(n < 0, NEG, 0.0).astype(np.float32)[None, :]
    c["iota128"] = np.tile(np.arange(128, dtype=np.float32)[None, :], (128, 1))
    c["thr"] = np.tile((128.0 * np.arange(16, dtype=np.float32))[None, :], (128, 1))
    c["pcol"] = np.arange(128, dtype=np.float32)[:, None].copy()
    return c


CONST_SHAPES = {"ident": [128, 128], "antiI": [128, 128], "tri": [128, 128], "ugt": [128, 128],
                "ones": [128, 128], "ohb": [32, 640], "negrow": [1, 640], "iota128": [128, 128],
                "thr": [128, 16], "pcol": [128, 1]}

WEIGHT_SHAPES = {
    "rel_bias_table": [32, 8], "w_ada": [D, 6 * D], "b_ada": [1, 6 * D], "g_mix_col": [128, 16],
    "w_in": [D, 10240], "lq1": [1, 64], "lk1": [1, 64], "lq2": [1, 64], "lk2": [1, 64],
    "g_subln": [1, 128], "w_proj_a": [1024, D], "w_proj_b": [1024, D], "w_out": [D, D],
    "g_ffn_col": [128, 16], "w_rg": [D, 8], "b_rg": [1, 8], "w_re": [D, 64], "b_re": [1, 64],
    "w_eg": [64 * D, 1024], "w_eu": [64 * D, 1024], "w_ed": [64 * 1024, D], "g_final": [1, D],
}


def build(stage=99, dbg=False):
    nc = bass.Bass("TRN2", target_bir_lowering=False)
    din = {}

    def dram_in(name, shape, dt=F32):
        din[name] = nc.dram_tensor(name, list(shape), dt, kind="ExternalInput").ap()
        return din[name]

    x_all = dram_in("x_all", [SEQ, D])
    x_own = dram_in("x_own", [NOWN, D])
    c_col = dram_in("c_col", [128, 16])
    halfv = dram_in("halfv", [128, 1])
    W = {k: dram_in(k, s) for k, s in WEIGHT_SHAPES.items() if stage >= 7 or k not in ("w_eg", "w_eu", "w_ed")}
    C = {k: dram_in("c_" + k, s) for k, s in CONST_SHAPES.items()}
    out_own = nc.dram_tensor("out_own", [NOWN, D], F32, kind="ExternalOutput").ap()
    dbg_out = {}

    def dbg_tensor(name, shape):
        dbg_out[name] = nc.dram_tensor("dbg_" + name, list(shape), F32, kind="ExternalOutput").ap()
        return dbg_out[name]

    tv_d = nc.dram_tensor("tv_scratch", [8, 640], F32, kind="Internal")
    mod_d = nc.dram_tensor("mod_scratch", [6, D], F32, kind="Internal").ap()
    x1_d = nc.dram_tensor("dbg_x1" if dbg else "x1_scratch", [NOWN, D], F32, kind="ExternalOutput" if dbg else "Internal").ap()

    with ExitStack() as st0:
        S = Sched(nc, st0)
        V, A_, P_, T_ = nc.vector, nc.scalar, nc.gpsimd, nc.tensor

        def sb(stack, name, shape, dt):
            return stack.enter_context(nc.sbuf_tensor(name, list(shape), dt))

        psb = [st0.enter_context(nc.psum_tensor("psb%d" % i, [128, 512], F32)) for i in range(8)]

        def ps_bf(i):
            return psb[i][:].bitcast(BF16)

        cs = {}
        for k in ("ident", "antiI", "tri", "ugt", "ones"):
            cs[k] = sb(st0, "k_" + k, [128, 128], F32)
            S.dma("sp", lambda k=k: nc.sync.dma_start(out=cs[k][:], in_=C[k][:, :]), writes=["c_" + k])
        identb = sb(st0, "identb", [128, 128], BF16)
        S.op("dve", lambda: V.tensor_copy(out=identb[:], in_=cs["ident"][:]), reads=["c_ident"], writes=["identb"])
        half_t = sb(st0, "half_t", [128, 1], F32)
        S.dma("sp", lambda: nc.sync.dma_start(out=half_t[:], in_=halfv[:, :]), writes=["half"])

        Gm_col = sb(st0, "Gm_col", [128, 16], F32)
        shm_col = sb(st0, "shm_col", [128, 16], F32)
        Gf_col = sb(st0, "Gf_col", [128, 16], F32)
        shf_col = sb(st0, "shf_col", [128, 16], F32)

        with ExitStack() as st:
            ccol = sb(st, "ccol", [128, 16], F32)
            scol = sb(st, "scol", [128, 16], F32)
            gmix = sb(st, "gmix", [128, 16], F32)
            S.dma("sp", lambda: nc.sync.dma_start(out=ccol[:], in_=c_col[:, :]), writes=["ccol"])
            S.dma("sp", lambda: nc.sync.dma_start(out=gmix[:], in_=W["g_mix_col"][:, :]), writes=["gmix"])
            gffn = sb(st, "gffn", [128, 16], F32)
            S.dma("sp", lambda: nc.sync.dma_start(out=gffn[:], in_=W["g_ffn_col"][:, :]), writes=["gffn"])
            S.op("act", lambda: A_.activation(out=scol[:], in_=ccol[:], func=AF.Silu), reads=["ccol"], writes=["scol"])
            wa = [sb(st, "wa%d" % i, [128, 16, 512], F32) for i in range(2)]
            brow = [sb(st, "brow%d" % i, [1, 512], F32) for i in range(2)]
            mrow = [sb(st, "mrow%d" % i, [1, 512], F32) for i in range(2)]
            one11 = sb(st, "one11", [1, 1], F32)
            S.op("dve", lambda: V.memset(one11[:], 1.0), writes=["one11"])
            w_ada_r = W["w_ada"].rearrange("(c p) n -> p c n", p=128)
            for j in range(24):
                m, jj = j // 4, j % 4
                b = j % 2
                for hh in range(2):
                    S.dma("sp", lambda b=b, j=j, hh=hh: nc.sync.dma_start(
                        out=wa[b][:, hh * 8:(hh + 1) * 8, :], in_=w_ada_r[:, hh * 8:(hh + 1) * 8, j * 512:(j + 1) * 512]),
                        writes=[("wa", b, hh)])
                S.dma("sp", lambda b=b, j=j: nc.sync.dma_start(out=brow[b][:], in_=W["b_ada"][0:1, j * 512:(j + 1) * 512]), writes=[("brow", b)])
                pb = psb[b]
                for c in range(16):
                    S.op("pe", lambda c=c, b=b, pb=pb: T_.matmul(pb[0:1, :], lhsT=scol[:, c:c + 1], rhs=wa[b][:, c, :], start=(c == 0), stop=(c == 15)),
                         reads=["scol", ("wa", b, c // 8)], writes=[("psb", b)])
                S.op("dve", lambda b=b, pb=pb: V.tensor_tensor(out=mrow[b][:], in0=pb[0:1, :], in1=brow[b][:], op=ALU.add),
                     reads=[("psb", b), ("brow", b)], writes=[("mrow", b)])
                if m in (0, 1, 3, 4):
                    pc = psb[2 + b]
                    for q in range(4):
                        S.op("pe", lambda q=q, b=b, pc=pc: T_.matmul(pc[:, q:q + 1], lhsT=mrow[b][0:1, q * 128:(q + 1) * 128], rhs=one11[0:1, 0:1], start=True, stop=True),
                             reads=[("mrow", b), "one11"], writes=[("psb", 2 + b)])
                    dst = {0: shm_col, 1: Gm_col, 3: shf_col, 4: Gf_col}[m]
                    key = {0: "shm_col", 1: "Gm_col", 3: "shf_col", 4: "Gf_col"}[m]
                    S.op("dve", lambda b=b, pc=pc, dst=dst, jj=jj: V.tensor_copy(out=dst[:, jj * 4:(jj + 1) * 4], in_=pc[:, 0:4]),
                         reads=[("psb", 2 + b)], writes=[key])
                S.dma("sp", lambda b=b, m=m, jj=jj: nc.sync.dma_start(out=mod_d[m:m + 1, jj * 512:(jj + 1) * 512], in_=mrow[b][:]),
                      reads=[("mrow", b)], writes=[("mod_d", j)])
            S.op("dve", lambda: V.scalar_tensor_tensor(out=Gm_col[:], in0=Gm_col[:], scalar=1.0, in1=gmix[:], op0=ALU.add, op1=ALU.mult),
                 reads=["Gm_col", "gmix"], writes=["Gm_col"])
            S.op("dve", lambda: V.scalar_tensor_tensor(out=Gf_col[:], in0=Gf_col[:], scalar=1.0, in1=gffn[:], op0=ALU.add, op1=ALU.mult),
                 reads=["Gf_col", "gffn"], writes=["Gf_col"])
            if dbg:
                d = dbg_tensor("Gm_col", [128, 16]); S.dma("sp", lambda: nc.sync.dma_start(out=d[:, :], in_=Gm_col[:]), reads=["Gm_col"])
                d2 = dbg_tensor("shm_col", [128, 16]); S.dma("sp", lambda: nc.sync.dma_start(out=d2[:, :], in_=shm_col[:]), reads=["shm_col"])
            S.barrier()
        if stage <= 0:
            S.finish("sp")
            return nc, dbg_out

        stR = ExitStack()
        stA = ExitStack()
        eps_t = sb(st0, "eps_t", [128, 1], F32)
        S.op("dve", lambda: V.memset(eps_t[:], EPS), writes=["eps"])
        hT_own = sb(stA, "hT_own", [128, 16, NOWN], BF16)
        oaT = sb(stA, "oaT", [128, NH, NOWN], BF16)
        obT = sb(stA, "obT", [128, NH, NOWN], BF16)

        def norm_tile_to_hT(stk_bufs, src_ap, dstT, col0, tag):
            xt, xb, ss, rs, junk = stk_bufs
            for hh in range(2):
                S.dma("sp", lambda hh=hh: nc.sync.dma_start(out=xt[:, hh * 1024:(hh + 1) * 1024], in_=src_ap[:, hh * 1024:(hh + 1) * 1024]), writes=[(tag, "xt", hh)])
            S.op("act", lambda: A_.activation(out=junk[:], in_=xt[:], func=AF.Square, accum_out=ss[:]),
                 reads=[(tag, "xt", 0), (tag, "xt", 1)], writes=[(tag, "ss"), (tag, "junk")])
            S.op("act", lambda: A_.activation(out=rs[:], in_=ss[:], func=AF.Sqrt, bias=eps_t[:], scale=1.0 / D),
                 reads=[(tag, "ss"), "eps"], writes=[(tag, "rs")])
            S.op("dve", lambda: V.reciprocal(out=rs[:], in_=rs[:]), reads=[(tag, "rs")], writes=[(tag, "rs")])
            S.op("dve", lambda: V.tensor_scalar(out=xb[:], in0=xt[:], scalar1=rs[:, 0:1], scalar2=None, op0=ALU.mult),
                 reads=[(tag, "xt", 0), (tag, "xt", 1), (tag, "rs")], writes=[(tag, "xb")])
            for g in range(2):
                pt = ps_bf(6 + g)
                for c8 in range(8):
                    c = g * 8 + c8
                    S.op("pe", lambda c=c, c8=c8, pt=pt: T_.transpose(pt[:, c8 * 128:(c8 + 1) * 128], xb[:, c * 128:(c + 1) * 128], identb[:]),
                         reads=[(tag, "xb"), "identb"], writes=[("psb", 6 + g)])
                for c8 in range(8):
                    c = g * 8 + c8
                    S.op("act", lambda c=c, c8=c8, pt=pt: A_.activation(out=dstT[:, c, col0:col0 + 128], in_=pt[:, c8 * 128:(c8 + 1) * 128],
                                                                      func=AF.Identity, bias=shm_col[:, c:c + 1], scale=Gm_col[:, c:c + 1]),
                         reads=[("psb", 6 + g), "shm_col", "Gm_col"], writes=[(tag, "hT", col0 // 512)])

        with ExitStack() as stB:
            hT_all = sb(stB, "hT_all", [128, 16, SEQ], BF16)
            with ExitStack() as st:
                bufs = []
                for i in range(2):
                    bufs.append((sb(st, "xt%d" % i, [128, D], F32), sb(st, "xb%d" % i, [128, D], BF16),
                                 sb(st, "ss%d" % i, [128, 1], F32), sb(st, "rs%d" % i, [128, 1], F32),
                                 sb(st, "junk%d" % i, [128, D], BF16)))
                for t in range(16):
                    norm_tile_to_hT(bufs[t % 2], x_all[t * 128:(t + 1) * 128, :], hT_all, t * 128, ("n", t % 2))
                S.barrier()
                for t in range(8):
                    norm_tile_to_hT(bufs[t % 2], x_own[t * 128:(t + 1) * 128, :], hT_own, t * 128, ("n", t % 2))
                S.barrier()
            attention_phase(nc, S, stB, sb, psb, ps_bf, cs, identb, half_t, W, C, tv_d, hT_all, hT_own, oaT, obT, False, dbg_tensor)
            S.barrier()
        mergedT = stR.enter_context(nc.sbuf_tensor("mergedT", [128, 16, NOWN], BF16, side="right"))
        merge_phase(nc, S, sb, psb, W, hT_own, oaT, obT, mergedT)
        S.barrier()
        stA.close()
        h2T = sb(st0, "h2T", [128, 16, NOWN], BF16)
        Gt = sb(st0, "Gt", [128, 8, 64], F32)
        resid_phase(nc, S, sb, psb, W, mergedT, x_own, x1_d, mod_d)
        S.barrier()
        stR.close()
        norm_router_phase(nc, S, sb, psb, W, cs, x1_d, h2T, Gt, Gf_col, shf_col, eps_t)
        S.barrier()
        if dbg:
            d = dbg_tensor("Gt", [128, 8 * 64])
            S.dma("sp", lambda: nc.sync.dma_start(out=d[:, :], in_=Gt[:].rearrange("p a b -> p (a b)")))
            d2 = dbg_tensor("h2T", [128, 16 * NOWN])
            with ExitStack() as st:
                tmpf = sb(st, "dbgtmp", [128, NOWN], F32)
                for c in range(16):
                    S.op("dve", lambda c=c: V.tensor_copy(out=tmpf[:], in_=h2T[:, c, :]), writes=["dbgtmp"])
                    S.dma("sp", lambda c=c: nc.sync.dma_start(out=d2[:, c * NOWN:(c + 1) * NOWN], in_=tmpf[:]), reads=["dbgtmp"])
                S.barrier()
        if stage <= 4:
            S.finish("sp")
            return nc, dbg_out
        acc = sb(st0, "acc", [128, 8, D], F32)
        if stage == 5:
            for t in range(8):
                S.op("dve", lambda: V.memset(acc[:, t, :], 0.0), writes=[("acc", t)])
        else:
            moe_phase(nc, S, sb, psb, W, h2T, Gt, acc)
        S.barrier()
        final_phase(nc, S, sb, W, acc, x1_d, mod_d, out_own, eps_t)
        S.finish("sp")
    return nc, dbg_out


def attention_phase(nc, S, stB, sb, psb, ps_bf, cs, identb, half_t, W, C, tv_d, hT_all, hT_own, oaT, obT, dbg, dbg_tensor):
    V, A_, P_, T_ = nc.vector, nc.scalar, nc.gpsimd, nc.tensor
    with ExitStack() as st:
        tab = sb(st, "tab", [32, 8], F32)
        ohb = sb(st, "ohb", [32, 640], F32)
        negrow = sb(st, "negrow", [1, 640], F32)
        tvs = sb(st, "tvs", [8, 640], F32)
        b31 = sb(st, "b31", [128, 8], F32)
        S.dma("sp", lambda: nc.sync.dma_start(out=tab[:], in_=W["rel_bias_table"][:, :]), writes=["tab"])
        S.dma("sp", lambda: nc.sync.dma_start(out=ohb[:], in_=C["ohb"][:, :]), writes=["ohb"])
        S.dma("sp", lambda: nc.sync.dma_start(out=negrow[:], in_=C["negrow"][:, :]), writes=["negrow"])
        S.dma("sp", lambda: nc.sync.dma_start(out=b31[:], in_=bcast_rows(W["rel_bias_table"][31:32, :])), writes=["b31"])
        for half in range(2):
            pb = psb[half]
            S.op("pe", lambda half=half, pb=pb: T_.matmul(pb[0:8, 0:320], lhsT=tab[:, :], rhs=ohb[:, half * 320:(half + 1) * 320], start=True, stop=False),
                 reads=["tab", "ohb"], writes=[("psb", half)])
            S.op("pe", lambda half=half, pb=pb: T_.matmul(pb[0:8, 0:320], lhsT=cs["ones"][0:1, 0:8], rhs=negrow[0:1, half * 320:(half + 1) * 320], start=False, stop=True),
                 reads=["c_ones", "negrow"], writes=[("psb", half)])
            S.op("act", lambda half=half, pb=pb: A_.mul(out=tvs[:, half * 320:(half + 1) * 320], in_=pb[0:8, 0:320], mul=8.0),
                 reads=[("psb", half)], writes=["tvs"])
        S.dma("sp", lambda: nc.sync.dma_start(out=tv_d.ap()[:, :], in_=tvs[:]), reads=["tvs"], writes=["tv_d"])

        lam4 = sb(st, "lam4", [128, 4, 64], F32)
        for i, nm in enumerate(("lq1", "lk1", "lq2", "lk2")):
            S.dma("sp", lambda i=i, nm=nm: nc.sync.dma_start(out=lam4[:, i, :], in_=W[nm][0:1, :].partition_broadcast(128)), writes=[("lam4", i)])
        lsum = sb(st, "lsum", [128, 2], F32)
        ljunk = sb(st, "ljunk", [128, 64], F32)
        for i in range(2):
            S.op("dve", lambda i=i: V.tensor_tensor(out=ljunk[:], in0=lam4[:, 2 * i, :], in1=lam4[:, 2 * i + 1, :], op=ALU.mult),
                 reads=[("lam4", 2 * i), ("lam4", 2 * i + 1)], writes=["ljunk"])
            S.op("dve", lambda i=i: V.tensor_reduce(out=lsum[:, i:i + 1], in_=ljunk[:], axis=AX.X, op=ALU.add),
                 reads=["ljunk"], writes=[("lsum", i)])
        S.op("act", lambda: A_.activation(out=lsum[:], in_=lsum[:], func=AF.Exp), reads=[("lsum", 0), ("lsum", 1)], writes=["lsume"])
        nlam = sb(st, "nlam", [128, 1], F32)
        S.op("dve", lambda: V.tensor_tensor(out=nlam[:], in0=lsum[:, 1:2], in1=lsum[:, 0:1], op=ALU.subtract), reads=["lsume"], writes=["nlam"])
        S.op("dve", lambda: V.tensor_scalar(out=nlam[:], in0=nlam[:], scalar1=-LAM_INIT, scalar2=None, op0=ALU.add), reads=["nlam"], writes=["nlam"])
        gs8 = sb(st, "gs8", [128, 128], F32)
        S.dma("sp", lambda: nc.sync.dma_start(out=gs8[:], in_=bcast_rows(W["g_subln"][0:1, :])), writes=["gs8"])
        S.op("dve", lambda: V.tensor_scalar(out=gs8[:], in0=gs8[:], scalar1=(1.0 - LAM_INIT), scalar2=None, op0=ALU.mult), reads=["gs8"], writes=["gs8"])
        eps_t = sb(st, "eps_t2", [128, 1], F32)
        S.op("dve", lambda: V.memset(eps_t[:], EPS), writes=["eps2"])

        tri, ones = cs["tri"], cs["ones"]
        omt = sb(st, "omt", [128, 128], F32)
        S.op("dve", lambda: V.tensor_tensor(out=omt[:], in0=ones[:], in1=tri[:], op=ALU.subtract), reads=["c_ones", "c_tri"], writes=["omt"])
        mk = {n: sb(st, "mk_" + n, [128, 128], F32) for n in ("XB", "XC", "YB", "YC")}
        omh = sb(st, "omh", [128, 1], F32)
        S.op("dve", lambda: V.tensor_scalar(out=omh[:], in0=half_t[:], scalar1=-1.0, scalar2=1.0, op0=ALU.mult, op1=ALU.add), reads=["half"], writes=["omh"])
        S.op("dve", lambda: V.scalar_tensor_tensor(out=mk["XB"][:], in0=omt[:], scalar=half_t[:, 0:1], in1=tri[:], op0=ALU.mult, op1=ALU.add),
             reads=["omt", "half", "c_tri"], writes=["mk_XB"])
        S.op("dve", lambda: V.tensor_scalar(out=mk["XC"][:], in0=tri[:], scalar1=half_t[:, 0:1], scalar2=None, op0=ALU.mult), reads=["c_tri", "half"], writes=["mk_XC"])
        S.op("dve", lambda: V.scalar_tensor_tensor(out=mk["YB"][:], in0=omt[:], scalar=omh[:, 0:1], in1=tri[:], op0=ALU.mult, op1=ALU.add),
             reads=["omt", "omh", "c_tri"], writes=["mk_YB"])
        S.op("dve", lambda: V.tensor_scalar(out=mk["YC"][:], in0=tri[:], scalar1=omh[:, 0:1], scalar2=None, op0=ALU.mult), reads=["c_tri", "omh"], writes=["mk_YC"])

        wq = [sb(st, "wq%d" % i, [128, 16, 128], BF16) for i in range(3)]
        qT = sb(st, "qT", [128, NOWN], BF16)
        kT = sb(st, "kT", [128, SEQ], BF16)
        vA = sb(st, "vA", [128, 16, 130], BF16)
        Ht = [sb(st, "Ht%d" % i, [128, 128], F32) for i in range(4)]
        slot = {n: sb(st, "slot" + n, [128, 128], F32) for n in ("XA", "XB", "XC", "YA", "YB", "YC")}
        hd = sb(st, "hd", [128, 128], F32)
        PT = [sb(st, "PT%d" % i, [128, 128], BF16) for i in range(4)]
        e_t = [sb(st, "e_t%d" % i, [128, 128], F32) for i in range(2)]
        lnp = [sb(st, "lnp%d" % i, [128, 128], F32) for i in range(2)]
        LK = [sb(st, "LK%d" % i, [128, 128], F32) for i in range(2)]
        arg = [sb(st, "arg%d" % i, [128, 128], F32) for i in range(2)]
        Acc = sb(st, "Acc", [128, 128], F32)
        o1 = sb(st, "o1", [128, 128], F32)
        o2 = sb(st, "o2", [128, 128], F32)
        obf = sb(st, "obf", [128, 128], BF16)
        rr = sb(st, "rr", [128, 4], F32)
        sjunk = sb(st, "sjunk", [128, 128], F32)
        S.op("dve", lambda: V.memset(vA[:], 1.0), writes=["vA_init"])

        w_in_r = W["w_in"].rearrange("(c p) n -> p c n", p=128)
        SC_A = 64 ** -0.5
        SC_B = 128 ** -0.5

        def own_tile_info(j):
            p = j // 2
            if j % 2 == 0:
                return 2 * p + 2, "X", (2 * p - 1, 2 * p, 2 * p + 1)
            return 16 - 2 * p, "Y", (13 - 2 * p, 14 - 2 * p, 15 - 2 * p)

        def project_head(h, colbase, with_ones):
            for i, off in enumerate((0, 1024, 2048)):
                c0 = colbase + off + h * 128
                for hh in range(2):
                    S.dma("pool", lambda i=i, c0=c0, hh=hh: nc.gpsimd.dma_start(out=wq[i][:, hh * 8:(hh + 1) * 8, :], in_=w_in_r[:, hh * 8:(hh + 1) * 8, c0:c0 + 128]),
                          writes=[("wq", i, hh)])
            n = 0
            for ch in range(2):
                pb = psb[n % 2]; n += 1
                for c in range(16):
                    S.op("pe", lambda c=c, ch=ch, pb=pb: T_.matmul(pb[:, :], lhsT=wq[0][:, c, :], rhs=hT_own[:, c, ch * 512:(ch + 1) * 512], start=(c == 0), stop=(c == 15)),
                         reads=[("wq", 0, c // 8)], writes=[("psb", (n - 1) % 2)])
                S.op("act", lambda ch=ch, pb=pb: A_.copy(out=qT[:, ch * 512:(ch + 1) * 512], in_=pb[:, :]), reads=[("psb", (n - 1) % 2)], writes=[("qT", ch)])
            for ch in range(4):
                pb = psb[n % 2]; n += 1
                for c in range(16):
                    S.op("pe", lambda c=c, ch=ch, pb=pb: T_.matmul(pb[:, :], lhsT=wq[1][:, c, :], rhs=hT_all[:, c, ch * 512:(ch + 1) * 512], start=(c == 0), stop=(c == 15)),
                         reads=[("wq", 1, c // 8)], writes=[("psb", (n - 1) % 2)])
                S.op("dve", lambda ch=ch, pb=pb: V.tensor_copy(out=kT[:, ch * 512:(ch + 1) * 512], in_=pb[:, :]), reads=[("psb", (n - 1) % 2)], writes=[("kT", ch)])
            for kb4 in range(4):
                pb = psb[n % 2]; n += 1
                for k4 in range(4):
                    kb = kb4 * 4 + k4
                    for c in range(16):
                        S.op("pe", lambda c=c, kb=kb, k4=k4, pb=pb: T_.matmul(pb[:, k4 * 128:(k4 + 1) * 128], lhsT=hT_all[:, c, kb * 128:(kb + 1) * 128], rhs=wq[2][:, c, :], start=(c == 0), stop=(c == 15)),
                             reads=[("wq", 2, c // 8)], writes=[("psb", (n - 1) % 2)])
                eng = "act" if kb4 % 2 == 0 else "dve"
                if eng == "act":
                    S.op("act", lambda kb4=kb4, pb=pb: A_.copy(out=vA[:, kb4 * 4:(kb4 + 1) * 4, 0:128], in_=pb[:, :].rearrange("p (a b) -> p a b", a=4)),
                         reads=[("psb", (n - 1) % 2), "vA_init"], writes=[("vA", kb4)])
                else:
                    S.op("dve", lambda kb4=kb4, pb=pb: V.tensor_copy(out=vA[:, kb4 * 4:(kb4 + 1) * 4, 0:128], in_=pb[:, :].rearrange("p (a b) -> p a b", a=4)),
                         reads=[("psb", (n - 1) % 2), "vA_init"], writes=[("vA", kb4)])

        def qk_keys(j, kb):
            return [("qT", j // 4), ("kT", kb // 4)]

        for h in range(NH):
            project_head(h, 0, True)
            for i, dl in enumerate((-128, 0, 128, 256)):
                src = bass.AP(tv_d, h * 640 + 129 + dl, [[1, 128], [1, 128]])
                S.dma("sp", lambda i=i, src=src: nc.sync.dma_start(out=Ht[i][:], in_=src), reads=["tv_d"], writes=[("Ht", i)])
            for nm, lo, hi in (("XA", 2, 3), ("XB", 1, 2), ("XC", 0, 1), ("YA", 3, 2), ("YB", 2, 1), ("YC", 1, 0)):
                S.op("dve", lambda lo=lo, hi=hi: V.tensor_tensor(out=hd[:], in0=Ht[hi][:], in1=Ht[lo][:], op=ALU.subtract),
                     reads=[("Ht", lo), ("Ht", hi)], writes=["hd"])
                S.op("dve", lambda nm=nm, lo=lo: V.scalar_tensor_tensor(out=slot[nm][:], in0=hd[:], scalar=half_t[:, 0:1], in1=Ht[lo][:], op0=ALU.mult, op1=ALU.add),
                     reads=["hd", "half", ("Ht", lo)], writes=[("slot", nm)])
            for j in range(8):
                L, sset, slots = own_tile_info(j)
                it = 0
                for m in range(2):
                    rows = slice(64 * m, 64 * m + 64)
                    ob = 4 + m
                    for kb in range(L):
                        sbk = 2 + (it % 2)
                        pt = PT[it % 4]
                        it += 1
                        sl = None
                        if kb in slots:
                            sl = sset + "ABC"[slots.index(kb)]
                        S.op("pe", lambda kb=kb, j=j, rows=rows, sbk=sbk, sl=sl: T_.matmul(psb[sbk][:, 0:128], lhsT=kT[rows, kb * 128:(kb + 1) * 128], rhs=qT[rows, j * 128:(j + 1) * 128], start=True, stop=(sl is None)),
                             reads=qk_keys(j, kb), writes=[("psb", sbk)])
                        if sl is not None:
                            S.op("pe", lambda sbk=sbk, sl=sl: T_.matmul(psb[sbk][:, 0:128], lhsT=cs["antiI"][:], rhs=slot[sl][:], start=False, stop=True),
                                 reads=["c_antiI", ("slot", sl)], writes=[("psb", sbk)])
                        S.op("act", lambda sbk=sbk, pt=pt, h=h: A_.activation(out=pt[:], in_=psb[sbk][:, 0:128], func=AF.Exp, bias=b31[:, h:h + 1], scale=SC_A),
                             reads=[("psb", sbk), "b31"], writes=[("PT", (it - 1) % 4)])
                        S.op("pe", lambda kb=kb, ob=ob, pt=pt, L=L: T_.matmul(psb[ob][:, 0:130], lhsT=pt[:], rhs=vA[:, kb, :], start=(kb == 0), stop=(kb == L - 1)),
                             reads=[("PT", (it - 1) % 4), ("vA", kb // 4)], writes=[("psb", ob)])
                S.op("dve", lambda: V.reciprocal(out=rr[:, 0:1], in_=psb[4][:, 128:129]), reads=[("psb", 4)], writes=["rr0"])
                S.op("dve", lambda: V.reciprocal(out=rr[:, 1:2], in_=psb[5][:, 128:129]), reads=[("psb", 5)], writes=["rr1"])
                S.op("dve", lambda: V.tensor_tensor(out=rr[:, 1:2], in0=rr[:, 1:2], in1=nlam[:], op=ALU.mult), reads=["rr1", "nlam"], writes=["rr1"])
                S.op("dve", lambda: V.tensor_scalar(out=o1[:], in0=psb[4][:, 0:128], scalar1=rr[:, 0:1], scalar2=None, op0=ALU.mult), reads=[("psb", 4), "rr0"], writes=["o1"])
                S.op("dve", lambda: V.scalar_tensor_tensor(out=o2[:], in0=psb[5][:, 0:128], scalar=rr[:, 1:2], in1=o1[:], op0=ALU.mult, op1=ALU.add),
                     reads=[("psb", 5), "rr1", "o1"], writes=["o2"])
                S.op("act", lambda: A_.activation(out=sjunk[:], in_=o2[:], func=AF.Square, accum_out=rr[:, 2:3]), reads=["o2"], writes=["rr2", "sjunk"])
                S.op("act", lambda: A_.activation(out=rr[:, 2:3], in_=rr[:, 2:3], func=AF.Sqrt, bias=eps_t[:], scale=1.0 / 128), reads=["rr2", "eps2"], writes=["rr2"])
                S.op("dve", lambda: V.reciprocal(out=rr[:, 2:3], in_=rr[:, 2:3]), reads=["rr2"], writes=["rr2"])
                S.op("dve", lambda: V.scalar_tensor_tensor(out=obf[:], in0=o2[:], scalar=rr[:, 2:3], in1=gs8[:], op0=ALU.mult, op1=ALU.mult),
                     reads=["o2", "rr2", "gs8"], writes=["obf"])
                ptb = ps_bf(6)
                S.op("pe", lambda ptb=ptb: T_.transpose(ptb[:, 0:128], obf[:], identb[:]), reads=["obf", "identb"], writes=[("psb", 6)])
                S.op("act", lambda h=h, j=j, ptb=ptb: A_.copy(out=oaT[:, h, j * 128:(j + 1) * 128], in_=ptb[:, 0:128]), reads=[("psb", 6)], writes=[("oaT", h)])

        for h in range(NH):
            project_head(h, 3072, False)
            for j in range(8):
                L, sset, slots = own_tile_info(j)
                it = 0
                first = True
                for kb in range(L - 1, -1, -1):
                    zb = 2 + (it % 2)
                    lb = 5 if it % 2 == 0 else 7
                    b2 = it % 2
                    pt = PT[it % 4]
                    it += 1
                    mkt = None
                    if kb in slots:
                        nm = sset + "ABC"[slots.index(kb)]
                        if nm[1] != "A":
                            mkt = nm
                    S.op("pe", lambda kb=kb, j=j, zb=zb: T_.matmul(psb[zb][:, 0:128], lhsT=kT[:, kb * 128:(kb + 1) * 128], rhs=qT[:, j * 128:(j + 1) * 128], start=True, stop=True),
                         reads=qk_keys(j, kb), writes=[("psb", zb)])
                    S.op("act", lambda zb=zb, b2=b2: A_.activation(out=e_t[b2][:], in_=psb[zb][:, 0:128], func=AF.Exp, scale=-SC_B), reads=[("psb", zb)], writes=[("e_t", b2)])
                    S.op("act", lambda b2=b2: A_.activation(out=lnp[b2][:], in_=e_t[b2][:], func=AF.Ln, bias=1.0, scale=1.0), reads=[("e_t", b2)], writes=[("lnp", b2)])
                    S.op("dve", lambda zb=zb, b2=b2: V.scalar_tensor_tensor(out=LK[b2][:], in0=psb[zb][:, 0:128], scalar=-SC_B, in1=lnp[b2][:], op0=ALU.mult, op1=ALU.subtract),
                         reads=[("psb", zb), ("lnp", b2)], writes=[("LK", b2)])
                    if mkt is not None:
                        S.op("pool", lambda b2=b2, mkt=mkt: P_.tensor_tensor(out=LK[b2][:], in0=LK[b2][:], in1=mk[mkt][:], op=ALU.mult),
                             reads=[("LK", b2), "mk_" + mkt], writes=[("LK", b2)])
                    S.op("pe", lambda b2=b2, lb=lb, first=first: T_.matmul(psb[lb][:, 0:128], lhsT=cs["ugt"][:], rhs=LK[b2][:], start=True, stop=first),
                         reads=["c_ugt", ("LK", b2)], writes=[("psb", lb)])
                    if not first:
                        S.op("pe", lambda lb=lb: T_.matmul(psb[lb][:, 0:128], lhsT=cs["ones"][:], rhs=Acc[:], start=False, stop=True),
                             reads=["c_ones", "Acc"], writes=[("psb", lb)])
                    S.op("dve", lambda lb=lb, b2=b2: V.tensor_tensor(out=arg[b2][:], in0=psb[lb][:, 0:128], in1=lnp[b2][:], op=ALU.subtract),
                         reads=[("psb", lb), ("lnp", b2)], writes=[("arg", b2)])
                    S.op("act", lambda b2=b2, pt=pt: A_.activation(out=pt[:], in_=arg[b2][:], func=AF.Exp), reads=[("arg", b2)], writes=[("PT", (it - 1) % 4)])
                    if mkt is not None:
                        S.op("pool", lambda pt=pt, mkt=mkt: P_.tensor_tensor(out=pt[:], in0=pt[:], in1=mk[mkt][:], op=ALU.mult),
                             reads=[("PT", (it - 1) % 4), "mk_" + mkt], writes=[("PT", (it - 1) % 4)])
                    S.op("pe", lambda kb=kb, pt=pt, first=first: T_.matmul(psb[4][:, 0:128], lhsT=pt[:], rhs=vA[:, kb, 0:128], start=first, stop=(kb == 0)),
                         reads=[("PT", (it - 1) % 4), ("vA", kb // 4)], writes=[("psb", 4)])
                    if kb > 0:
                        if first:
                            S.op("pool", lambda b2=b2: P_.tensor_copy(out=Acc[:], in_=LK[b2][:]), reads=[("LK", b2)], writes=["Acc"])
                        else:
                            S.op("pool", lambda b2=b2: P_.tensor_tensor(out=Acc[:], in0=Acc[:], in1=LK[b2][:], op=ALU.add), reads=[("LK", b2), "Acc"], writes=["Acc"])
                    first = False
                S.op("dve", lambda: V.tensor_copy(out=obf[:], in_=psb[4][:, 0:128]), reads=[("psb", 4)], writes=["obf"])
                ptb = ps_bf(6)
                S.op("pe", lambda ptb=ptb: T_.transpose(ptb[:, 0:128], obf[:], identb[:]), reads=["obf", "identb"], writes=[("psb", 6)])
                S.op("act", lambda h=h, j=j, ptb=ptb: A_.copy(out=obT[:, h, j * 128:(j + 1) * 128], in_=ptb[:, 0:128]), reads=[("psb", 6)], writes=[("obT", h)])
        if dbg:
            for nm, tt in (("oaT", oaT), ("obT", obT)):
                d = dbg_tensor(nm, [128, NH * NOWN])
                tmp = sb(st, "dtmp_" + nm, [128, NOWN], F32)
                for h in range(NH):
                    S.op("dve", lambda h=h, tt=tt, tmp=tmp: V.tensor_copy(out=tmp[:], in_=tt[:, h, :]), reads=[(nm, h)], writes=["dbg_" + nm])
                    S.dma("sp", lambda h=h, d=d, tmp=tmp: nc.sync.dma_start(out=d[:, h * NOWN:(h + 1) * NOWN], in_=tmp[:]), reads=["dbg_" + nm])


def merge_phase(nc, S, sb, psb, W, hT_own, oaT, obT, mergedT):
    V, A_, P_, T_ = nc.vector, nc.scalar, nc.gpsimd, nc.tensor
    with ExitStack() as st:
        wpa = [sb(st, "wpa%d" % i, [128, 8, 128], BF16) for i in range(2)]
        wpb = [sb(st, "wpb%d" % i, [128, 8, 128], BF16) for i in range(2)]
        wga = [sb(st, "wga%d" % i, [128, 16, 128], BF16) for i in range(2)]
        wgb = [sb(st, "wgb%d" % i, [128, 16, 128], BF16) for i in range(2)]
        sg = [[sb(st, "sg%d_%d" % (i, j), [128, 512], F32) for j in range(2)] for i in range(2)]
        tt = [[sb(st, "tt%d_%d" % (i, j), [128, 512], F32) for j in range(2)] for i in range(2)]
        wpa_r = W["w_proj_a"].rearrange("(k p) n -> p k n", p=128)
        wpb_r = W["w_proj_b"].rearrange("(k p) n -> p k n", p=128)
        w_in_r = W["w_in"].rearrange("(c p) n -> p c n", p=128)
        it = 0
        for m in range(16):
            b = m % 2
            cs_ = slice(m * 128, (m + 1) * 128)
            S.dma("pool", lambda: nc.gpsimd.dma_start(out=wpa[b][:], in_=wpa_r[:, :, cs_]), writes=[("wpa", b)])
            S.dma("pool", lambda: nc.gpsimd.dma_start(out=wpb[b][:], in_=wpb_r[:, :, cs_]), writes=[("wpb", b)])
            for hh in range(2):
                S.dma("pool", lambda: nc.gpsimd.dma_start(out=wga[b][:, hh * 8:(hh + 1) * 8, :], in_=w_in_r[:, hh * 8:(hh + 1) * 8, 6144 + m * 128:6144 + (m + 1) * 128]),
                      writes=[("wga", b, hh)])
                S.dma("pool", lambda: nc.gpsimd.dma_start(out=wgb[b][:, hh * 8:(hh + 1) * 8, :], in_=w_in_r[:, hh * 8:(hh + 1) * 8, 8192 + m * 128:8192 + (m + 1) * 128]),
                      writes=[("wgb", b, hh)])
            for th in range(2):
                base = 4 * (it % 2)
                q = it % 2
                it += 1
                tok = slice(th * 512, (th + 1) * 512)
                for k in range(8):
                    S.op("pe", lambda: T_.matmul(psb[base][:, :], lhsT=wpa[b][:, k, :], rhs=oaT[:, k, tok], start=(k == 0), stop=(k == 7)),
                         reads=[("wpa", b)], writes=[("psb", base)])
                for k in range(8):
                    S.op("pe", lambda: T_.matmul(psb[base + 1][:, :], lhsT=wpb[b][:, k, :], rhs=obT[:, k, tok], start=(k == 0), stop=(k == 7)),
                         reads=[("wpb", b)], writes=[("psb", base + 1)])
                for c in range(16):
                    S.op("pe", lambda: T_.matmul(psb[base + 2][:, :], lhsT=wga[b][:, c, :], rhs=hT_own[:, c, tok], start=(c == 0), stop=(c == 15)),
                         reads=[("wga", b, c // 8)], writes=[("psb", base + 2)])
                for c in range(16):
                    S.op("pe", lambda: T_.matmul(psb[base + 3][:, :], lhsT=wgb[b][:, c, :], rhs=hT_own[:, c, tok], start=(c == 0), stop=(c == 15)),
                         reads=[("wgb", b, c // 8)], writes=[("psb", base + 3)])
                S.op("act", lambda: A_.activation(out=sg[q][0][:], in_=psb[base + 2][:, :], func=AF.Sigmoid), reads=[("psb", base + 2)], writes=[("sg", q, 0)])
                S.op("act", lambda: A_.activation(out=sg[q][1][:], in_=psb[base + 3][:, :], func=AF.Sigmoid), reads=[("psb", base + 3)], writes=[("sg", q, 1)])
                S.op("dve", lambda: V.tensor_tensor(out=tt[q][0][:], in0=psb[base][:, :], in1=sg[q][0][:], op=ALU.mult),
                     reads=[("psb", base), ("sg", q, 0)], writes=[("tt", q, 0)])
                S.op("dve", lambda: V.tensor_tensor(out=tt[q][1][:], in0=psb[base + 1][:, :], in1=sg[q][1][:], op=ALU.mult),
                     reads=[("psb", base + 1), ("sg", q, 1)], writes=[("tt", q, 1)])
                S.op("pool", lambda: P_.tensor_tensor(out=mergedT[:, m, tok], in0=tt[q][0][:], in1=tt[q][1][:], op=ALU.add),
                     reads=[("tt", q, 0), ("tt", q, 1)], writes=[("mergedT", m, th)])
        S.barrier()


def resid_phase(nc, S, sb, psb, W, mergedT, x_own, x1_d, mod_d):
    V, A_, P_, T_ = nc.vector, nc.scalar, nc.gpsimd, nc.tensor
    with ExitStack() as st:
        wo = [sb(st, "wo%d" % i, [128, 16, 512], BF16) for i in range(2)]
        gm_bc = sb(st, "gm_bc", [128, D], F32)
        xo = [sb(st, "xo%d" % i, [128, 512], F32) for i in range(2)]
        tmp = [sb(st, "rtmp%d" % i, [128, 512], F32) for i in range(2)]
        x1c = [sb(st, "x1c%d" % i, [128, 512], F32) for i in range(2)]
        S.dma("sp", lambda: nc.sync.dma_start(out=gm_bc[:], in_=bcast_rows(mod_d[2:3, :])), writes=["gm_bc"])
        w_out_r = W["w_out"].rearrange("(c p) n -> p c n", p=128)
        it = 0
        for n in range(4):
            b = n % 2
            ns = slice(n * 512, (n + 1) * 512)
            for q in range(4):
                S.dma("pool", lambda: nc.gpsimd.dma_start(out=wo[b][:, q * 4:(q + 1) * 4, :], in_=w_out_r[:, q * 4:(q + 1) * 4, ns]), writes=[("wo", b, q)])
            for t in range(8):
                pb = it % 4
                i2 = it % 2
                it += 1
                ts_ = slice(t * 128, (t + 1) * 128)
                S.dma("sp", lambda: nc.sync.dma_start(out=xo[i2][:], in_=x_own[ts_, ns]), writes=[("xo", i2)])
                for c in range(16):
                    S.op("pe", lambda: T_.matmul(psb[pb][:, :], lhsT=mergedT[:, c, ts_], rhs=wo[b][:, c, :], start=(c == 0), stop=(c == 15)),
                         reads=[("wo", b, c // 4)], writes=[("psb", pb)])
                S.op("dve", lambda: V.tensor_tensor(out=tmp[i2][:], in0=psb[pb][:, :], in1=gm_bc[:, ns], op=ALU.mult),
                     reads=[("psb", pb), "gm_bc"], writes=[("rtmp", i2)])
                S.op("pool", lambda: P_.tensor_tensor(out=x1c[i2][:], in0=tmp[i2][:], in1=xo[i2][:], op=ALU.add),
                     reads=[("rtmp", i2), ("xo", i2)], writes=[("x1c", i2)])
                S.dma("sp", lambda: nc.sync.dma_start(out=x1_d[ts_, ns], in_=x1c[i2][:]), reads=[("x1c", i2)], writes=[("x1d", t, n)])
        S.barrier()


def norm_router_phase(nc, S, sb, psb, W, cs, x1_d, h2T, Gt, Gf_col, shf_col, eps_t):
    V, A_, P_, T_ = nc.vector, nc.scalar, nc.gpsimd, nc.tensor
    BIG = 30000.0
    with ExitStack() as st:
        x1t = [sb(st, "x1t%d" % i, [128, D], F32) for i in range(2)]
        xn = [sb(st, "xn%d" % i, [128, D], F32) for i in range(2)]
        hf = [sb(st, "hf%d" % i, [128, 16, 128], F32) for i in range(2)]
        junk = sb(st, "njunk", [128, D], BF16)
        ss = [sb(st, "nss%d" % i, [128, 1], F32) for i in range(2)]
        rs = [sb(st, "nrs%d" % i, [128, 1], F32) for i in range(2)]
        wr = sb(st, "wr", [128, 16, 72], F32)
        brt = sb(st, "brt", [128, 72], F32)
        lg = sb(st, "lg", [128, 72], F32)
        sm = {n: sb(st, "r_" + n, [128, 1], F32) for n in ("gmax", "ngmax", "gsum", "pg", "m1", "m2", "dd", "rr", "den", "p1", "c1", "c2")}
        ohg = sb(st, "ohg", [128, 8], F32)
        pen = sb(st, "pen", [128, 8], F32)
        gjunk = sb(st, "gjunk", [128, 8], F32)
        em = sb(st, "em", [128, 64], F32)
        em2 = sb(st, "em2", [128, 64], F32)
        mask1 = sb(st, "mask1", [128, 64], F32)
        mask2 = sb(st, "mask2", [128, 64], F32)
        w_rg_r = W["w_rg"].rearrange("(c p) n -> p c n", p=128)
        w_re_r = W["w_re"].rearrange("(c p) n -> p c n", p=128)
        for hh in range(2):
            S.dma("sp", lambda: nc.sync.dma_start(out=wr[:, hh * 8:(hh + 1) * 8, 0:8], in_=w_rg_r[:, hh * 8:(hh + 1) * 8, :]), writes=[("wr", 0, hh)])
            S.dma("sp", lambda: nc.sync.dma_start(out=wr[:, hh * 8:(hh + 1) * 8, 8:72], in_=w_re_r[:, hh * 8:(hh + 1) * 8, :]), writes=[("wr", 1, hh)])
        S.dma("sp", lambda: nc.sync.dma_start(out=brt[:, 0:8], in_=bcast_rows(W["b_rg"][0:1, :])), writes=[("brt", 0)])
        S.dma("sp", lambda: nc.sync.dma_start(out=brt[:, 8:72], in_=bcast_rows(W["b_re"][0:1, :])), writes=[("brt", 1)])
        wr_keys = [("wr", 0, 0), ("wr", 0, 1), ("wr", 1, 0), ("wr", 1, 1)]
        for t in range(8):
            b = t % 2
            ts_ = slice(t * 128, (t + 1) * 128)
            for hh in range(2):
                S.dma("sp", lambda: nc.sync.dma_start(out=x1t[b][:, hh * 1024:(hh + 1) * 1024], in_=x1_d[ts_, hh * 1024:(hh + 1) * 1024]), writes=[("x1t", b, hh)])
            S.op("act", lambda: A_.activation(out=junk[:], in_=x1t[b][:], func=AF.Square, accum_out=ss[b][:]),
                 reads=[("x1t", b, 0), ("x1t", b, 1)], writes=[("nss", b), "njunk"])
            S.op("act", lambda: A_.activation(out=rs[b][:], in_=ss[b][:], func=AF.Sqrt, bias=eps_t[:], scale=1.0 / D),
                 reads=[("nss", b), "eps"], writes=[("nrs", b)])
            S.op("dve", lambda: V.reciprocal(out=rs[b][:], in_=rs[b][:]), reads=[("nrs", b)], writes=[("nrs", b)])
            S.op("dve", lambda: V.tensor_scalar(out=xn[b][:], in0=x1t[b][:], scalar1=rs[b][:, 0:1], scalar2=None, op0=ALU.mult),
                 reads=[("x1t", b, 0), ("x1t", b, 1), ("nrs", b)], writes=[("xn", b)])
            for g in range(4):
                for k in range(4):
                    c = g * 4 + k
                    S.op("pe", lambda: T_.matmul(psb[g][:, k * 128:(k + 1) * 128], lhsT=xn[b][:, c * 128:(c + 1) * 128], rhs=cs["ident"][:], start=True, stop=True),
                         reads=[("xn", b), "c_ident"], writes=[("psb", g)])
                for k in range(4):
                    c = g * 4 + k
                    S.op("act", lambda: A_.activation(out=hf[b][:, c, :], in_=psb[g][:, k * 128:(k + 1) * 128], func=AF.Identity,
                                                      bias=shf_col[:, c:c + 1], scale=Gf_col[:, c:c + 1]),
                         reads=[("psb", g), "shf_col", "Gf_col"], writes=[("hf", b, g)])
            S.op("pool", lambda: P_.tensor_copy(out=h2T[:, :, ts_], in_=hf[b][:, :, :]), reads=[("hf", b, g) for g in range(4)], writes=[("h2T", t)])
            rb = 4 + b
            for c in range(16):
                S.op("pe", lambda: T_.matmul(psb[rb][:, 0:72], lhsT=hf[b][:, c, :], rhs=wr[:, c, :], start=(c == 0), stop=(c == 15)),
                     reads=[("hf", b, c // 4)] + wr_keys, writes=[("psb", rb)])
            S.op("dve", lambda: V.tensor_tensor(out=lg[:], in0=psb[rb][:, 0:72], in1=brt[:], op=ALU.add), reads=[("psb", rb), ("brt", 0), ("brt", 1)], writes=["lg"])
            S.op("dve", lambda: V.tensor_reduce(out=sm["gmax"][:], in_=lg[:, 0:8], axis=AX.X, op=ALU.max), reads=["lg"], writes=["gmax"])
            S.op("dve", lambda: V.tensor_scalar(out=ohg[:], in0=lg[:, 0:8], scalar1=sm["gmax"][:, 0:1], scalar2=None, op0=ALU.is_ge), reads=["lg", "gmax"], writes=["ohg"])
            S.op("dve", lambda: V.tensor_scalar(out=sm["ngmax"][:], in0=sm["gmax"][:], scalar1=-1.0, scalar2=None, op0=ALU.mult), reads=["gmax"], writes=["ngmax"])
            S.op("act", lambda: A_.activation(out=gjunk[:], in_=lg[:, 0:8], func=AF.Exp, bias=sm["ngmax"][:, 0:1], scale=1.0, accum_out=sm["gsum"][:]),
                 reads=["lg", "ngmax"], writes=["gsum", "gjunk"])
            S.op("dve", lambda: V.reciprocal(out=sm["pg"][:], in_=sm["gsum"][:]), reads=["gsum"], writes=["pg"])
            S.op("dve", lambda: V.tensor_scalar(out=pen[:], in0=ohg[:], scalar1=BIG, scalar2=-BIG, op0=ALU.mult, op1=ALU.add), reads=["ohg"], writes=["pen"])
            for g in range(8):
                S.op("dve", lambda: V.tensor_scalar(out=em[:, g * 8:(g + 1) * 8], in0=lg[:, 8 + g * 8:16 + g * 8], scalar1=pen[:, g:g + 1], scalar2=None, op0=ALU.add),
                     reads=["lg", "pen"], writes=[("em", g)])
            emk = [("em", g) for g in range(8)]
            S.op("dve", lambda: V.tensor_reduce(out=sm["m1"][:], in_=em[:], axis=AX.X, op=ALU.max), reads=emk, writes=["m1"])
            S.op("dve", lambda: V.tensor_scalar(out=mask1[:], in0=em[:], scalar1=sm["m1"][:, 0:1], scalar2=None, op0=ALU.is_ge), reads=emk + ["m1"], writes=["mask1"])
            S.op("dve", lambda: V.scalar_tensor_tensor(out=em2[:], in0=mask1[:], scalar=-BIG, in1=em[:], op0=ALU.mult, op1=ALU.add), reads=emk + ["mask1"], writes=["em2"])
            S.op("dve", lambda: V.tensor_reduce(out=sm["m2"][:], in_=em2[:], axis=AX.X, op=ALU.max), reads=["em2"], writes=["m2"])
            S.op("dve", lambda: V.tensor_scalar(out=mask2[:], in0=em2[:], scalar1=sm["m2"][:, 0:1], scalar2=None, op0=ALU.is_ge), reads=["em2", "m2"], writes=["mask2"])
            S.op("dve", lambda: V.tensor_tensor(out=sm["dd"][:], in0=sm["m2"][:], in1=sm["m1"][:], op=ALU.subtract), reads=["m1", "m2"], writes=["dd"])
            S.op("act", lambda: A_.activation(out=sm["rr"][:], in_=sm["dd"][:], func=AF.Exp), reads=["dd"], writes=["rr"])
            S.op("dve", lambda: V.tensor_scalar(out=sm["den"][:], in0=sm["rr"][:], scalar1=1.0, scalar2=None, op0=ALU.add), reads=["rr"], writes=["den"])
            S.op("dve", lambda: V.reciprocal(out=sm["p1"][:], in_=sm["den"][:]), reads=["den"], writes=["p1"])
            S.op("dve", lambda: V.tensor_tensor(out=sm["c1"][:], in0=sm["p1"][:], in1=sm["pg"][:], op=ALU.mult), reads=["p1", "pg"], writes=["c1"])
            S.op("dve", lambda: V.tensor_tensor(out=sm["c2"][:], in0=sm["c1"][:], in1=sm["rr"][:], op=ALU.mult), reads=["c1", "rr"], writes=["c2"])
            S.op("dve", lambda: V.tensor_scalar(out=Gt[:, t, :], in0=mask1[:], scalar1=sm["c1"][:, 0:1], scalar2=None, op0=ALU.mult), reads=["mask1", "c1"], writes=[("Gt", t)])
            S.op("dve", lambda: V.scalar_tensor_tensor(out=Gt[:, t, :], in0=mask2[:], scalar=sm["c2"][:, 0:1], in1=Gt[:, t, :], op0=ALU.mult, op1=ALU.add),
                 reads=["mask2", "c2", ("Gt", t)], writes=[("Gt", t)])
        S.barrier()


def moe_phase(nc, S, sb, psb, W, h2T, Gt, acc, NE=64):
    V, A_, P_, T_ = nc.vector, nc.scalar, nc.gpsimd, nc.tensor
    NWB = 4
    NU = NE * 8
    with ExitStack() as st:
        wg = [sb(st, "wg%d" % i, [128, 16, 128], BF16) for i in range(NWB)]
        wu = [sb(st, "wu%d" % i, [128, 16, 128], BF16) for i in range(NWB)]
        wd = sb(st, "wd", [128, 8, D], BF16)
        actT = sb(st, "actT", [128, 8, NOWN], BF16)
        sl = [sb(st, "sl%d" % i, [128, 512], F32) for i in range(2)]
        SBUF_LOG.append(("moe", nc.sbuf_bytes_remaining))
        for t in range(8):
            S.op("dve", lambda: V.memset(acc[:, t, :], 0.0), writes=[("acc", t, n) for n in range(4)])

        def issue_unit(u):
            e, f = u // 8, u % 8
            wb = u % NWB
            rows_g = W["w_eg"][e * D:(e + 1) * D, :].rearrange("(c p) n -> p c n", p=128)
            rows_u = W["w_eu"][e * D:(e + 1) * D, :].rearrange("(c p) n -> p c n", p=128)
            fs = slice(f * 128, (f + 1) * 128)
            for hh in range(2):
                S.dma("pool", lambda: nc.gpsimd.dma_start(out=wg[wb][:, hh * 8:(hh + 1) * 8, :], in_=rows_g[:, hh * 8:(hh + 1) * 8, fs]), writes=[("wg", wb, hh)])
            for hh in range(2):
                S.dma("pool", lambda: nc.gpsimd.dma_start(out=wu[wb][:, hh * 8:(hh + 1) * 8, :], in_=rows_u[:, hh * 8:(hh + 1) * 8, fs]), writes=[("wu", wb, hh)])

        def issue_wd(e):
            rows_d = W["w_ed"][e * 1024:(e + 1) * 1024, :].rearrange("(f p) n -> p f n", p=128)
            for f2 in range(8):
                S.dma("pool", lambda: nc.gpsimd.dma_start(out=wd[:, f2, :], in_=rows_d[:, f2, :]), writes=[("wd", f2)])

        issue_wd(0)
        for u in range(NWB - 1):
            issue_unit(u)
        it = 0
        dn = 0
        for u in range(NU):
            e, f = u // 8, u % 8
            wb = u % NWB
            if u + NWB - 1 < NU:
                issue_unit(u + NWB - 1)
            for half in range(2):
                ab = it % 2
                it += 1
                tok = slice(half * 512, (half + 1) * 512)
                for c in range(16):
                    S.op("pe", lambda: T_.matmul(psb[ab][:, :], lhsT=wg[wb][:, c, :], rhs=h2T[:, c, tok], start=(c == 0), stop=(c == 15)),
                         reads=[("wg", wb, c // 8)], writes=[("psb", ab)])
                for c in range(16):
                    S.op("pe", lambda: T_.matmul(psb[2 + ab][:, :], lhsT=wu[wb][:, c, :], rhs=h2T[:, c, tok], start=(c == 0), stop=(c == 15)),
                         reads=[("wu", wb, c // 8)], writes=[("psb", 2 + ab)])
                S.op("act", lambda: A_.activation(out=sl[ab][:], in_=psb[ab][:, :], func=AF.Silu), reads=[("psb", ab)], writes=[("sl", ab)])
                S.op("dve", lambda: V.tensor_tensor(out=actT[:, f, tok], in0=psb[2 + ab][:, :], in1=sl[ab][:], op=ALU.mult),
                     reads=[("psb", 2 + ab), ("sl", ab)], writes=[("actT", f, half)])
            if f == 7:
                for t in range(8):
                    ts_ = slice(t * 128, (t + 1) * 128)
                    for n in range(4):
                        db = 4 + dn % 4
                        dn += 1
                        ns = slice(n * 512, (n + 1) * 512)
                        for f2 in range(8):
                            S.op("pe", lambda: T_.matmul(psb[db][:, :], lhsT=actT[:, f2, ts_], rhs=wd[:, f2, ns], start=(f2 == 0), stop=(f2 == 7)),
                                 reads=[("actT", f2, t // 4), ("wd", f2)], writes=[("psb", db)])
                        S.op("dve", lambda: V.scalar_tensor_tensor(out=acc[:, t, ns], in0=psb[db][:, :], scalar=Gt[:, t, e:e + 1], in1=acc[:, t, ns], op0=ALU.mult, op1=ALU.add),
                             reads=[("psb", db), ("acc", t, n)], writes=[("acc", t, n)])
                if e + 1 < NE:
                    issue_wd(e + 1)
        S.barrier()


def final_phase(nc, S, sb, W, acc, x1_d, mod_d, out_own, eps_t):
    V, A_, P_ = nc.vector, nc.scalar, nc.gpsimd
    with ExitStack() as st:
        gf_bc = sb(st, "gf_bc", [128, D], F32)
        gfin_bc = sb(st, "gfin_bc", [128, D], F32)
        x1t = [sb(st, "fx1t%d" % i, [128, D], F32) for i in range(2)]
        ot = [sb(st, "fot%d" % i, [128, D], F32) for i in range(2)]
        junk = sb(st, "fjunk", [128, D], BF16)
        ss = [sb(st, "fss%d" % i, [128, 1], F32) for i in range(2)]
        rs = [sb(st, "frs%d" % i, [128, 1], F32) for i in range(2)]
        S.dma("sp", lambda: nc.sync.dma_start(out=gf_bc[:], in_=bcast_rows(mod_d[5:6, :])), writes=["gf_bc"])
        S.dma("sp", lambda: nc.sync.dma_start(out=gfin_bc[:], in_=bcast_rows(W["g_final"][0:1, :])), writes=["gfin_bc"])
        for t in range(8):
            b = t % 2
            ts_ = slice(t * 128, (t + 1) * 128)
            for hh in range(2):
                S.dma("sp", lambda: nc.sync.dma_start(out=x1t[b][:, hh * 1024:(hh + 1) * 1024], in_=x1_d[ts_, hh * 1024:(hh + 1) * 1024]), writes=[("fx1t", b, hh)])
            S.op("dve", lambda: V.tensor_tensor(out=ot[b][:], in0=acc[:, t, :], in1=gf_bc[:], op=ALU.mult), reads=["gf_bc"], writes=[("fot", b)])
            S.op("pool", lambda: P_.tensor_tensor(out=ot[b][:], in0=ot[b][:], in1=x1t[b][:], op=ALU.add),
                 reads=[("fot", b), ("fx1t", b, 0), ("fx1t", b, 1)], writes=[("fot", b)])
            S.op("act", lambda: A_.activation(out=junk[:], in_=ot[b][:], func=AF.Square, accum_out=ss[b][:]), reads=[("fot", b)], writes=[("fss", b), "fjunk"])
            S.op("act", lambda: A_.activation(out=rs[b][:], in_=ss[b][:], func=AF.Sqrt, bias=eps_t[:], scale=1.0 / D), reads=[("fss", b), "eps"], writes=[("frs", b)])
            S.op("dve", lambda: V.reciprocal(out=rs[b][:], in_=rs[b][:]), reads=[("frs", b)], writes=[("frs", b)])
            S.op("dve", lambda: V.scalar_tensor_tensor(out=ot[b][:], in0=ot[b][:], scalar=rs[b][:, 0:1], in1=gfin_bc[:], op0=ALU.mult, op1=ALU.mult),
                 reads=[("fot", b), ("frs", b), "gfin_bc"], writes=[("fot", b)])
            for hh in range(2):
                S.dma("sp", lambda: nc.sync.dma_start(out=out_own[ts_, hh * 1024:(hh + 1) * 1024], in_=ot[b][:, hh * 1024:(hh + 1) * 1024]), reads=[("fot", b)], writes=[("out", t, hh)])


def own_qblocks(half):
    qb = []
    for p in range(4):
        qb.append(2 * p + half)
        qb.append(15 - 2 * p - half)
    return qb


def own_token_index(half):
    return np.concatenate([np.arange(q * 128, (q + 1) * 128) for q in own_qblocks(half)])


def col_layout(v):
    return np.ascontiguousarray(np.asarray(v, np.float32).reshape(-1, 128).T)


def make_shared(inp, stage):
    f = lambda a: np.ascontiguousarray(np.asarray(a, np.float32))
    sh = {
        "rel_bias_table": f(inp["rel_bias_table"]), "w_ada": f(inp["w_ada"][0]), "b_ada": f(inp["b_ada"][0]).reshape(1, -1),
        "g_mix_col": col_layout(inp["g_mix"][0]), "w_in": f(inp["w_in"][0]),
        "lq1": f(inp["lambda_q1"][0]).reshape(1, -1), "lk1": f(inp["lambda_k1"][0]).reshape(1, -1),
        "lq2": f(inp["lambda_q2"][0]).reshape(1, -1), "lk2": f(inp["lambda_k2"][0]).reshape(1, -1),
        "g_subln": f(inp["g_subln"][0]).reshape(1, -1), "w_proj_a": f(inp["w_proj_a"][0]), "w_proj_b": f(inp["w_proj_b"][0]),
        "w_out": f(inp["w_out"][0]), "g_ffn_col": col_layout(inp["g_ffn"][0]),
        "w_rg": f(inp["w_router_group"][0]), "b_rg": f(inp["b_router_group"][0]).reshape(1, -1),
        "w_re": f(inp["w_router_expert"][0]), "b_re": f(inp["b_router_expert"][0]).reshape(1, -1),
        "g_final": f(inp["g_final"]).reshape(1, -1),
    }
    if stage >= 7:
        sh["w_eg"] = f(inp["w_expert_gate"][0]).reshape(64 * D, 1024)
        sh["w_eu"] = f(inp["w_expert_up"][0]).reshape(64 * D, 1024)
        sh["w_ed"] = f(inp["w_expert_down"][0]).reshape(64 * 1024, D)
    for k, v in host_consts().items():
        sh["c_" + k] = v
    return sh


def make_core_map(inp, shared, core):
    b, half = core // 2, core % 2
    xb = np.asarray(inp["x"][b], np.float32)
    m = dict(shared)
    m["x_all"] = np.ascontiguousarray(xb)
    m["x_own"] = np.ascontiguousarray(xb[own_token_index(half)])
    m["c_col"] = col_layout(inp["c"][b])
    m["halfv"] = np.full((128, 1), float(half), np.float32)
    return m


def kernel(**inputs):
    nc, _ = build(stage=99, dbg=False)
    shared = make_shared(inputs, 99)
    in_maps = [make_core_map(inputs, shared, c) for c in range(8)]
    res = run_bass_kernel_spmd(nc, in_maps, core_ids=list(range(8)))
    out = np.zeros((4, SEQ, D), np.float32)
    for c in range(8):
        out[c // 2, own_token_index(c % 2)] = res.results[c]["out_own"]
    return out
```

```python
import math
from contextlib import ExitStack

import numpy as np
import concourse.bass as bass
import concourse.mybir as mybir
from concourse.bass_utils import run_bass_kernel_spmd

F32 = mybir.dt.float32
BF16 = mybir.dt.bfloat16
I32 = mybir.dt.int32
U32 = mybir.dt.uint32
AF = mybir.ActivationFunctionType
ALU = mybir.AluOpType
AX = mybir.AxisListType

D = 2048
SEQ = 2048
NOWN = 1024
NH = 8
NBLK = 80
EPS = 1e-6
LAM_INIT = 0.8 - 0.6 * math.exp(0.0)
NEG = -30000.0
SBUF_LOG = []


class Sched:
    def __init__(self, nc, stack, n_dma_sems=32):
        self.nc = nc
        self.eng = {"pe": nc.tensor, "act": nc.scalar, "dve": nc.vector,
                    "pool": nc.gpsimd, "sp": nc.sync}
        self.sem = {e: stack.enter_context(nc.semaphore("s_" + e)) for e in self.eng}
        self.cnt = {e: 0 for e in self.eng}
        self.dsem = [stack.enter_context(nc.semaphore("d%d" % i)) for i in range(n_dma_sems)]
        self.dcnt = [0] * n_dma_sems
        self.dnext = 0
        self.seen = {e: {} for e in self.eng}
        self.lastw = {}
        self.reads = {}
        self.semobj = {}
        for e, s in self.sem.items():
            self.semobj[("e", e)] = s
        for i, s in enumerate(self.dsem):
            self.semobj[("d", i)] = s

    def _wait(self, e, ev):
        sid, val, src = ev
        if src == "pe" and e == "pe":
            return
        if self.seen[e].get(sid, 0) >= val:
            return
        self.seen[e][sid] = val
        self.eng[e].wait_ge(self.semobj[sid], val)

    def _deps(self, e, reads, writes, extra):
        evs = []
        for k in reads:
            if k in self.lastw:
                evs.append(self.lastw[k])
        for k in writes:
            if k in self.lastw:
                evs.append(self.lastw[k])
            evs.extend(self.reads.get(k, []))
        evs.extend(extra)
        best = {}
        for ev in evs:
            sid, val, src = ev
            if src == "pe" and e == "pe":
                continue
            if sid not in best or best[sid][1] < val:
                best[sid] = ev
        for ev in best.values():
            self._wait(e, ev)

    def _record(self, ev, reads, writes):
        for k in reads:
            lst = self.reads.setdefault(k, [])
            lst.append(ev)
        for k in writes:
            self.lastw[k] = ev
            self.reads[k] = []

    def op(self, e, fn, reads=(), writes=(), extra=()):
        self._deps(e, reads, writes, extra)
        ins = fn()
        self.cnt[e] += 1
        ins.then_inc(self.sem[e], 1)
        ev = (("e", e), self.cnt[e], e)
        self._record(ev, reads, writes)
        return ev

    def dma(self, q, fn, reads=(), writes=(), extra=()):
        i = self.dnext
        self.dnext = (self.dnext + 1) % len(self.dsem)
        sid = ("d", i)
        if self.dcnt[i] > 0:
            self._wait(q, (sid, self.dcnt[i], "dma"))
        self._deps(q, reads, writes, extra)
        ins = fn()
        self.dcnt[i] += 16
        ins.then_inc(self.dsem[i], 16)
        ev = (sid, self.dcnt[i], "dma")
        self._record(ev, reads, writes)
        return ev

    def all_events(self):
        evs = []
        for i, c in enumerate(self.dcnt):
            if c:
                evs.append((("d", i), c, "dma"))
        for en in self.eng:
            if self.cnt[en]:
                evs.append((("e", en), self.cnt[en], "x"))
        return evs

    def barrier(self):
        evs = self.all_events()
        for e in self.eng:
            for ev in evs:
                self._wait(e, ev)
        self.lastw = {}
        self.reads = {}

    def finish(self, e="sp"):
        for ev in self.all_events():
            self._wait(e, ev)


def bcast_rows(ap, nparts=128):
    n = ap.shape[-1]
    return bass.AP(ap.tensor, ap.offset, [[0, nparts], [1, n]])


def rel_bucket_np(n):
    n = np.maximum(n, 0)
    nf = np.maximum(n, 1).astype(np.float32)
    large = 16 + (np.log(nf / np.float32(16)) / np.float32(math.log(128 / 16)) * np.float32(16)).astype(np.int32)
    large = np.minimum(large, 31)
    return np.where(n < 16, n, large)


def host_consts():
    i = np.arange(128)
    c = {}
    c["ident"] = np.eye(128, dtype=np.float32)
    c["antiI"] = np.eye(128, dtype=np.float32)[::-1].copy()
    c["tri"] = (i[:, None] < i[None, :]).astype(np.float32)
    c["ugt"] = (i[:, None] > i[None, :]).astype(np.float32)
    c["ones"] = np.ones((128, 128), np.float32)
    n = np.arange(640) - 256
    oh = np.zeros((32, 640), np.float32)
    valid = (n >= 0) & (n < 256)
    bk = rel_bucket_np(np.clip(n, 0, None))
    oh[bk[valid], np.nonzero(valid)[0]] = 1.0
    oh[31, valid] -= 1.0
    c["ohb"] = oh
    c["negrow"] = np.where(n < 0, NEG, 0.0).astype(np.float32)[None, :]
    c["iota128"] = np.tile(np.arange(128, dtype=np.float32)[None, :], (128, 1))
    c["thr"] = np.tile((128.0 * np.arange(16, dtype=np.float32))[None, :], (128, 1))
    c["pcol"] = np.arange(128, dtype=np.float32)[:, None].copy()
    return c


CONST_SHAPES = {"ident": [128, 128], "antiI": [128, 128], "tri": [128, 128], "ugt": [128, 128],
                "ones": [128, 128], "ohb": [32, 640], "negrow": [1, 640], "iota128": [128, 128],
                "thr": [128, 16], "pcol": [128, 1]}

WEIGHT_SHAPES = {
    "rel_bias_table": [32, 8], "w_ada": [D, 6 * D], "b_ada": [1, 6 * D], "g_mix_col": [128, 16],
    "w_in": [D, 10240], "lq1": [1, 64], "lk1": [1, 64], "lq2": [1, 64], "lk2": [1, 64],
    "g_subln": [1, 128], "w_proj_a": [1024, D], "w_proj_b": [1024, D], "w_out": [D, D],
    "g_ffn_col": [128, 16], "w_rg": [D, 8], "b_rg": [1, 8], "w_re": [D, 64], "b_re": [1, 64],
    "w_eg": [64 * D, 1024], "w_eu": [64 * D, 1024], "w_ed": [64 * 1024, D], "g_final": [1, D],
}


def build(stage=99, dbg=False):
    nc = bass.Bass("TRN2", target_bir_lowering=False)
    din = {}

    def dram_in(name, shape, dt=F32):
        din[name] = nc.dram_tensor(name, list(shape), dt, kind="ExternalInput").ap()
        return din[name]

    x_all = dram_in("x_all", [SEQ, D])
    x_own = dram_in("x_own", [NOWN, D])
    c_col = dram_in("c_col", [128, 16])
    halfv = dram_in("halfv", [128, 1])
    W = {k: dram_in(k, s) for k, s in WEIGHT_SHAPES.items() if stage >= 7 or k not in ("w_eg", "w_eu", "w_ed")}
    C = {k: dram_in("c_" + k, s) for k, s in CONST_SHAPES.items()}
    out_own = nc.dram_tensor("out_own", [NOWN, D], F32, kind="ExternalOutput").ap()
    dbg_out = {}

    def dbg_tensor(name, shape):
        dbg_out[name] = nc.dram_tensor("dbg_" + name, list(shape), F32, kind="ExternalOutput").ap()
        return dbg_out[name]

    tv_d = nc.dram_tensor("tv_scratch", [8, 640], F32, kind="Internal")
    mod_d = nc.dram_tensor("mod_scratch", [6, D], F32, kind="Internal").ap()
    x1_d = nc.dram_tensor("dbg_x1" if dbg else "x1_scratch", [NOWN, D], F32, kind="ExternalOutput" if dbg else "Internal").ap()

    with ExitStack() as st0:
        S = Sched(nc, st0)
        V, A_, P_, T_ = nc.vector, nc.scalar, nc.gpsimd, nc.tensor

        def sb(stack, name, shape, dt):
            return stack.enter_context(nc.sbuf_tensor(name, list(shape), dt))

        psb = [st0.enter_context(nc.psum_tensor("psb%d" % i, [128, 512], F32)) for i in range(8)]

        def ps_bf(i):
            return psb[i][:].bitcast(BF16)

        cs = {}
        for k in ("ident", "antiI", "tri", "ugt", "ones"):
            cs[k] = sb(st0, "k_" + k, [128, 128], F32)
            S.dma("sp", lambda k=k: nc.sync.dma_start(out=cs[k][:], in_=C[k][:, :]), writes=["c_" + k])
        identb = sb(st0, "identb", [128, 128], BF16)
        S.op("dve", lambda: V.tensor_copy(out=identb[:], in_=cs["ident"][:]), reads=["c_ident"], writes=["identb"])
        half_t = sb(st0, "half_t", [128, 1], F32)
        S.dma("sp", lambda: nc.sync.dma_start(out=half_t[:], in_=halfv[:, :]), writes=["half"])

        Gm_col = sb(st0, "Gm_col", [128, 16], F32)
        shm_col = sb(st0, "shm_col", [128, 16], F32)
        Gf_col = sb(st0, "Gf_col", [128, 16], F32)
        shf_col = sb(st0, "shf_col", [128, 16], F32)

        with ExitStack() as st:
            ccol = sb(st, "ccol", [128, 16], F32)
            scol = sb(st, "scol", [128, 16], F32)
            gmix = sb(st, "gmix", [128, 16], F32)
            S.dma("sp", lambda: nc.sync.dma_start(out=ccol[:], in_=c_col[:, :]), writes=["ccol"])
            S.dma("sp", lambda: nc.sync.dma_start(out=gmix[:], in_=W["g_mix_col"][:, :]), writes=["gmix"])
            gffn = sb(st, "gffn", [128, 16], F32)
            S.dma("sp", lambda: nc.sync.dma_start(out=gffn[:], in_=W["g_ffn_col"][:, :]), writes=["gffn"])
            S.op("act", lambda: A_.activation(out=scol[:], in_=ccol[:], func=AF.Silu), reads=["ccol"], writes=["scol"])
            wa = [sb(st, "wa%d" % i, [128, 16, 512], F32) for i in range(2)]
            brow = [sb(st, "brow%d" % i, [1, 512], F32) for i in range(2)]
            mrow = [sb(st, "mrow%d" % i, [1, 512], F32) for i in range(2)]
            one11 = sb(st, "one11", [1, 1], F32)
            S.op("dve", lambda: V.memset(one11[:], 1.0), writes=["one11"])
            w_ada_r = W["w_ada"].rearrange("(c p) n -> p c n", p=128)
            for j in range(24):
                m, jj = j // 4, j % 4
                b = j % 2
                for hh in range(2):
                    S.dma("sp", lambda b=b, j=j, hh=hh: nc.sync.dma_start(
                        out=wa[b][:, hh * 8:(hh + 1) * 8, :], in_=w_ada_r[:, hh * 8:(hh + 1) * 8, j * 512:(j + 1) * 512]),
                        writes=[("wa", b, hh)])
                S.dma("sp", lambda b=b, j=j: nc.sync.dma_start(out=brow[b][:], in_=W["b_ada"][0:1, j * 512:(j + 1) * 512]), writes=[("brow", b)])
                pb = psb[b]
                for c in range(16):
                    S.op("pe", lambda c=c, b=b, pb=pb: T_.matmul(pb[0:1, :], lhsT=scol[:, c:c + 1], rhs=wa[b][:, c, :], start=(c == 0), stop=(c == 15)),
                         reads=["scol", ("wa", b, c // 8)], writes=[("psb", b)])
                S.op("dve", lambda b=b, pb=pb: V.tensor_tensor(out=mrow[b][:], in0=pb[0:1, :], in1=brow[b][:], op=ALU.add),
                     reads=[("psb", b), ("brow", b)], writes=[("mrow", b)])
                if m in (0, 1, 3, 4):
                    pc = psb[2 + b]
                    for q in range(4):
                        S.op("pe", lambda q=q, b=b, pc=pc: T_.matmul(pc[:, q:q + 1], lhsT=mrow[b][0:1, q * 128:(q + 1) * 128], rhs=one11[0:1, 0:1], start=True, stop=True),
                             reads=[("mrow", b), "one11"], writes=[("psb", 2 + b)])
                    dst = {0: shm_col, 1: Gm_col, 3: shf_col, 4: Gf_col}[m]
                    key = {0: "shm_col", 1: "Gm_col", 3: "shf_col", 4: "Gf_col"}[m]
                    S.op("dve", lambda b=b, pc=pc, dst=dst, jj=jj: V.tensor_copy(out=dst[:, jj * 4:(jj + 1) * 4], in_=pc[:, 0:4]),
                         reads=[("psb", 2 + b)], writes=[key])
                S.dma("sp", lambda b=b, m=m, jj=jj: nc.sync.dma_start(out=mod_d[m:m + 1, jj * 512:(jj + 1) * 512], in_=mrow[b][:]),
                      reads=[("mrow", b)], writes=[("mod_d", j)])
            S.op("dve", lambda: V.scalar_tensor_tensor(out=Gm_col[:], in0=Gm_col[:], scalar=1.0, in1=gmix[:], op0=ALU.add, op1=ALU.mult),
                 reads=["Gm_col", "gmix"], writes=["Gm_col"])
            S.op("dve", lambda: V.scalar_tensor_tensor(out=Gf_col[:], in0=Gf_col[:], scalar=1.0, in1=gffn[:], op0=ALU.add, op1=ALU.mult),
                 reads=["Gf_col", "gffn"], writes=["Gf_col"])
            if dbg:
                d = dbg_tensor("Gm_col", [128, 16]); S.dma("sp", lambda: nc.sync.dma_start(out=d[:, :], in_=Gm_col[:]), reads=["Gm_col"])
                d2 = dbg_tensor("shm_col", [128, 16]); S.dma("sp", lambda: nc.sync.dma_start(out=d2[:, :], in_=shm_col[:]), reads=["shm_col"])
            S.barrier()
        if stage <= 0:
            S.finish("sp")
            return nc, dbg_out

        stR = ExitStack()
        stA = ExitStack()
        eps_t = sb(st0, "eps_t", [128, 1], F32)
        S.op("dve", lambda: V.memset(eps_t[:], EPS), writes=["eps"])
        hT_own = sb(stA, "hT_own", [128, 16, NOWN], BF16)
        oaT = sb(stA, "oaT", [128, NH, NOWN], BF16)
        obT = sb(stA, "obT", [128, NH, NOWN], BF16)

        def norm_tile_to_hT(stk_bufs, src_ap, dstT, col0, tag):
            xt, xb, ss, rs, junk = stk_bufs
            for hh in range(2):
                S.dma("sp", lambda hh=hh: nc.sync.dma_start(out=xt[:, hh * 1024:(hh + 1) * 1024], in_=src_ap[:, hh * 1024:(hh + 1) * 1024]), writes=[(tag, "xt", hh)])
            S.op("act", lambda: A_.activation(out=junk[:], in_=xt[:], func=AF.Square, accum_out=ss[:]),
                 reads=[(tag, "xt", 0), (tag, "xt", 1)], writes=[(tag, "ss"), (tag, "junk")])
            S.op("act", lambda: A_.activation(out=rs[:], in_=ss[:], func=AF.Sqrt, bias=eps_t[:], scale=1.0 / D),
                 reads=[(tag, "ss"), "eps"], writes=[(tag, "rs")])
            S.op("dve", lambda: V.reciprocal(out=rs[:], in_=rs[:]), reads=[(tag, "rs")], writes=[(tag, "rs")])
            S.op("dve", lambda: V.tensor_scalar(out=xb[:], in0=xt[:], scalar1=rs[:, 0:1], scalar2=None, op0=ALU.mult),
                 reads=[(tag, "xt", 0), (tag, "xt", 1), (tag, "rs")], writes=[(tag, "xb")])
            for g in range(2):
                pt = ps_bf(6 + g)
                for c8 in range(8):
                    c = g * 8 + c8
                    S.op("pe", lambda c=c, c8=c8, pt=pt: T_.transpose(pt[:, c8 * 128:(c8 + 1) * 128], xb[:, c * 128:(c + 1) * 128], identb[:]),
                         reads=[(tag, "xb"), "identb"], writes=[("psb", 6 + g)])
                for c8 in range(8):
                    c = g * 8 + c8
                    S.op("act", lambda c=c, c8=c8, pt=pt: A_.activation(out=dstT[:, c, col0:col0 + 128], in_=pt[:, c8 * 128:(c8 + 1) * 128],
                                                                      func=AF.Identity, bias=shm_col[:, c:c + 1], scale=Gm_col[:, c:c + 1]),
                         reads=[("psb", 6 + g), "shm_col", "Gm_col"], writes=[(tag, "hT", col0 // 512)])

        with ExitStack() as stB:
            hT_all = sb(stB, "hT_all", [128, 16, SEQ], BF16)
            with ExitStack() as st:
                bufs = []
                for i in range(2):
                    bufs.append((sb(st, "xt%d" % i, [128, D], F32), sb(st, "xb%d" % i, [128, D], BF16),
                                 sb(st, "ss%d" % i, [128, 1], F32), sb(st, "rs%d" % i, [128, 1], F32),
                                 sb(st, "junk%d" % i, [128, D], BF16)))
                for t in range(16):
                    norm_tile_to_hT(bufs[t % 2], x_all[t * 128:(t + 1) * 128, :], hT_all, t * 128, ("n", t % 2))
                S.barrier()
                for t in range(8):
                    norm_tile_to_hT(bufs[t % 2], x_own[t * 128:(t + 1) * 128, :], hT_own, t * 128, ("n", t % 2))
                S.barrier()
            attention_phase(nc, S, stB, sb, psb, ps_bf, cs, identb, half_t, W, C, tv_d, hT_all, hT_own, oaT, obT, False, dbg_tensor)
            S.barrier()
        mergedT = stR.enter_context(nc.sbuf_tensor("mergedT", [128, 16, NOWN], BF16, side="right"))
        merge_phase(nc, S, sb, psb, W, hT_own, oaT, obT, mergedT)
        S.barrier()
        stA.close()
        h2T = sb(st0, "h2T", [128, 16, NOWN], BF16)
        Gt = sb(st0, "Gt", [128, 8, 64], F32)
        resid_phase(nc, S, sb, psb, W, mergedT, x_own, x1_d, mod_d)
        S.barrier()
        stR.close()
        norm_router_phase(nc, S, sb, psb, W, cs, x1_d, h2T, Gt, Gf_col, shf_col, eps_t)
        S.barrier()
        if dbg:
            d = dbg_tensor("Gt", [128, 8 * 64])
            S.dma("sp", lambda: nc.sync.dma_start(out=d[:, :], in_=Gt[:].rearrange("p a b -> p (a b)")))
            d2 = dbg_tensor("h2T", [128, 16 * NOWN])
            with ExitStack() as st:
                tmpf = sb(st, "dbgtmp", [128, NOWN], F32)
                for c in range(16):
                    S.op("dve", lambda c=c: V.tensor_copy(out=tmpf[:], in_=h2T[:, c, :]), writes=["dbgtmp"])
                    S.dma("sp", lambda c=c: nc.sync.dma_start(out=d2[:, c * NOWN:(c + 1) * NOWN], in_=tmpf[:]), reads=["dbgtmp"])
                S.barrier()
        if stage <= 4:
            S.finish("sp")
            return nc, dbg_out
        acc = sb(st0, "acc", [128, 8, D], F32)
        if stage == 5:
            for t in range(8):
                S.op("dve", lambda: V.memset(acc[:, t, :], 0.0), writes=[("acc", t)])
        else:
            moe_phase(nc, S, sb, psb, W, h2T, Gt, acc)
        S.barrier()
        final_phase(nc, S, sb, W, acc, x1_d, mod_d, out_own, eps_t)
        S.finish("sp")
    return nc, dbg_out


def attention_phase(nc, S, stB, sb, psb, ps_bf, cs, identb, half_t, W, C, tv_d, hT_all, hT_own, oaT, obT, dbg, dbg_tensor):
    V, A_, P_, T_ = nc.vector, nc.scalar, nc.gpsimd, nc.tensor
    with ExitStack() as st:
        tab = sb(st, "tab", [32, 8], F32)
        ohb = sb(st, "ohb", [32, 640], F32)
        negrow = sb(st, "negrow", [1, 640], F32)
        tvs = sb(st, "tvs", [8, 640], F32)
        b31 = sb(st, "b31", [128, 8], F32)
        S.dma("sp", lambda: nc.sync.dma_start(out=tab[:], in_=W["rel_bias_table"][:, :]), writes=["tab"])
        S.dma("sp", lambda: nc.sync.dma_start(out=ohb[:], in_=C["ohb"][:, :]), writes=["ohb"])
        S.dma("sp", lambda: nc.sync.dma_start(out=negrow[:], in_=C["negrow"][:, :]), writes=["negrow"])
        S.dma("sp", lambda: nc.sync.dma_start(out=b31[:], in_=bcast_rows(W["rel_bias_table"][31:32, :])), writes=["b31"])
        for half in range(2):
            pb = psb[half]
            S.op("pe", lambda half=half, pb=pb: T_.matmul(pb[0:8, 0:320], lhsT=tab[:, :], rhs=ohb[:, half * 320:(half + 1) * 320], start=True, stop=False),
                 reads=["tab", "ohb"], writes=[("psb", half)])
            S.op("pe", lambda half=half, pb=pb: T_.matmul(pb[0:8, 0:320], lhsT=cs["ones"][0:1, 0:8], rhs=negrow[0:1, half * 320:(half + 1) * 320], start=False, stop=True),
                 reads=["c_ones", "negrow"], writes=[("psb", half)])
            S.op("act", lambda half=half, pb=pb: A_.mul(out=tvs[:, half * 320:(half + 1) * 320], in_=pb[0:8, 0:320], mul=8.0),
                 reads=[("psb", half)], writes=["tvs"])
        S.dma("sp", lambda: nc.sync.dma_start(out=tv_d.ap()[:, :], in_=tvs[:]), reads=["tvs"], writes=["tv_d"])

        lam4 = sb(st, "lam4", [128, 4, 64], F32)
        for i, nm in enumerate(("lq1", "lk1", "lq2", "lk2")):
            S.dma("sp", lambda i=i, nm=nm: nc.sync.dma_start(out=lam4[:, i, :], in_=W[nm][0:1, :].partition_broadcast(128)), writes=[("lam4", i)])
        lsum = sb(st, "lsum", [128, 2], F32)
        ljunk = sb(st, "ljunk", [128, 64], F32)
        for i in range(2):
            S.op("dve", lambda i=i: V.tensor_tensor(out=ljunk[:], in0=lam4[:, 2 * i, :], in1=lam4[:, 2 * i + 1, :], op=ALU.mult),
                 reads=[("lam4", 2 * i), ("lam4", 2 * i + 1)], writes=["ljunk"])
            S.op("dve", lambda i=i: V.tensor_reduce(out=lsum[:, i:i + 1], in_=ljunk[:], axis=AX.X, op=ALU.add),
                 reads=["ljunk"], writes=[("lsum", i)])
        S.op("act", lambda: A_.activation(out=lsum[:], in_=lsum[:], func=AF.Exp), reads=[("lsum", 0), ("lsum", 1)], writes=["lsume"])
        nlam = sb(st, "nlam", [128, 1], F32)
        S.op("dve", lambda: V.tensor_tensor(out=nlam[:], in0=lsum[:, 1:2], in1=lsum[:, 0:1], op=ALU.subtract), reads=["lsume"], writes=["nlam"])
        S.op("dve", lambda: V.tensor_scalar(out=nlam[:], in0=nlam[:], scalar1=-LAM_INIT, scalar2=None, op0=ALU.add), reads=["nlam"], writes=["nlam"])
        gs8 = sb(st, "gs8", [128, 128], F32)
        S.dma("sp", lambda: nc.sync.dma_start(out=gs8[:], in_=bcast_rows(W["g_subln"][0:1, :])), writes=["gs8"])
        S.op("dve", lambda: V.tensor_scalar(out=gs8[:], in0=gs8[:], scalar1=(1.0 - LAM_INIT), scalar2=None, op0=ALU.mult), reads=["gs8"], writes=["gs8"])
        eps_t = sb(st, "eps_t2", [128, 1], F32)
        S.op("dve", lambda: V.memset(eps_t[:], EPS), writes=["eps2"])

        tri, ones = cs["tri"], cs["ones"]
        omt = sb(st, "omt", [128, 128], F32)
        S.op("dve", lambda: V.tensor_tensor(out=omt[:], in0=ones[:], in1=tri[:], op=ALU.subtract), reads=["c_ones", "c_tri"], writes=["omt"])
        mk = {n: sb(st, "mk_" + n, [128, 128], F32) for n in ("XB", "XC", "YB", "YC")}
        omh = sb(st, "omh", [128, 1], F32)
        S.op("dve", lambda: V.tensor_scalar(out=omh[:], in0=half_t[:], scalar1=-1.0, scalar2=1.0, op0=ALU.mult, op1=ALU.add), reads=["half"], writes=["omh"])
        S.op("dve", lambda: V.scalar_tensor_tensor(out=mk["XB"][:], in0=omt[:], scalar=half_t[:, 0:1], in1=tri[:], op0=ALU.mult, op1=ALU.add),
             reads=["omt", "half", "c_tri"], writes=["mk_XB"])
        S.op("dve", lambda: V.tensor_scalar(out=mk["XC"][:], in0=tri[:], scalar1=half_t[:, 0:1], scalar2=None, op0=ALU.mult), reads=["c_tri", "half"], writes=["mk_XC"])
        S.op("dve", lambda: V.scalar_tensor_tensor(out=mk["YB"][:], in0=omt[:], scalar=omh[:, 0:1], in1=tri[:], op0=ALU.mult, op1=ALU.add),
             reads=["omt", "omh", "c_tri"], writes=["mk_YB"])
        S.op("dve", lambda: V.tensor_scalar(out=mk["YC"][:], in0=tri[:], scalar1=omh[:, 0:1], scalar2=None, op0=ALU.mult), reads=["c_tri", "omh"], writes=["mk_YC"])

        wq = [sb(st, "wq%d" % i, [128, 16, 128], BF16) for i in range(3)]
        qT = sb(st, "qT", [128, NOWN], BF16)
        kT = sb(st, "kT", [128, SEQ], BF16)
        vA = sb(st, "vA", [128, 16, 130], BF16)
        Ht = [sb(st, "Ht%d" % i, [128, 128], F32) for i in range(4)]
        slot = {n: sb(st, "slot" + n, [128, 128], F32) for n in ("XA", "XB", "XC", "YA", "YB", "YC")}
        hd = sb(st, "hd", [128, 128], F32)
        PT = [sb(st, "PT%d" % i, [128, 128], BF16) for i in range(4)]
        e_t = [sb(st, "e_t%d" % i, [128, 128], F32) for i in range(2)]
        lnp = [sb(st, "lnp%d" % i, [128, 128], F32) for i in range(2)]
        LK = [sb(st, "LK%d" % i, [128, 128], F32) for i in range(2)]
        arg = [sb(st, "arg%d" % i, [128, 128], F32) for i in range(2)]
        Acc = sb(st, "Acc", [128, 128], F32)
        o1 = sb(st, "o1", [128, 128], F32)
        o2 = sb(st, "o2", [128, 128], F32)
        obf = sb(st, "obf", [128, 128], BF16)
        rr = sb(st, "rr", [128, 4], F32)
        sjunk = sb(st, "sjunk", [128, 128], F32)
        S.op("dve", lambda: V.memset(vA[:], 1.0), writes=["vA_init"])

        w_in_r = W["w_in"].rearrange("(c p) n -> p c n", p=128)
        SC_A = 64 ** -0.5
        SC_B = 128 ** -0.5

        def own_tile_info(j):
            p = j // 2
            if j % 2 == 0:
                return 2 * p + 2, "X", (2 * p - 1, 2 * p, 2 * p + 1)
            return 16 - 2 * p, "Y", (13 - 2 * p, 14 - 2 * p, 15 - 2 * p)

        def project_head(h, colbase, with_ones):
            for i, off in enumerate((0, 1024, 2048)):
                c0 = colbase + off + h * 128
                for hh in range(2):
                    S.dma("pool", lambda i=i, c0=c0, hh=hh: nc.gpsimd.dma_start(out=wq[i][:, hh * 8:(hh + 1) * 8, :], in_=w_in_r[:, hh * 8:(hh + 1) * 8, c0:c0 + 128]),
                          writes=[("wq", i, hh)])
            n = 0
            for ch in range(2):
                pb = psb[n % 2]; n += 1
                for c in range(16):
                    S.op("pe", lambda c=c, ch=ch, pb=pb: T_.matmul(pb[:, :], lhsT=wq[0][:, c, :], rhs=hT_own[:, c, ch * 512:(ch + 1) * 512], start=(c == 0), stop=(c == 15)),
                         reads=[("wq", 0, c // 8)], writes=[("psb", (n - 1) % 2)])
                S.op("act", lambda ch=ch, pb=pb: A_.copy(out=qT[:, ch * 512:(ch + 1) * 512], in_=pb[:, :]), reads=[("psb", (n - 1) % 2)], writes=[("qT", ch)])
            for ch in range(4):
                pb = psb[n % 2]; n += 1
                for c in range(16):
                    S.op("pe", lambda c=c, ch=ch, pb=pb: T_.matmul(pb[:, :], lhsT=wq[1][:, c, :], rhs=hT_all[:, c, ch * 512:(ch + 1) * 512], start=(c == 0), stop=(c == 15)),
                         reads=[("wq", 1, c // 8)], writes=[("psb", (n - 1) % 2)])
                S.op("dve", lambda ch=ch, pb=pb: V.tensor_copy(out=kT[:, ch * 512:(ch + 1) * 512], in_=pb[:, :]), reads=[("psb", (n - 1) % 2)], writes=[("kT", ch)])
            for kb4 in range(4):
                pb = psb[n % 2]; n += 1
                for k4 in range(4):
                    kb = kb4 * 4 + k4
                    for c in range(16):
                        S.op("pe", lambda c=c, kb=kb, k4=k4, pb=pb: T_.matmul(pb[:, k4 * 128:(k4 + 1) * 128], lhsT=hT_all[:, c, kb * 128:(kb + 1) * 128], rhs=wq[2][:, c, :], start=(c == 0), stop=(c == 15)),
                             reads=[("wq", 2, c // 8)], writes=[("psb", (n - 1) % 2)])
                eng = "act" if kb4 % 2 == 0 else "dve"
                if eng == "act":
                    S.op("act", lambda kb4=kb4, pb=pb: A_.copy(out=vA[:, kb4 * 4:(kb4 + 1) * 4, 0:128], in_=pb[:, :].rearrange("p (a b) -> p a b", a=4)),
                         reads=[("psb", (n - 1) % 2), "vA_init"], writes=[("vA", kb4)])
                else:
                    S.op("dve", lambda kb4=kb4, pb=pb: V.tensor_copy(out=vA[:, kb4 * 4:(kb4 + 1) * 4, 0:128], in_=pb[:, :].rearrange("p (a b) -> p a b", a=4)),
                         reads=[("psb", (n - 1) % 2), "vA_init"], writes=[("vA", kb4)])

        def qk_keys(j, kb):
            return [("qT", j // 4), ("kT", kb // 4)]

        for h in range(NH):
            project_head(h, 0, True)
            for i, dl in enumerate((-128, 0, 128, 256)):
                src = bass.AP(tv_d, h * 640 + 129 + dl, [[1, 128], [1, 128]])
                S.dma("sp", lambda i=i, src=src: nc.sync.dma_start(out=Ht[i][:], in_=src), reads=["tv_d"], writes=[("Ht", i)])
            for nm, lo, hi in (("XA", 2, 3), ("XB", 1, 2), ("XC", 0, 1), ("YA", 3, 2), ("YB", 2, 1), ("YC", 1, 0)):
                S.op("dve", lambda lo=lo, hi=hi: V.tensor_tensor(out=hd[:], in0=Ht[hi][:], in1=Ht[lo][:], op=ALU.subtract),
                     reads=[("Ht", lo), ("Ht", hi)], writes=["hd"])
                S.op("dve", lambda nm=nm, lo=lo: V.scalar_tensor_tensor(out=slot[nm][:], in0=hd[:], scalar=half_t[:, 0:1], in1=Ht[lo][:], op0=ALU.mult, op1=ALU.add),
                     reads=["hd", "half", ("Ht", lo)], writes=[("slot", nm)])
            git = 0
            for j in range(8):
                L, sset, slots = own_tile_info(j)
                obase = 4 if j % 2 == 0 else 0
                steps = [(m, kb) for m in range(2) for kb in range(L)]

                def da_front(i):
                    m, kb = steps[i]
                    rows = slice(64 * m, 64 * m + 64)
                    g = git + i
                    sbk = 2 + (g % 2)
                    pt = PT[g % 4]
                    sl = None
                    if kb in slots:
                        sl = sset + "ABC"[slots.index(kb)]
                    S.op("pe", lambda: T_.matmul(psb[sbk][:, 0:128], lhsT=kT[rows, kb * 128:(kb + 1) * 128], rhs=qT[rows, j * 128:(j + 1) * 128], start=True, stop=(sl is None)),
                         reads=qk_keys(j, kb), writes=[("psb", sbk)])
                    if sl is not None:
                        S.op("pe", lambda: T_.matmul(psb[sbk][:, 0:128], lhsT=cs["antiI"][:], rhs=slot[sl][:], start=False, stop=True),
                             reads=["c_antiI", ("slot", sl)], writes=[("psb", sbk)])
                    S.op("act", lambda: A_.activation(out=pt[:], in_=psb[sbk][:, 0:128], func=AF.Exp, bias=b31[:, h:h + 1], scale=SC_A),
                         reads=[("psb", sbk), "b31"], writes=[("PT", g % 4)])

                def da_back(i):
                    m, kb = steps[i]
                    g = git + i
                    ob = obase + m
                    pt = PT[g % 4]
                    S.op("pe", lambda: T_.matmul(psb[ob][:, 0:130], lhsT=pt[:], rhs=vA[:, kb, :], start=(kb == 0), stop=(kb == L - 1)),
                         reads=[("PT", g % 4), ("vA", kb // 4)], writes=[("psb", ob)])

                for i in range(len(steps)):
                    da_front(i)
                    if i >= 1:
                        da_back(i - 1)
                da_back(len(steps) - 1)
                git += len(steps)
                o1b, o2b = obase, obase + 1
                S.op("dve", lambda: V.reciprocal(out=rr[:, 0:1], in_=psb[o1b][:, 128:129]), reads=[("psb", o1b)], writes=["rr0"])
                S.op("dve", lambda: V.reciprocal(out=rr[:, 1:2], in_=psb[o2b][:, 128:129]), reads=[("psb", o2b)], writes=["rr1"])
                S.op("dve", lambda: V.tensor_tensor(out=rr[:, 1:2], in0=rr[:, 1:2], in1=nlam[:], op=ALU.mult), reads=["rr1", "nlam"], writes=["rr1"])
                S.op("dve", lambda: V.tensor_scalar(out=o1[:], in0=psb[o1b][:, 0:128], scalar1=rr[:, 0:1], scalar2=None, op0=ALU.mult), reads=[("psb", o1b), "rr0"], writes=["o1"])
                S.op("dve", lambda: V.scalar_tensor_tensor(out=o2[:], in0=psb[o2b][:, 0:128], scalar=rr[:, 1:2], in1=o1[:], op0=ALU.mult, op1=ALU.add),
                     reads=[("psb", o2b), "rr1", "o1"], writes=["o2"])
                S.op("act", lambda: A_.activation(out=sjunk[:], in_=o2[:], func=AF.Square, accum_out=rr[:, 2:3]), reads=["o2"], writes=["rr2", "sjunk"])
                S.op("act", lambda: A_.activation(out=rr[:, 2:3], in_=rr[:, 2:3], func=AF.Sqrt, bias=eps_t[:], scale=1.0 / 128), reads=["rr2", "eps2"], writes=["rr2"])
                S.op("dve", lambda: V.reciprocal(out=rr[:, 2:3], in_=rr[:, 2:3]), reads=["rr2"], writes=["rr2"])
                S.op("dve", lambda: V.scalar_tensor_tensor(out=obf[:], in0=o2[:], scalar=rr[:, 2:3], in1=gs8[:], op0=ALU.mult, op1=ALU.mult),
                     reads=["o2", "rr2", "gs8"], writes=["obf"])
                ptb = ps_bf(6)
                S.op("pe", lambda ptb=ptb: T_.transpose(ptb[:, 0:128], obf[:], identb[:]), reads=["obf", "identb"], writes=[("psb", 6)])
                S.op("act", lambda h=h, j=j, ptb=ptb: A_.copy(out=oaT[:, h, j * 128:(j + 1) * 128], in_=ptb[:, 0:128]), reads=[("psb", 6)], writes=[("oaT", h)])

        for h in range(NH):
            project_head(h, 3072, False)
            git = 0
            for j in range(8):
                L, sset, slots = own_tile_info(j)
                kbs = list(range(L - 1, -1, -1))
                ab = 4 if j % 2 == 0 else 0

                def sb_mask(kb):
                    if kb in slots:
                        nm = sset + "ABC"[slots.index(kb)]
                        if nm[1] != "A":
                            return nm
                    return None

                def sb_front(i):
                    kb = kbs[i]
                    g = git + i
                    zb = 2 + (g % 2)
                    b2 = g % 2
                    mkt = sb_mask(kb)
                    S.op("pe", lambda: T_.matmul(psb[zb][:, 0:128], lhsT=kT[:, kb * 128:(kb + 1) * 128], rhs=qT[:, j * 128:(j + 1) * 128], start=True, stop=True),
                         reads=qk_keys(j, kb), writes=[("psb", zb)])
                    S.op("act", lambda: A_.activation(out=e_t[b2][:], in_=psb[zb][:, 0:128], func=AF.Exp, scale=-SC_B), reads=[("psb", zb)], writes=[("e_t", b2)])
                    S.op("act", lambda: A_.activation(out=lnp[b2][:], in_=e_t[b2][:], func=AF.Ln, bias=1.0, scale=1.0), reads=[("e_t", b2)], writes=[("lnp", b2)])
                    S.op("dve", lambda: V.scalar_tensor_tensor(out=LK[b2][:], in0=psb[zb][:, 0:128], scalar=-SC_B, in1=lnp[b2][:], op0=ALU.mult, op1=ALU.subtract),
                         reads=[("psb", zb), ("lnp", b2)], writes=[("LK", b2)])
                    if mkt is not None:
                        S.op("pool", lambda: P_.tensor_tensor(out=LK[b2][:], in0=LK[b2][:], in1=mk[mkt][:], op=ALU.mult),
                             reads=[("LK", b2), "mk_" + mkt], writes=[("LK", b2)])

                def sb_mid(i):
                    kb = kbs[i]
                    g = git + i
                    b2 = g % 2
                    lb = 5 if g % 2 == 0 else 7
                    pt = PT[g % 4]
                    first = (i == 0)
                    mkt = sb_mask(kb)
                    S.op("pe", lambda: T_.matmul(psb[lb][:, 0:128], lhsT=cs["ugt"][:], rhs=LK[b2][:], start=True, stop=first),
                         reads=["c_ugt", ("LK", b2)], writes=[("psb", lb)])
                    if not first:
                        S.op("pe", lambda: T_.matmul(psb[lb][:, 0:128], lhsT=cs["ones"][:], rhs=Acc[:], start=False, stop=True),
                             reads=["c_ones", "Acc"], writes=[("psb", lb)])
                    if kb > 0:
                        if first:
                            S.op("pool", lambda: P_.tensor_copy(out=Acc[:], in_=LK[b2][:]), reads=[("LK", b2)], writes=["Acc"])
                        else:
                            S.op("pool", lambda: P_.tensor_tensor(out=Acc[:], in0=Acc[:], in1=LK[b2][:], op=ALU.add), reads=[("LK", b2), "Acc"], writes=["Acc"])
                    S.op("dve", lambda: V.tensor_tensor(out=arg[b2][:], in0=psb[lb][:, 0:128], in1=lnp[b2][:], op=ALU.subtract),
                         reads=[("psb", lb), ("lnp", b2)], writes=[("arg", b2)])
                    S.op("act", lambda: A_.activation(out=pt[:], in_=arg[b2][:], func=AF.Exp), reads=[("arg", b2)], writes=[("PT", g % 4)])
                    if mkt is not None:
                        S.op("pool", lambda: P_.tensor_tensor(out=pt[:], in0=pt[:], in1=mk[mkt][:], op=ALU.mult),
                             reads=[("PT", g % 4), "mk_" + mkt], writes=[("PT", g % 4)])

                def sb_av(i):
                    kb = kbs[i]
                    g = git + i
                    pt = PT[g % 4]
                    S.op("pe", lambda: T_.matmul(psb[ab][:, 0:128], lhsT=pt[:], rhs=vA[:, kb, 0:128], start=(i == 0), stop=(kb == 0)),
                         reads=[("PT", g % 4), ("vA", kb // 4)], writes=[("psb", ab)])

                n_it = len(kbs)
                for i in range(n_it):
                    sb_front(i)
                    if i >= 1:
                        sb_mid(i - 1)
                    if i >= 2:
                        sb_av(i - 2)
                sb_mid(n_it - 1)
                if n_it >= 2:
                    sb_av(n_it - 2)
                sb_av(n_it - 1)
                git += len(kbs)
                S.op("dve", lambda: V.tensor_copy(out=obf[:], in_=psb[ab][:, 0:128]), reads=[("psb", ab)], writes=["obf"])
                ptb = ps_bf(6)
                S.op("pe", lambda ptb=ptb: T_.transpose(ptb[:, 0:128], obf[:], identb[:]), reads=["obf", "identb"], writes=[("psb", 6)])
                S.op("act", lambda h=h, j=j, ptb=ptb: A_.copy(out=obT[:, h, j * 128:(j + 1) * 128], in_=ptb[:, 0:128]), reads=[("psb", 6)], writes=[("obT", h)])
        if dbg:
            for nm, tt in (("oaT", oaT), ("obT", obT)):
                d = dbg_tensor(nm, [128, NH * NOWN])
                tmp = sb(st, "dtmp_" + nm, [128, NOWN], F32)
                for h in range(NH):
                    S.op("dve", lambda h=h, tt=tt, tmp=tmp: V.tensor_copy(out=tmp[:], in_=tt[:, h, :]), reads=[(nm, h)], writes=["dbg_" + nm])
                    S.dma("sp", lambda h=h, d=d, tmp=tmp: nc.sync.dma_start(out=d[:, h * NOWN:(h + 1) * NOWN], in_=tmp[:]), reads=["dbg_" + nm])


def merge_phase(nc, S, sb, psb, W, hT_own, oaT, obT, mergedT):
    V, A_, P_, T_ = nc.vector, nc.scalar, nc.gpsimd, nc.tensor
    with ExitStack() as st:
        wpa = [sb(st, "wpa%d" % i, [128, 8, 128], BF16) for i in range(2)]
        wpb = [sb(st, "wpb%d" % i, [128, 8, 128], BF16) for i in range(2)]
        wga = [sb(st, "wga%d" % i, [128, 16, 128], BF16) for i in range(2)]
        wgb = [sb(st, "wgb%d" % i, [128, 16, 128], BF16) for i in range(2)]
        sg = [[sb(st, "sg%d_%d" % (i, j), [128, 512], F32) for j in range(2)] for i in range(2)]
        tt = [[sb(st, "tt%d_%d" % (i, j), [128, 512], F32) for j in range(2)] for i in range(2)]
        wpa_r = W["w_proj_a"].rearrange("(k p) n -> p k n", p=128)
        wpb_r = W["w_proj_b"].rearrange("(k p) n -> p k n", p=128)
        w_in_r = W["w_in"].rearrange("(c p) n -> p c n", p=128)
        it = 0
        for m in range(16):
            b = m % 2
            cs_ = slice(m * 128, (m + 1) * 128)
            S.dma("pool", lambda: nc.gpsimd.dma_start(out=wpa[b][:], in_=wpa_r[:, :, cs_]), writes=[("wpa", b)])
            S.dma("pool", lambda: nc.gpsimd.dma_start(out=wpb[b][:], in_=wpb_r[:, :, cs_]), writes=[("wpb", b)])
            for hh in range(2):
                S.dma("pool", lambda: nc.gpsimd.dma_start(out=wga[b][:, hh * 8:(hh + 1) * 8, :], in_=w_in_r[:, hh * 8:(hh + 1) * 8, 6144 + m * 128:6144 + (m + 1) * 128]),
                      writes=[("wga", b, hh)])
                S.dma("pool", lambda: nc.gpsimd.dma_start(out=wgb[b][:, hh * 8:(hh + 1) * 8, :], in_=w_in_r[:, hh * 8:(hh + 1) * 8, 8192 + m * 128:8192 + (m + 1) * 128]),
                      writes=[("wgb", b, hh)])
            for th in range(2):
                base = 4 * (it % 2)
                q = it % 2
                it += 1
                tok = slice(th * 512, (th + 1) * 512)
                for k in range(8):
                    S.op("pe", lambda: T_.matmul(psb[base][:, :], lhsT=wpa[b][:, k, :], rhs=oaT[:, k, tok], start=(k == 0), stop=(k == 7)),
                         reads=[("wpa", b)], writes=[("psb", base)])
                for k in range(8):
                    S.op("pe", lambda: T_.matmul(psb[base + 1][:, :], lhsT=wpb[b][:, k, :], rhs=obT[:, k, tok], start=(k == 0), stop=(k == 7)),
                         reads=[("wpb", b)], writes=[("psb", base + 1)])
                for c in range(16):
                    S.op("pe", lambda: T_.matmul(psb[base + 2][:, :], lhsT=wga[b][:, c, :], rhs=hT_own[:, c, tok], start=(c == 0), stop=(c == 15)),
                         reads=[("wga", b, c // 8)], writes=[("psb", base + 2)])
                for c in range(16):
                    S.op("pe", lambda: T_.matmul(psb[base + 3][:, :], lhsT=wgb[b][:, c, :], rhs=hT_own[:, c, tok], start=(c == 0), stop=(c == 15)),
                         reads=[("wgb", b, c // 8)], writes=[("psb", base + 3)])
                S.op("act", lambda: A_.activation(out=sg[q][0][:], in_=psb[base + 2][:, :], func=AF.Sigmoid), reads=[("psb", base + 2)], writes=[("sg", q, 0)])
                S.op("act", lambda: A_.activation(out=sg[q][1][:], in_=psb[base + 3][:, :], func=AF.Sigmoid), reads=[("psb", base + 3)], writes=[("sg", q, 1)])
                S.op("dve", lambda: V.tensor_tensor(out=tt[q][0][:], in0=psb[base][:, :], in1=sg[q][0][:], op=ALU.mult),
                     reads=[("psb", base), ("sg", q, 0)], writes=[("tt", q, 0)])
                S.op("dve", lambda: V.tensor_tensor(out=tt[q][1][:], in0=psb[base + 1][:, :], in1=sg[q][1][:], op=ALU.mult),
                     reads=[("psb", base + 1), ("sg", q, 1)], writes=[("tt", q, 1)])
                S.op("pool", lambda: P_.tensor_tensor(out=mergedT[:, m, tok], in0=tt[q][0][:], in1=tt[q][1][:], op=ALU.add),
                     reads=[("tt", q, 0), ("tt", q, 1)], writes=[("mergedT", m, th)])
        S.barrier()


def resid_phase(nc, S, sb, psb, W, mergedT, x_own, x1_d, mod_d):
    V, A_, P_, T_ = nc.vector, nc.scalar, nc.gpsimd, nc.tensor
    with ExitStack() as st:
        wo = [sb(st, "wo%d" % i, [128, 16, 512], BF16) for i in range(2)]
        gm_bc = sb(st, "gm_bc", [128, D], F32)
        xo = [sb(st, "xo%d" % i, [128, 512], F32) for i in range(2)]
        tmp = [sb(st, "rtmp%d" % i, [128, 512], F32) for i in range(2)]
        x1c = [sb(st, "x1c%d" % i, [128, 512], F32) for i in range(2)]
        S.dma("sp", lambda: nc.sync.dma_start(out=gm_bc[:], in_=bcast_rows(mod_d[2:3, :])), writes=["gm_bc"])
        w_out_r = W["w_out"].rearrange("(c p) n -> p c n", p=128)
        it = 0
        for n in range(4):
            b = n % 2
            ns = slice(n * 512, (n + 1) * 512)
            for q in range(4):
                S.dma("pool", lambda: nc.gpsimd.dma_start(out=wo[b][:, q * 4:(q + 1) * 4, :], in_=w_out_r[:, q * 4:(q + 1) * 4, ns]), writes=[("wo", b, q)])
            for t in range(8):
                pb = it % 4
                i2 = it % 2
                it += 1
                ts_ = slice(t * 128, (t + 1) * 128)
                S.dma("sp", lambda: nc.sync.dma_start(out=xo[i2][:], in_=x_own[ts_, ns]), writes=[("xo", i2)])
                for c in range(16):
                    S.op("pe", lambda: T_.matmul(psb[pb][:, :], lhsT=mergedT[:, c, ts_], rhs=wo[b][:, c, :], start=(c == 0), stop=(c == 15)),
                         reads=[("wo", b, c // 4)], writes=[("psb", pb)])
                S.op("dve", lambda: V.tensor_tensor(out=tmp[i2][:], in0=psb[pb][:, :], in1=gm_bc[:, ns], op=ALU.mult),
                     reads=[("psb", pb), "gm_bc"], writes=[("rtmp", i2)])
                S.op("pool", lambda: P_.tensor_tensor(out=x1c[i2][:], in0=tmp[i2][:], in1=xo[i2][:], op=ALU.add),
                     reads=[("rtmp", i2), ("xo", i2)], writes=[("x1c", i2)])
                S.dma("sp", lambda: nc.sync.dma_start(out=x1_d[ts_, ns], in_=x1c[i2][:]), reads=[("x1c", i2)], writes=[("x1d", t, n)])
        S.barrier()


def norm_router_phase(nc, S, sb, psb, W, cs, x1_d, h2T, Gt, Gf_col, shf_col, eps_t):
    V, A_, P_, T_ = nc.vector, nc.scalar, nc.gpsimd, nc.tensor
    BIG = 30000.0
    with ExitStack() as st:
        x1t = [sb(st, "x1t%d" % i, [128, D], F32) for i in range(2)]
        xn = [sb(st, "xn%d" % i, [128, D], F32) for i in range(2)]
        hf = [sb(st, "hf%d" % i, [128, 16, 128], F32) for i in range(2)]
        junk = sb(st, "njunk", [128, D], BF16)
        ss = [sb(st, "nss%d" % i, [128, 1], F32) for i in range(2)]
        rs = [sb(st, "nrs%d" % i, [128, 1], F32) for i in range(2)]
        wr = sb(st, "wr", [128, 16, 72], F32)
        brt = sb(st, "brt", [128, 72], F32)
        lg = sb(st, "lg", [128, 72], F32)
        sm = {n: sb(st, "r_" + n, [128, 1], F32) for n in ("gmax", "ngmax", "gsum", "pg", "m1", "m2", "dd", "rr", "den", "p1", "c1", "c2")}
        ohg = sb(st, "ohg", [128, 8], F32)
        pen = sb(st, "pen", [128, 8], F32)
        gjunk = sb(st, "gjunk", [128, 8], F32)
        em = sb(st, "em", [128, 64], F32)
        em2 = sb(st, "em2", [128, 64], F32)
        mask1 = sb(st, "mask1", [128, 64], F32)
        mask2 = sb(st, "mask2", [128, 64], F32)
        w_rg_r = W["w_rg"].rearrange("(c p) n -> p c n", p=128)
        w_re_r = W["w_re"].rearrange("(c p) n -> p c n", p=128)
        for hh in range(2):
            S.dma("sp", lambda: nc.sync.dma_start(out=wr[:, hh * 8:(hh + 1) * 8, 0:8], in_=w_rg_r[:, hh * 8:(hh + 1) * 8, :]), writes=[("wr", 0, hh)])
            S.dma("sp", lambda: nc.sync.dma_start(out=wr[:, hh * 8:(hh + 1) * 8, 8:72], in_=w_re_r[:, hh * 8:(hh + 1) * 8, :]), writes=[("wr", 1, hh)])
        S.dma("sp", lambda: nc.sync.dma_start(out=brt[:, 0:8], in_=bcast_rows(W["b_rg"][0:1, :])), writes=[("brt", 0)])
        S.dma("sp", lambda: nc.sync.dma_start(out=brt[:, 8:72], in_=bcast_rows(W["b_re"][0:1, :])), writes=[("brt", 1)])
        wr_keys = [("wr", 0, 0), ("wr", 0, 1), ("wr", 1, 0), ("wr", 1, 1)]
        for t in range(8):
            b = t % 2
            ts_ = slice(t * 128, (t + 1) * 128)
            for hh in range(2):
                S.dma("sp", lambda: nc.sync.dma_start(out=x1t[b][:, hh * 1024:(hh + 1) * 1024], in_=x1_d[ts_, hh * 1024:(hh + 1) * 1024]), writes=[("x1t", b, hh)])
            S.op("act", lambda: A_.activation(out=junk[:], in_=x1t[b][:], func=AF.Square, accum_out=ss[b][:]),
                 reads=[("x1t", b, 0), ("x1t", b, 1)], writes=[("nss", b), "njunk"])
            S.op("act", lambda: A_.activation(out=rs[b][:], in_=ss[b][:], func=AF.Sqrt, bias=eps_t[:], scale=1.0 / D),
                 reads=[("nss", b), "eps"], writes=[("nrs", b)])
            S.op("dve", lambda: V.reciprocal(out=rs[b][:], in_=rs[b][:]), reads=[("nrs", b)], writes=[("nrs", b)])
            S.op("dve", lambda: V.tensor_scalar(out=xn[b][:], in0=x1t[b][:], scalar1=rs[b][:, 0:1], scalar2=None, op0=ALU.mult),
                 reads=[("x1t", b, 0), ("x1t", b, 1), ("nrs", b)], writes=[("xn", b)])
            for g in range(4):
                for k in range(4):
                    c = g * 4 + k
                    S.op("pe", lambda: T_.matmul(psb[g][:, k * 128:(k + 1) * 128], lhsT=xn[b][:, c * 128:(c + 1) * 128], rhs=cs["ident"][:], start=True, stop=True),
                         reads=[("xn", b), "c_ident"], writes=[("psb", g)])
                for k in range(4):
                    c = g * 4 + k
                    S.op("act", lambda: A_.activation(out=hf[b][:, c, :], in_=psb[g][:, k * 128:(k + 1) * 128], func=AF.Identity,
                                                      bias=shf_col[:, c:c + 1], scale=Gf_col[:, c:c + 1]),
                         reads=[("psb", g), "shf_col", "Gf_col"], writes=[("hf", b, g)])
            S.op("pool", lambda: P_.tensor_copy(out=h2T[:, :, ts_], in_=hf[b][:, :, :]), reads=[("hf", b, g) for g in range(4)], writes=[("h2T", t)])
            rb = 4 + b
            for c in range(16):
                S.op("pe", lambda: T_.matmul(psb[rb][:, 0:72], lhsT=hf[b][:, c, :], rhs=wr[:, c, :], start=(c == 0), stop=(c == 15)),
                     reads=[("hf", b, c // 4)] + wr_keys, writes=[("psb", rb)])
            S.op("dve", lambda: V.tensor_tensor(out=lg[:], in0=psb[rb][:, 0:72], in1=brt[:], op=ALU.add), reads=[("psb", rb), ("brt", 0), ("brt", 1)], writes=["lg"])
            S.op("dve", lambda: V.tensor_reduce(out=sm["gmax"][:], in_=lg[:, 0:8], axis=AX.X, op=ALU.max), reads=["lg"], writes=["gmax"])
            S.op("dve", lambda: V.tensor_scalar(out=ohg[:], in0=lg[:, 0:8], scalar1=sm["gmax"][:, 0:1], scalar2=None, op0=ALU.is_ge), reads=["lg", "gmax"], writes=["ohg"])
            S.op("dve", lambda: V.tensor_scalar(out=sm["ngmax"][:], in0=sm["gmax"][:], scalar1=-1.0, scalar2=None, op0=ALU.mult), reads=["gmax"], writes=["ngmax"])
            S.op("act", lambda: A_.activation(out=gjunk[:], in_=lg[:, 0:8], func=AF.Exp, bias=sm["ngmax"][:, 0:1], scale=1.0, accum_out=sm["gsum"][:]),
                 reads=["lg", "ngmax"], writes=["gsum", "gjunk"])
            S.op("dve", lambda: V.reciprocal(out=sm["pg"][:], in_=sm["gsum"][:]), reads=["gsum"], writes=["pg"])
            S.op("dve", lambda: V.tensor_scalar(out=pen[:], in0=ohg[:], scalar1=BIG, scalar2=-BIG, op0=ALU.mult, op1=ALU.add), reads=["ohg"], writes=["pen"])
            for g in range(8):
                S.op("dve", lambda: V.tensor_scalar(out=em[:, g * 8:(g + 1) * 8], in0=lg[:, 8 + g * 8:16 + g * 8], scalar1=pen[:, g:g + 1], scalar2=None, op0=ALU.add),
                     reads=["lg", "pen"], writes=[("em", g)])
            emk = [("em", g) for g in range(8)]
            S.op("dve", lambda: V.tensor_reduce(out=sm["m1"][:], in_=em[:], axis=AX.X, op=ALU.max), reads=emk, writes=["m1"])
            S.op("dve", lambda: V.tensor_scalar(out=mask1[:], in0=em[:], scalar1=sm["m1"][:, 0:1], scalar2=None, op0=ALU.is_ge), reads=emk + ["m1"], writes=["mask1"])
            S.op("dve", lambda: V.scalar_tensor_tensor(out=em2[:], in0=mask1[:], scalar=-BIG, in1=em[:], op0=ALU.mult, op1=ALU.add), reads=emk + ["mask1"], writes=["em2"])
            S.op("dve", lambda: V.tensor_reduce(out=sm["m2"][:], in_=em2[:], axis=AX.X, op=ALU.max), reads=["em2"], writes=["m2"])
            S.op("dve", lambda: V.tensor_scalar(out=mask2[:], in0=em2[:], scalar1=sm["m2"][:, 0:1], scalar2=None, op0=ALU.is_ge), reads=["em2", "m2"], writes=["mask2"])
            S.op("dve", lambda: V.tensor_tensor(out=sm["dd"][:], in0=sm["m2"][:], in1=sm["m1"][:], op=ALU.subtract), reads=["m1", "m2"], writes=["dd"])
            S.op("act", lambda: A_.activation(out=sm["rr"][:], in_=sm["dd"][:], func=AF.Exp), reads=["dd"], writes=["rr"])
            S.op("dve", lambda: V.tensor_scalar(out=sm["den"][:], in0=sm["rr"][:], scalar1=1.0, scalar2=None, op0=ALU.add), reads=["rr"], writes=["den"])
            S.op("dve", lambda: V.reciprocal(out=sm["p1"][:], in_=sm["den"][:]), reads=["den"], writes=["p1"])
            S.op("dve", lambda: V.tensor_tensor(out=sm["c1"][:], in0=sm["p1"][:], in1=sm["pg"][:], op=ALU.mult), reads=["p1", "pg"], writes=["c1"])
            S.op("dve", lambda: V.tensor_tensor(out=sm["c2"][:], in0=sm["c1"][:], in1=sm["rr"][:], op=ALU.mult), reads=["c1", "rr"], writes=["c2"])
            S.op("dve", lambda: V.tensor_scalar(out=Gt[:, t, :], in0=mask1[:], scalar1=sm["c1"][:, 0:1], scalar2=None, op0=ALU.mult), reads=["mask1", "c1"], writes=[("Gt", t)])
            S.op("dve", lambda: V.scalar_tensor_tensor(out=Gt[:, t, :], in0=mask2[:], scalar=sm["c2"][:, 0:1], in1=Gt[:, t, :], op0=ALU.mult, op1=ALU.add),
                 reads=["mask2", "c2", ("Gt", t)], writes=[("Gt", t)])
        S.barrier()


def moe_phase(nc, S, sb, psb, W, h2T, Gt, acc, NE=64):
    V, A_, P_, T_ = nc.vector, nc.scalar, nc.gpsimd, nc.tensor
    NWB = 4
    NU = NE * 8
    with ExitStack() as st:
        wg = [sb(st, "wg%d" % i, [128, 16, 128], BF16) for i in range(NWB)]
        wu = [sb(st, "wu%d" % i, [128, 16, 128], BF16) for i in range(NWB)]
        wd = sb(st, "wd", [128, 8, D], BF16)
        actT = sb(st, "actT", [128, 8, NOWN], BF16)
        sl = [sb(st, "sl%d" % i, [128, 512], F32) for i in range(2)]
        SBUF_LOG.append(("moe", nc.sbuf_bytes_remaining))
        for t in range(8):
            S.op("dve", lambda: V.memset(acc[:, t, :], 0.0), writes=[("acc", t, n) for n in range(4)])

        def issue_unit(u):
            e, f = u // 8, u % 8
            wb = u % NWB
            rows_g = W["w_eg"][e * D:(e + 1) * D, :].rearrange("(c p) n -> p c n", p=128)
            rows_u = W["w_eu"][e * D:(e + 1) * D, :].rearrange("(c p) n -> p c n", p=128)
            fs = slice(f * 128, (f + 1) * 128)
            for hh in range(2):
                S.dma("pool", lambda: nc.gpsimd.dma_start(out=wg[wb][:, hh * 8:(hh + 1) * 8, :], in_=rows_g[:, hh * 8:(hh + 1) * 8, fs]), writes=[("wg", wb, hh)])
            for hh in range(2):
                S.dma("pool", lambda: nc.gpsimd.dma_start(out=wu[wb][:, hh * 8:(hh + 1) * 8, :], in_=rows_u[:, hh * 8:(hh + 1) * 8, fs]), writes=[("wu", wb, hh)])

        def issue_wd(e):
            rows_d = W["w_ed"][e * 1024:(e + 1) * 1024, :].rearrange("(f p) n -> p f n", p=128)
            for f2 in range(8):
                S.dma("pool", lambda: nc.gpsimd.dma_start(out=wd[:, f2, :], in_=rows_d[:, f2, :]), writes=[("wd", f2)])

        issue_wd(0)
        for u in range(NWB - 1):
            issue_unit(u)
        it = 0
        dn = 0
        for u in range(NU):
            e, f = u // 8, u % 8
            wb = u % NWB
            if u + NWB - 1 < NU:
                issue_unit(u + NWB - 1)
            for half in range(2):
                ab = it % 2
                it += 1
                tok = slice(half * 512, (half + 1) * 512)
                for c in range(16):
                    S.op("pe", lambda: T_.matmul(psb[ab][:, :], lhsT=wg[wb][:, c, :], rhs=h2T[:, c, tok], start=(c == 0), stop=(c == 15)),
                         reads=[("wg", wb, c // 8)], writes=[("psb", ab)])
                for c in range(16):
                    S.op("pe", lambda: T_.matmul(psb[2 + ab][:, :], lhsT=wu[wb][:, c, :], rhs=h2T[:, c, tok], start=(c == 0), stop=(c == 15)),
                         reads=[("wu", wb, c // 8)], writes=[("psb", 2 + ab)])
                S.op("act", lambda: A_.activation(out=sl[ab][:], in_=psb[ab][:, :], func=AF.Silu), reads=[("psb", ab)], writes=[("sl", ab)])
                S.op("dve", lambda: V.tensor_tensor(out=actT[:, f, tok], in0=psb[2 + ab][:, :], in1=sl[ab][:], op=ALU.mult),
                     reads=[("psb", 2 + ab), ("sl", ab)], writes=[("actT", f, half)])
            if f == 7:
                for t in range(8):
                    ts_ = slice(t * 128, (t + 1) * 128)
                    for n in range(4):
                        db = 4 + dn % 4
                        dn += 1
                        ns = slice(n * 512, (n + 1) * 512)
                        for f2 in range(8):
                            S.op("pe", lambda: T_.matmul(psb[db][:, :], lhsT=actT[:, f2, ts_], rhs=wd[:, f2, ns], start=(f2 == 0), stop=(f2 == 7)),
                                 reads=[("actT", f2, t // 4), ("wd", f2)], writes=[("psb", db)])
                        S.op("dve", lambda: V.scalar_tensor_tensor(out=acc[:, t, ns], in0=psb[db][:, :], scalar=Gt[:, t, e:e + 1], in1=acc[:, t, ns], op0=ALU.mult, op1=ALU.add),
                             reads=[("psb", db), ("acc", t, n)], writes=[("acc", t, n)])
                if e + 1 < NE:
                    issue_wd(e + 1)
        S.barrier()


def final_phase(nc, S, sb, W, acc, x1_d, mod_d, out_own, eps_t):
    V, A_, P_ = nc.vector, nc.scalar, nc.gpsimd
    with ExitStack() as st:
        gf_bc = sb(st, "gf_bc", [128, D], F32)
        gfin_bc = sb(st, "gfin_bc", [128, D], F32)
        x1t = [sb(st, "fx1t%d" % i, [128, D], F32) for i in range(2)]
        ot = [sb(st, "fot%d" % i, [128, D], F32) for i in range(2)]
        junk = sb(st, "fjunk", [128, D], BF16)
        ss = [sb(st, "fss%d" % i, [128, 1], F32) for i in range(2)]
        rs = [sb(st, "frs%d" % i, [128, 1], F32) for i in range(2)]
        S.dma("sp", lambda: nc.sync.dma_start(out=gf_bc[:], in_=bcast_rows(mod_d[5:6, :])), writes=["gf_bc"])
        S.dma("sp", lambda: nc.sync.dma_start(out=gfin_bc[:], in_=bcast_rows(W["g_final"][0:1, :])), writes=["gfin_bc"])
        for t in range(8):
            b = t % 2
            ts_ = slice(t * 128, (t + 1) * 128)
            for hh in range(2):
                S.dma("sp", lambda: nc.sync.dma_start(out=x1t[b][:, hh * 1024:(hh + 1) * 1024], in_=x1_d[ts_, hh * 1024:(hh + 1) * 1024]), writes=[("fx1t", b, hh)])
            S.op("dve", lambda: V.tensor_tensor(out=ot[b][:], in0=acc[:, t, :], in1=gf_bc[:], op=ALU.mult), reads=["gf_bc"], writes=[("fot", b)])
            S.op("pool", lambda: P_.tensor_tensor(out=ot[b][:], in0=ot[b][:], in1=x1t[b][:], op=ALU.add),
                 reads=[("fot", b), ("fx1t", b, 0), ("fx1t", b, 1)], writes=[("fot", b)])
            S.op("act", lambda: A_.activation(out=junk[:], in_=ot[b][:], func=AF.Square, accum_out=ss[b][:]), reads=[("fot", b)], writes=[("fss", b), "fjunk"])
            S.op("act", lambda: A_.activation(out=rs[b][:], in_=ss[b][:], func=AF.Sqrt, bias=eps_t[:], scale=1.0 / D), reads=[("fss", b), "eps"], writes=[("frs", b)])
            S.op("dve", lambda: V.reciprocal(out=rs[b][:], in_=rs[b][:]), reads=[("frs", b)], writes=[("frs", b)])
            S.op("dve", lambda: V.scalar_tensor_tensor(out=ot[b][:], in0=ot[b][:], scalar=rs[b][:, 0:1], in1=gfin_bc[:], op0=ALU.mult, op1=ALU.mult),
                 reads=[("fot", b), ("frs", b), "gfin_bc"], writes=[("fot", b)])
            for hh in range(2):
                S.dma("sp", lambda: nc.sync.dma_start(out=out_own[ts_, hh * 1024:(hh + 1) * 1024], in_=ot[b][:, hh * 1024:(hh + 1) * 1024]), reads=[("fot", b)], writes=[("out", t, hh)])


def own_qblocks(half):
    qb = []
    for p in range(4):
        qb.append(2 * p + half)
        qb.append(15 - 2 * p - half)
    return qb


def own_token_index(half):
    return np.concatenate([np.arange(q * 128, (q + 1) * 128) for q in own_qblocks(half)])


def col_layout(v):
    return np.ascontiguousarray(np.asarray(v, np.float32).reshape(-1, 128).T)


def make_shared(inp, stage):
    f = lambda a: np.ascontiguousarray(np.asarray(a, np.float32))
    sh = {
        "rel_bias_table": f(inp["rel_bias_table"]), "w_ada": f(inp["w_ada"][0]), "b_ada": f(inp["b_ada"][0]).reshape(1, -1),
        "g_mix_col": col_layout(inp["g_mix"][0]), "w_in": f(inp["w_in"][0]),
        "lq1": f(inp["lambda_q1"][0]).reshape(1, -1), "lk1": f(inp["lambda_k1"][0]).reshape(1, -1),
        "lq2": f(inp["lambda_q2"][0]).reshape(1, -1), "lk2": f(inp["lambda_k2"][0]).reshape(1, -1),
        "g_subln": f(inp["g_subln"][0]).reshape(1, -1), "w_proj_a": f(inp["w_proj_a"][0]), "w_proj_b": f(inp["w_proj_b"][0]),
        "w_out": f(inp["w_out"][0]), "g_ffn_col": col_layout(inp["g_ffn"][0]),
        "w_rg": f(inp["w_router_group"][0]), "b_rg": f(inp["b_router_group"][0]).reshape(1, -1),
        "w_re": f(inp["w_router_expert"][0]), "b_re": f(inp["b_router_expert"][0]).reshape(1, -1),
        "g_final": f(inp["g_final"]).reshape(1, -1),
    }
    if stage >= 7:
        sh["w_eg"] = f(inp["w_expert_gate"][0]).reshape(64 * D, 1024)
        sh["w_eu"] = f(inp["w_expert_up"][0]).reshape(64 * D, 1024)
        sh["w_ed"] = f(inp["w_expert_down"][0]).reshape(64 * 1024, D)
    for k, v in host_consts().items():
        sh["c_" + k] = v
    return sh


def make_core_map(inp, shared, core):
    b, half = core // 2, core % 2
    xb = np.asarray(inp["x"][b], np.float32)
    m = dict(shared)
    m["x_all"] = np.ascontiguousarray(xb)
    m["x_own"] = np.ascontiguousarray(xb[own_token_index(half)])
    m["c_col"] = col_layout(inp["c"][b])
    m["halfv"] = np.full((128, 1), float(half), np.float32)
    return m


def kernel(**inputs):
    nc, _ = build(stage=99, dbg=False)
    shared = make_shared(inputs, 99)
    in_maps = [make_core_map(inputs, shared, c) for c in range(8)]
    res = run_bass_kernel_spmd(nc, in_maps, core_ids=list(range(8)))
    out = np.zeros((4, SEQ, D), np.float32)
    for c in range(8):
        out[c // 2, own_token_index(c % 2)] = res.results[c]["out_own"]
    return out
```

```python
import math
from contextlib import ExitStack

import numpy as np
import concourse.bass as bass
import concourse.mybir as mybir
from concourse.bass_utils import run_bass_kernel_spmd

F32 = mybir.dt.float32
BF16 = mybir.dt.bfloat16
I32 = mybir.dt.int32
U32 = mybir.dt.uint32
AF = mybir.ActivationFunctionType
ALU = mybir.AluOpType
AX = mybir.AxisListType

D = 2048
SEQ = 2048
NOWN = 1024
NH = 8
NBLK = 80
EPS = 1e-6
LAM_INIT = 0.8 - 0.6 * math.exp(0.0)
NEG = -30000.0
SBUF_LOG = []
MOE_NWB = 4


class Sched:
    def __init__(self, nc, stack, n_dma_sems=32):
        self.nc = nc
        self.eng = {"pe": nc.tensor, "act": nc.scalar, "dve": nc.vector,
                    "pool": nc.gpsimd, "sp": nc.sync}
        self.sem = {e: stack.enter_context(nc.semaphore("s_" + e)) for e in self.eng}
        self.cnt = {e: 0 for e in self.eng}
        self.dsem = [stack.enter_context(nc.semaphore("d%d" % i)) for i in range(n_dma_sems)]
        self.dcnt = [0] * n_dma_sems
        self.dnext = 0
        self.seen = {e: {} for e in self.eng}
        self.lastw = {}
        self.reads = {}
        self.semobj = {}
        for e, s in self.sem.items():
            self.semobj[("e", e)] = s
        for i, s in enumerate(self.dsem):
            self.semobj[("d", i)] = s

    def _wait(self, e, ev):
        sid, val, src = ev
        if src == "pe" and e == "pe":
            return
        if self.seen[e].get(sid, 0) >= val:
            return
        self.seen[e][sid] = val
        self.eng[e].wait_ge(self.semobj[sid], val)

    def _deps(self, e, reads, writes, extra):
        evs = []
        for k in reads:
            if k in self.lastw:
                evs.append(self.lastw[k])
        for k in writes:
            if k in self.lastw:
                evs.append(self.lastw[k])
            evs.extend(self.reads.get(k, []))
        evs.extend(extra)
        best = {}
        for ev in evs:
            sid, val, src = ev
            if src == "pe" and e == "pe":
                continue
            if sid not in best or best[sid][1] < val:
                best[sid] = ev
        for ev in best.values():
            self._wait(e, ev)

    def _record(self, ev, reads, writes):
        for k in reads:
            lst = self.reads.setdefault(k, [])
            lst.append(ev)
        for k in writes:
            self.lastw[k] = ev
            self.reads[k] = []

    def op(self, e, fn, reads=(), writes=(), extra=()):
        self._deps(e, reads, writes, extra)
        ins = fn()
        self.cnt[e] += 1
        ins.then_inc(self.sem[e], 1)
        ev = (("e", e), self.cnt[e], e)
        self._record(ev, reads, writes)
        return ev

    def dma(self, q, fn, reads=(), writes=(), extra=()):
        i = self.dnext
        self.dnext = (self.dnext + 1) % len(self.dsem)
        sid = ("d", i)
        if self.dcnt[i] > 0:
            self._wait(q, (sid, self.dcnt[i], "dma"))
        self._deps(q, reads, writes, extra)
        ins = fn()
        self.dcnt[i] += 16
        ins.then_inc(self.dsem[i], 16)
        ev = (sid, self.dcnt[i], "dma")
        self._record(ev, reads, writes)
        return ev

    def all_events(self):
        evs = []
        for i, c in enumerate(self.dcnt):
            if c:
                evs.append((("d", i), c, "dma"))
        for en in self.eng:
            if self.cnt[en]:
                evs.append((("e", en), self.cnt[en], "x"))
        return evs

    def barrier(self):
        evs = self.all_events()
        for e in self.eng:
            for ev in evs:
                self._wait(e, ev)
        self.lastw = {}
        self.reads = {}

    def finish(self, e="sp"):
        for ev in self.all_events():
            self._wait(e, ev)


def bcast_rows(ap, nparts=128):
    n = ap.shape[-1]
    return bass.AP(ap.tensor, ap.offset, [[0, nparts], [1, n]])


def rel_bucket_np(n):
    n = np.maximum(n, 0)
    nf = np.maximum(n, 1).astype(np.float32)
    large = 16 + (np.log(nf / np.float32(16)) / np.float32(math.log(128 / 16)) * np.float32(16)).astype(np.int32)
    large = np.minimum(large, 31)
    return np.where(n < 16, n, large)


def host_consts():
    i = np.arange(128)
    c = {}
    c["ident"] = np.eye(128, dtype=np.float32)
    c["antiI"] = np.eye(128, dtype=np.float32)[::-1].copy()
    c["tri"] = (i[:, None] < i[None, :]).astype(np.float32)
    c["ugt"] = (i[:, None] > i[None, :]).astype(np.float32)
    c["ones"] = np.ones((128, 128), np.float32)
    n = np.arange(640) - 256
    oh = np.zeros((32, 640), np.float32)
    valid = (n >= 0) & (n < 256)
    bk = rel_bucket_np(np.clip(n, 0, None))
    oh[bk[valid], np.nonzero(valid)[0]] = 1.0
    oh[31, valid] -= 1.0
    c["ohb"] = oh
    c["negrow"] = np.where(n < 0, NEG, 0.0).astype(np.float32)[None, :]
    c["iota128"] = np.tile(np.arange(128, dtype=np.float32)[None, :], (128, 1))
    c["thr"] = np.tile((128.0 * np.arange(16, dtype=np.float32))[None, :], (128, 1))
    c["pcol"] = np.arange(128, dtype=np.float32)[:, None].copy()
    return c


CONST_SHAPES = {"ident": [128, 128], "antiI": [128, 128], "tri": [128, 128], "ugt": [128, 128],
                "ones": [128, 128], "ohb": [32, 640], "negrow": [1, 640], "iota128": [128, 128],
                "thr": [128, 16], "pcol": [128, 1]}

WEIGHT_SHAPES = {
    "rel_bias_table": [32, 8], "w_ada": [D, 6 * D], "b_ada": [1, 6 * D], "g_mix_col": [128, 16],
    "w_in": [D, 10240], "lq1": [1, 64], "lk1": [1, 64], "lq2": [1, 64], "lk2": [1, 64],
    "g_subln": [1, 128], "w_proj_a": [1024, D], "w_proj_b": [1024, D], "w_out": [D, D],
    "g_ffn_col": [128, 16], "w_rg": [D, 8], "b_rg": [1, 8], "w_re": [D, 64], "b_re": [1, 64],
    "w_eg": [64 * D, 1024], "w_eu": [64 * D, 1024], "w_ed": [64 * 1024, D], "g_final": [1, D],
}


def build(stage=99, dbg=False):
    nc = bass.Bass("TRN2", target_bir_lowering=False)
    din = {}

    def dram_in(name, shape, dt=F32):
        din[name] = nc.dram_tensor(name, list(shape), dt, kind="ExternalInput").ap()
        return din[name]

    x_all = dram_in("x_all", [SEQ, D])
    x_own = dram_in("x_own", [NOWN, D])
    c_col = dram_in("c_col", [128, 16])
    halfv = dram_in("halfv", [128, 1])
    W = {k: dram_in(k, s) for k, s in WEIGHT_SHAPES.items() if stage >= 7 or k not in ("w_eg", "w_eu", "w_ed")}
    C = {k: dram_in("c_" + k, s) for k, s in CONST_SHAPES.items()}
    out_own = nc.dram_tensor("out_own", [NOWN, D], F32, kind="ExternalOutput").ap()
    dbg_out = {}

    def dbg_tensor(name, shape):
        dbg_out[name] = nc.dram_tensor("dbg_" + name, list(shape), F32, kind="ExternalOutput").ap()
        return dbg_out[name]

    tv_d = nc.dram_tensor("tv_scratch", [8, 640], F32, kind="Internal")
    mod_d = nc.dram_tensor("mod_scratch", [6, D], F32, kind="Internal").ap()
    x1_d = nc.dram_tensor("dbg_x1" if dbg else "x1_scratch", [NOWN, D], F32, kind="ExternalOutput" if dbg else "Internal").ap()

    with ExitStack() as st0:
        S = Sched(nc, st0)
        V, A_, P_, T_ = nc.vector, nc.scalar, nc.gpsimd, nc.tensor

        def sb(stack, name, shape, dt):
            return stack.enter_context(nc.sbuf_tensor(name, list(shape), dt))

        psb = [st0.enter_context(nc.psum_tensor("psb%d" % i, [128, 512], F32)) for i in range(8)]

        def ps_bf(i):
            return psb[i][:].bitcast(BF16)

        cs = {}
        for k in ("ident", "antiI", "tri", "ugt", "ones"):
            cs[k] = sb(st0, "k_" + k, [128, 128], F32)
            S.dma("sp", lambda k=k: nc.sync.dma_start(out=cs[k][:], in_=C[k][:, :]), writes=["c_" + k])
        identb = sb(st0, "identb", [128, 128], BF16)
        S.op("dve", lambda: V.tensor_copy(out=identb[:], in_=cs["ident"][:]), reads=["c_ident"], writes=["identb"])
        half_t = sb(st0, "half_t", [128, 1], F32)
        S.dma("sp", lambda: nc.sync.dma_start(out=half_t[:], in_=halfv[:, :]), writes=["half"])

        Gm_col = sb(st0, "Gm_col", [128, 16], F32)
        shm_col = sb(st0, "shm_col", [128, 16], F32)
        Gf_col = sb(st0, "Gf_col", [128, 16], F32)
        shf_col = sb(st0, "shf_col", [128, 16], F32)

        with ExitStack() as st:
            ccol = sb(st, "ccol", [128, 16], F32)
            scol = sb(st, "scol", [128, 16], F32)
            gmix = sb(st, "gmix", [128, 16], F32)
            S.dma("sp", lambda: nc.sync.dma_start(out=ccol[:], in_=c_col[:, :]), writes=["ccol"])
            S.dma("sp", lambda: nc.sync.dma_start(out=gmix[:], in_=W["g_mix_col"][:, :]), writes=["gmix"])
            gffn = sb(st, "gffn", [128, 16], F32)
            S.dma("sp", lambda: nc.sync.dma_start(out=gffn[:], in_=W["g_ffn_col"][:, :]), writes=["gffn"])
            S.op("act", lambda: A_.activation(out=scol[:], in_=ccol[:], func=AF.Silu), reads=["ccol"], writes=["scol"])
            wa = [sb(st, "wa%d" % i, [128, 16, 512], F32) for i in range(2)]
            brow = [sb(st, "brow%d" % i, [1, 512], F32) for i in range(2)]
            mrow = [sb(st, "mrow%d" % i, [1, 512], F32) for i in range(2)]
            one11 = sb(st, "one11", [1, 1], F32)
            S.op("dve", lambda: V.memset(one11[:], 1.0), writes=["one11"])
            w_ada_r = W["w_ada"].rearrange("(c p) n -> p c n", p=128)
            for j in range(24):
                m, jj = j // 4, j % 4
                b = j % 2
                for hh in range(2):
                    S.dma("sp", lambda b=b, j=j, hh=hh: nc.sync.dma_start(
                        out=wa[b][:, hh * 8:(hh + 1) * 8, :], in_=w_ada_r[:, hh * 8:(hh + 1) * 8, j * 512:(j + 1) * 512]),
                        writes=[("wa", b, hh)])
                S.dma("sp", lambda b=b, j=j: nc.sync.dma_start(out=brow[b][:], in_=W["b_ada"][0:1, j * 512:(j + 1) * 512]), writes=[("brow", b)])
                pb = psb[b]
                for c in range(16):
                    S.op("pe", lambda c=c, b=b, pb=pb: T_.matmul(pb[0:1, :], lhsT=scol[:, c:c + 1], rhs=wa[b][:, c, :], start=(c == 0), stop=(c == 15)),
                         reads=["scol", ("wa", b, c // 8)], writes=[("psb", b)])
                S.op("dve", lambda b=b, pb=pb: V.tensor_tensor(out=mrow[b][:], in0=pb[0:1, :], in1=brow[b][:], op=ALU.add),
                     reads=[("psb", b), ("brow", b)], writes=[("mrow", b)])
                if m in (0, 1, 3, 4):
                    pc = psb[2 + b]
                    for q in range(4):
                        S.op("pe", lambda q=q, b=b, pc=pc: T_.matmul(pc[:, q:q + 1], lhsT=mrow[b][0:1, q * 128:(q + 1) * 128], rhs=one11[0:1, 0:1], start=True, stop=True),
                             reads=[("mrow", b), "one11"], writes=[("psb", 2 + b)])
                    dst = {0: shm_col, 1: Gm_col, 3: shf_col, 4: Gf_col}[m]
                    key = {0: "shm_col", 1: "Gm_col", 3: "shf_col", 4: "Gf_col"}[m]
                    S.op("dve", lambda b=b, pc=pc, dst=dst, jj=jj: V.tensor_copy(out=dst[:, jj * 4:(jj + 1) * 4], in_=pc[:, 0:4]),
                         reads=[("psb", 2 + b)], writes=[key])
                S.dma("sp", lambda b=b, m=m, jj=jj: nc.sync.dma_start(out=mod_d[m:m + 1, jj * 512:(jj + 1) * 512], in_=mrow[b][:]),
                      reads=[("mrow", b)], writes=[("mod_d", j)])
            S.op("dve", lambda: V.scalar_tensor_tensor(out=Gm_col[:], in0=Gm_col[:], scalar=1.0, in1=gmix[:], op0=ALU.add, op1=ALU.mult),
                 reads=["Gm_col", "gmix"], writes=["Gm_col"])
            S.op("dve", lambda: V.scalar_tensor_tensor(out=Gf_col[:], in0=Gf_col[:], scalar=1.0, in1=gffn[:], op0=ALU.add, op1=ALU.mult),
                 reads=["Gf_col", "gffn"], writes=["Gf_col"])
            if dbg:
                d = dbg_tensor("Gm_col", [128, 16]); S.dma("sp", lambda: nc.sync.dma_start(out=d[:, :], in_=Gm_col[:]), reads=["Gm_col"])
                d2 = dbg_tensor("shm_col", [128, 16]); S.dma("sp", lambda: nc.sync.dma_start(out=d2[:, :], in_=shm_col[:]), reads=["shm_col"])
            S.barrier()
        if stage <= 0:
            S.finish("sp")
            return nc, dbg_out

        stR = ExitStack()
        stA = ExitStack()
        eps_t = sb(st0, "eps_t", [128, 1], F32)
        S.op("dve", lambda: V.memset(eps_t[:], EPS), writes=["eps"])
        hT_own = sb(stA, "hT_own", [128, 16, NOWN], BF16)
        oaT = sb(stA, "oaT", [128, NH, NOWN], BF16)
        obT = sb(stA, "obT", [128, NH, NOWN], BF16)

        def norm_tile_to_hT(stk_bufs, src_ap, dstT, col0, tag):
            xt, xb, ss, rs, junk = stk_bufs
            for hh in range(2):
                S.dma("sp", lambda hh=hh: nc.sync.dma_start(out=xt[:, hh * 1024:(hh + 1) * 1024], in_=src_ap[:, hh * 1024:(hh + 1) * 1024]), writes=[(tag, "xt", hh)])
            S.op("act", lambda: A_.activation(out=junk[:], in_=xt[:], func=AF.Square, accum_out=ss[:]),
                 reads=[(tag, "xt", 0), (tag, "xt", 1)], writes=[(tag, "ss"), (tag, "junk")])
            S.op("act", lambda: A_.activation(out=rs[:], in_=ss[:], func=AF.Sqrt, bias=eps_t[:], scale=1.0 / D),
                 reads=[(tag, "ss"), "eps"], writes=[(tag, "rs")])
            S.op("dve", lambda: V.reciprocal(out=rs[:], in_=rs[:]), reads=[(tag, "rs")], writes=[(tag, "rs")])
            S.op("dve", lambda: V.tensor_scalar(out=xb[:], in0=xt[:], scalar1=rs[:, 0:1], scalar2=None, op0=ALU.mult),
                 reads=[(tag, "xt", 0), (tag, "xt", 1), (tag, "rs")], writes=[(tag, "xb")])
            for g in range(2):
                pt = ps_bf(6 + g)
                for c8 in range(8):
                    c = g * 8 + c8
                    S.op("pe", lambda c=c, c8=c8, pt=pt: T_.transpose(pt[:, c8 * 128:(c8 + 1) * 128], xb[:, c * 128:(c + 1) * 128], identb[:]),
                         reads=[(tag, "xb"), "identb"], writes=[("psb", 6 + g)])
                for c8 in range(8):
                    c = g * 8 + c8
                    S.op("act", lambda c=c, c8=c8, pt=pt: A_.activation(out=dstT[:, c, col0:col0 + 128], in_=pt[:, c8 * 128:(c8 + 1) * 128],
                                                                      func=AF.Identity, bias=shm_col[:, c:c + 1], scale=Gm_col[:, c:c + 1]),
                         reads=[("psb", 6 + g), "shm_col", "Gm_col"], writes=[(tag, "hT", col0 // 512)])

        with ExitStack() as stB:
            hT_all = sb(stB, "hT_all", [128, 16, SEQ], BF16)
            with ExitStack() as st:
                bufs = []
                for i in range(2):
                    bufs.append((sb(st, "xt%d" % i, [128, D], F32), sb(st, "xb%d" % i, [128, D], BF16),
                                 sb(st, "ss%d" % i, [128, 1], F32), sb(st, "rs%d" % i, [128, 1], F32),
                                 sb(st, "junk%d" % i, [128, D], BF16)))
                for t in range(16):
                    norm_tile_to_hT(bufs[t % 2], x_all[t * 128:(t + 1) * 128, :], hT_all, t * 128, ("n", t % 2))
                S.barrier()
                for t in range(8):
                    norm_tile_to_hT(bufs[t % 2], x_own[t * 128:(t + 1) * 128, :], hT_own, t * 128, ("n", t % 2))
                S.barrier()
            attention_phase(nc, S, stB, sb, psb, ps_bf, cs, identb, half_t, W, C, tv_d, hT_all, hT_own, oaT, obT, False, dbg_tensor)
            S.barrier()
        mergedT = stR.enter_context(nc.sbuf_tensor("mergedT", [128, 16, NOWN], BF16, side="right"))
        merge_phase(nc, S, sb, psb, W, hT_own, oaT, obT, mergedT)
        S.barrier()
        stA.close()
        h2T = sb(st0, "h2T", [128, 16, NOWN], BF16)
        Gt = sb(st0, "Gt", [128, 8, 64], F32)
        resid_phase(nc, S, sb, psb, W, mergedT, x_own, x1_d, mod_d)
        S.barrier()
        stR.close()
        stW = ExitStack()
        moe_bufs = None
        if stage > 5:
            moe_bufs = moe_weight_bufs(nc, stW, side="right")
            moe_prefetch(nc, S, W, *moe_bufs)
        norm_router_phase(nc, S, sb, psb, W, cs, x1_d, h2T, Gt, Gf_col, shf_col, eps_t)
        S.barrier()
        if dbg:
            d = dbg_tensor("Gt", [128, 8 * 64])
            S.dma("sp", lambda: nc.sync.dma_start(out=d[:, :], in_=Gt[:].rearrange("p a b -> p (a b)")))
            d2 = dbg_tensor("h2T", [128, 16 * NOWN])
            with ExitStack() as st:
                tmpf = sb(st, "dbgtmp", [128, NOWN], F32)
                for c in range(16):
                    S.op("dve", lambda c=c: V.tensor_copy(out=tmpf[:], in_=h2T[:, c, :]), writes=["dbgtmp"])
                    S.dma("sp", lambda c=c: nc.sync.dma_start(out=d2[:, c * NOWN:(c + 1) * NOWN], in_=tmpf[:]), reads=["dbgtmp"])
                S.barrier()
        if stage <= 4:
            S.finish("sp")
            return nc, dbg_out
        acc = sb(st0, "acc", [128, 8, D], F32)
        if stage == 5:
            for t in range(8):
                S.op("dve", lambda: V.memset(acc[:, t, :], 0.0), writes=[("acc", t)])
        else:
            moe_phase(nc, S, sb, psb, W, h2T, Gt, acc, bufs=moe_bufs, prefetched=True)
        S.barrier()
        stW.close()
        final_phase(nc, S, sb, W, acc, x1_d, mod_d, out_own, eps_t)
        S.finish("sp")
    return nc, dbg_out


def attention_phase(nc, S, stB, sb, psb, ps_bf, cs, identb, half_t, W, C, tv_d, hT_all, hT_own, oaT, obT, dbg, dbg_tensor):
    V, A_, P_, T_ = nc.vector, nc.scalar, nc.gpsimd, nc.tensor
    with ExitStack() as st:
        tab = sb(st, "tab", [32, 8], F32)
        ohb = sb(st, "ohb", [32, 640], F32)
        negrow = sb(st, "negrow", [1, 640], F32)
        tvs = sb(st, "tvs", [8, 640], F32)
        b31 = sb(st, "b31", [128, 8], F32)
        S.dma("sp", lambda: nc.sync.dma_start(out=tab[:], in_=W["rel_bias_table"][:, :]), writes=["tab"])
        S.dma("sp", lambda: nc.sync.dma_start(out=ohb[:], in_=C["ohb"][:, :]), writes=["ohb"])
        S.dma("sp", lambda: nc.sync.dma_start(out=negrow[:], in_=C["negrow"][:, :]), writes=["negrow"])
        S.dma("sp", lambda: nc.sync.dma_start(out=b31[:], in_=bcast_rows(W["rel_bias_table"][31:32, :])), writes=["b31"])
        for half in range(2):
            pb = psb[half]
            S.op("pe", lambda half=half, pb=pb: T_.matmul(pb[0:8, 0:320], lhsT=tab[:, :], rhs=ohb[:, half * 320:(half + 1) * 320], start=True, stop=False),
                 reads=["tab", "ohb"], writes=[("psb", half)])
            S.op("pe", lambda half=half, pb=pb: T_.matmul(pb[0:8, 0:320], lhsT=cs["ones"][0:1, 0:8], rhs=negrow[0:1, half * 320:(half + 1) * 320], start=False, stop=True),
                 reads=["c_ones", "negrow"], writes=[("psb", half)])
            S.op("act", lambda half=half, pb=pb: A_.mul(out=tvs[:, half * 320:(half + 1) * 320], in_=pb[0:8, 0:320], mul=8.0),
                 reads=[("psb", half)], writes=["tvs"])
        S.dma("sp", lambda: nc.sync.dma_start(out=tv_d.ap()[:, :], in_=tvs[:]), reads=["tvs"], writes=["tv_d"])

        lam4 = sb(st, "lam4", [128, 4, 64], F32)
        for i, nm in enumerate(("lq1", "lk1", "lq2", "lk2")):
            S.dma("sp", lambda i=i, nm=nm: nc.sync.dma_start(out=lam4[:, i, :], in_=W[nm][0:1, :].partition_broadcast(128)), writes=[("lam4", i)])
        lsum = sb(st, "lsum", [128, 2], F32)
        ljunk = sb(st, "ljunk", [128, 64], F32)
        for i in range(2):
            S.op("dve", lambda i=i: V.tensor_tensor(out=ljunk[:], in0=lam4[:, 2 * i, :], in1=lam4[:, 2 * i + 1, :], op=ALU.mult),
                 reads=[("lam4", 2 * i), ("lam4", 2 * i + 1)], writes=["ljunk"])
            S.op("dve", lambda i=i: V.tensor_reduce(out=lsum[:, i:i + 1], in_=ljunk[:], axis=AX.X, op=ALU.add),
                 reads=["ljunk"], writes=[("lsum", i)])
        S.op("act", lambda: A_.activation(out=lsum[:], in_=lsum[:], func=AF.Exp), reads=[("lsum", 0), ("lsum", 1)], writes=["lsume"])
        nlam = sb(st, "nlam", [128, 1], F32)
        S.op("dve", lambda: V.tensor_tensor(out=nlam[:], in0=lsum[:, 1:2], in1=lsum[:, 0:1], op=ALU.subtract), reads=["lsume"], writes=["nlam"])
        S.op("dve", lambda: V.tensor_scalar(out=nlam[:], in0=nlam[:], scalar1=-LAM_INIT, scalar2=None, op0=ALU.add), reads=["nlam"], writes=["nlam"])
        gs8 = sb(st, "gs8", [128, 128], F32)
        S.dma("sp", lambda: nc.sync.dma_start(out=gs8[:], in_=bcast_rows(W["g_subln"][0:1, :])), writes=["gs8"])
        S.op("dve", lambda: V.tensor_scalar(out=gs8[:], in0=gs8[:], scalar1=(1.0 - LAM_INIT), scalar2=None, op0=ALU.mult), reads=["gs8"], writes=["gs8"])
        eps_t = sb(st, "eps_t2", [128, 1], F32)
        S.op("dve", lambda: V.memset(eps_t[:], EPS), writes=["eps2"])

        tri, ones = cs["tri"], cs["ones"]
        omt = sb(st, "omt", [128, 128], F32)
        S.op("dve", lambda: V.tensor_tensor(out=omt[:], in0=ones[:], in1=tri[:], op=ALU.subtract), reads=["c_ones", "c_tri"], writes=["omt"])
        mk = {n: sb(st, "mk_" + n, [128, 128], F32) for n in ("XB", "XC", "YB", "YC")}
        omh = sb(st, "omh", [128, 1], F32)
        S.op("dve", lambda: V.tensor_scalar(out=omh[:], in0=half_t[:], scalar1=-1.0, scalar2=1.0, op0=ALU.mult, op1=ALU.add), reads=["half"], writes=["omh"])
        S.op("dve", lambda: V.scalar_tensor_tensor(out=mk["XB"][:], in0=omt[:], scalar=half_t[:, 0:1], in1=tri[:], op0=ALU.mult, op1=ALU.add),
             reads=["omt", "half", "c_tri"], writes=["mk_XB"])
        S.op("dve", lambda: V.tensor_scalar(out=mk["XC"][:], in0=tri[:], scalar1=half_t[:, 0:1], scalar2=None, op0=ALU.mult), reads=["c_tri", "half"], writes=["mk_XC"])
        S.op("dve", lambda: V.scalar_tensor_tensor(out=mk["YB"][:], in0=omt[:], scalar=omh[:, 0:1], in1=tri[:], op0=ALU.mult, op1=ALU.add),
             reads=["omt", "omh", "c_tri"], writes=["mk_YB"])
        S.op("dve", lambda: V.tensor_scalar(out=mk["YC"][:], in0=tri[:], scalar1=omh[:, 0:1], scalar2=None, op0=ALU.mult), reads=["c_tri", "omh"], writes=["mk_YC"])

        wq = [sb(st, "wq%d" % i, [128, 16, 128], BF16) for i in range(3)]
        qT = sb(st, "qT", [128, NOWN], BF16)
        kT = sb(st, "kT", [128, SEQ], BF16)
        vA = sb(st, "vA", [128, 16, 130], BF16)
        Ht = [sb(st, "Ht%d" % i, [128, 128], F32) for i in range(4)]
        slot = {n: sb(st, "slot" + n, [128, 128], F32) for n in ("XA", "XB", "XC", "YA", "YB", "YC")}
        hd = sb(st, "hd", [128, 128], F32)
        PT = [sb(st, "PT%d" % i, [128, 128], BF16) for i in range(4)]
        e_t = [sb(st, "e_t%d" % i, [128, 128], F32) for i in range(2)]
        lnp = [sb(st, "lnp%d" % i, [128, 128], F32) for i in range(2)]
        LK = [sb(st, "LK%d" % i, [128, 128], F32) for i in range(2)]
        arg = [sb(st, "arg%d" % i, [128, 128], F32) for i in range(2)]
        Acc = sb(st, "Acc", [128, 128], F32)
        o1 = sb(st, "o1", [128, 128], F32)
        o2 = sb(st, "o2", [128, 128], F32)
        obf = sb(st, "obf", [128, 128], BF16)
        rr = sb(st, "rr", [128, 4], F32)
        sjunk = sb(st, "sjunk", [128, 128], F32)
        S.op("dve", lambda: V.memset(vA[:], 1.0), writes=["vA_init"])

        w_in_r = W["w_in"].rearrange("(c p) n -> p c n", p=128)
        SC_A = 64 ** -0.5
        SC_B = 128 ** -0.5

        def own_tile_info(j):
            p = j // 2
            if j % 2 == 0:
                return 2 * p + 2, "X", (2 * p - 1, 2 * p, 2 * p + 1)
            return 16 - 2 * p, "Y", (13 - 2 * p, 14 - 2 * p, 15 - 2 * p)

        def project_head(h, colbase, with_ones):
            for i, off in enumerate((0, 1024, 2048)):
                c0 = colbase + off + h * 128
                for hh in range(2):
                    S.dma("pool", lambda i=i, c0=c0, hh=hh: nc.gpsimd.dma_start(out=wq[i][:, hh * 8:(hh + 1) * 8, :], in_=w_in_r[:, hh * 8:(hh + 1) * 8, c0:c0 + 128]),
                          writes=[("wq", i, hh)])
            n = 0
            for ch in range(2):
                pb = psb[n % 2]; n += 1
                for c in range(16):
                    S.op("pe", lambda c=c, ch=ch, pb=pb: T_.matmul(pb[:, :], lhsT=wq[0][:, c, :], rhs=hT_own[:, c, ch * 512:(ch + 1) * 512], start=(c == 0), stop=(c == 15)),
                         reads=[("wq", 0, c // 8)], writes=[("psb", (n - 1) % 2)])
                S.op("act", lambda ch=ch, pb=pb: A_.copy(out=qT[:, ch * 512:(ch + 1) * 512], in_=pb[:, :]), reads=[("psb", (n - 1) % 2)], writes=[("qT", ch)])
            for ch in range(4):
                pb = psb[n % 2]; n += 1
                for c in range(16):
                    S.op("pe", lambda c=c, ch=ch, pb=pb: T_.matmul(pb[:, :], lhsT=wq[1][:, c, :], rhs=hT_all[:, c, ch * 512:(ch + 1) * 512], start=(c == 0), stop=(c == 15)),
                         reads=[("wq", 1, c // 8)], writes=[("psb", (n - 1) % 2)])
                S.op("dve", lambda ch=ch, pb=pb: V.tensor_copy(out=kT[:, ch * 512:(ch + 1) * 512], in_=pb[:, :]), reads=[("psb", (n - 1) % 2)], writes=[("kT", ch)])
            for kb4 in range(4):
                pb = psb[n % 2]; n += 1
                for k4 in range(4):
                    kb = kb4 * 4 + k4
                    for c in range(16):
                        S.op("pe", lambda c=c, kb=kb, k4=k4, pb=pb: T_.matmul(pb[:, k4 * 128:(k4 + 1) * 128], lhsT=hT_all[:, c, kb * 128:(kb + 1) * 128], rhs=wq[2][:, c, :], start=(c == 0), stop=(c == 15)),
                             reads=[("wq", 2, c // 8)], writes=[("psb", (n - 1) % 2)])
                eng = "act" if kb4 % 2 == 0 else "dve"
                if eng == "act":
                    S.op("act", lambda kb4=kb4, pb=pb: A_.copy(out=vA[:, kb4 * 4:(kb4 + 1) * 4, 0:128], in_=pb[:, :].rearrange("p (a b) -> p a b", a=4)),
                         reads=[("psb", (n - 1) % 2), "vA_init"], writes=[("vA", kb4)])
                else:
                    S.op("dve", lambda kb4=kb4, pb=pb: V.tensor_copy(out=vA[:, kb4 * 4:(kb4 + 1) * 4, 0:128], in_=pb[:, :].rearrange("p (a b) -> p a b", a=4)),
                         reads=[("psb", (n - 1) % 2), "vA_init"], writes=[("vA", kb4)])

        def qk_keys(j, kb):
            return [("qT", j // 4), ("kT", kb // 4)]

        pend = [None]
        pendB = [None]
        for h in range(NH):
            project_head(h, 0, True)
            for i, dl in enumerate((-128, 0, 128, 256)):
                src = bass.AP(tv_d, h * 640 + 129 + dl, [[1, 128], [1, 128]])
                S.dma("sp", lambda i=i, src=src: nc.sync.dma_start(out=Ht[i][:], in_=src), reads=["tv_d"], writes=[("Ht", i)])
            for nm, lo, hi in (("XA", 2, 3), ("XB", 1, 2), ("XC", 0, 1), ("YA", 3, 2), ("YB", 2, 1), ("YC", 1, 0)):
                S.op("dve", lambda lo=lo, hi=hi: V.tensor_tensor(out=hd[:], in0=Ht[hi][:], in1=Ht[lo][:], op=ALU.subtract),
                     reads=[("Ht", lo), ("Ht", hi)], writes=["hd"])
                S.op("dve", lambda nm=nm, lo=lo: V.scalar_tensor_tensor(out=slot[nm][:], in0=hd[:], scalar=half_t[:, 0:1], in1=Ht[lo][:], op0=ALU.mult, op1=ALU.add),
                     reads=["hd", "half", ("Ht", lo)], writes=[("slot", nm)])
            git = 0
            for j in range(8):
                L, sset, slots = own_tile_info(j)
                obase = 4 if j % 2 == 0 else 0
                steps = [(m, kb) for m in range(2) for kb in range(L)]

                def da_front(i):
                    m, kb = steps[i]
                    rows = slice(64 * m, 64 * m + 64)
                    g = git + i
                    sbk = 2 + (g % 2)
                    pt = PT[g % 4]
                    sl = None
                    if kb in slots:
                        sl = sset + "ABC"[slots.index(kb)]
                    S.op("pe", lambda: T_.matmul(psb[sbk][:, 0:128], lhsT=kT[rows, kb * 128:(kb + 1) * 128], rhs=qT[rows, j * 128:(j + 1) * 128], start=True, stop=(sl is None)),
                         reads=qk_keys(j, kb), writes=[("psb", sbk)])
                    if sl is not None:
                        S.op("pe", lambda: T_.matmul(psb[sbk][:, 0:128], lhsT=cs["antiI"][:], rhs=slot[sl][:], start=False, stop=True),
                             reads=["c_antiI", ("slot", sl)], writes=[("psb", sbk)])
                    S.op("act", lambda: A_.activation(out=pt[:], in_=psb[sbk][:, 0:128], func=AF.Exp, bias=b31[:, h:h + 1], scale=SC_A),
                         reads=[("psb", sbk), "b31"], writes=[("PT", g % 4)])

                def da_back(i):
                    m, kb = steps[i]
                    g = git + i
                    ob = obase + m
                    pt = PT[g % 4]
                    S.op("pe", lambda: T_.matmul(psb[ob][:, 0:130], lhsT=pt[:], rhs=vA[:, kb, :], start=(kb == 0), stop=(kb == L - 1)),
                         reads=[("PT", g % 4), ("vA", kb // 4)], writes=[("psb", ob)])

                for i in range(len(steps)):
                    da_front(i)
                    if i >= 1:
                        da_back(i - 1)
                    if i == 2 and pendB[0] is not None:
                        pendB[0]()
                        pendB[0] = None
                    if i == min(5, len(steps) - 1) and pend[0] is not None:
                        pend[0]()
                        pend[0] = None
                da_back(len(steps) - 1)
                git += len(steps)
                o1b, o2b = obase, obase + 1
                S.op("dve", lambda: V.reciprocal(out=rr[:, 0:1], in_=psb[o1b][:, 128:129]), reads=[("psb", o1b)], writes=["rr0"])
                S.op("dve", lambda: V.reciprocal(out=rr[:, 1:2], in_=psb[o2b][:, 128:129]), reads=[("psb", o2b)], writes=["rr1"])
                S.op("dve", lambda: V.tensor_tensor(out=rr[:, 1:2], in0=rr[:, 1:2], in1=nlam[:], op=ALU.mult), reads=["rr1", "nlam"], writes=["rr1"])
                S.op("dve", lambda: V.tensor_scalar(out=o1[:], in0=psb[o1b][:, 0:128], scalar1=rr[:, 0:1], scalar2=None, op0=ALU.mult), reads=[("psb", o1b), "rr0"], writes=["o1"])
                S.op("dve", lambda: V.scalar_tensor_tensor(out=o2[:], in0=psb[o2b][:, 0:128], scalar=rr[:, 1:2], in1=o1[:], op0=ALU.mult, op1=ALU.add),
                     reads=[("psb", o2b), "rr1", "o1"], writes=["o2"])
                def ln_a():
                    S.op("act", lambda: A_.activation(out=sjunk[:], in_=o2[:], func=AF.Square, accum_out=rr[:, 2:3]), reads=["o2"], writes=["rr2", "sjunk"])
                    S.op("act", lambda: A_.activation(out=rr[:, 2:3], in_=rr[:, 2:3], func=AF.Sqrt, bias=eps_t[:], scale=1.0 / 128), reads=["rr2", "eps2"], writes=["rr2"])
                    S.op("dve", lambda: V.reciprocal(out=rr[:, 2:3], in_=rr[:, 2:3]), reads=["rr2"], writes=["rr2"])
                    S.op("dve", lambda: V.scalar_tensor_tensor(out=obf[:], in0=o2[:], scalar=rr[:, 2:3], in1=gs8[:], op0=ALU.mult, op1=ALU.mult),
                         reads=["o2", "rr2", "gs8"], writes=["obf"])
                pendB[0] = ln_a

                def tr_a(h=h, j=j):
                    ptb = ps_bf(6)
                    S.op("pe", lambda: T_.transpose(ptb[:, 0:128], obf[:], identb[:]), reads=["obf", "identb"], writes=[("psb", 6)])
                    S.op("act", lambda: A_.copy(out=oaT[:, h, j * 128:(j + 1) * 128], in_=ptb[:, 0:128]), reads=[("psb", 6)], writes=[("oaT", h)])
                pend[0] = tr_a
            pendB[0]()
            pendB[0] = None
            pend[0]()
            pend[0] = None

        for h in range(NH):
            project_head(h, 3072, False)
            git = 0
            for j in range(8):
                L, sset, slots = own_tile_info(j)
                kbs = list(range(L - 1, -1, -1))
                ab = 4 if j % 2 == 0 else 0

                def sb_mask(kb):
                    if kb in slots:
                        nm = sset + "ABC"[slots.index(kb)]
                        if nm[1] != "A":
                            return nm
                    return None

                def sb_front(i):
                    kb = kbs[i]
                    g = git + i
                    zb = 2 + (g % 2)
                    b2 = g % 2
                    mkt = sb_mask(kb)
                    S.op("pe", lambda: T_.matmul(psb[zb][:, 0:128], lhsT=kT[:, kb * 128:(kb + 1) * 128], rhs=qT[:, j * 128:(j + 1) * 128], start=True, stop=True),
                         reads=qk_keys(j, kb), writes=[("psb", zb)])
                    S.op("act", lambda: A_.activation(out=e_t[b2][:], in_=psb[zb][:, 0:128], func=AF.Exp, scale=-SC_B), reads=[("psb", zb)], writes=[("e_t", b2)])
                    S.op("act", lambda: A_.activation(out=lnp[b2][:], in_=e_t[b2][:], func=AF.Ln, bias=1.0, scale=1.0), reads=[("e_t", b2)], writes=[("lnp", b2)])
                    S.op("dve", lambda: V.scalar_tensor_tensor(out=LK[b2][:], in0=psb[zb][:, 0:128], scalar=-SC_B, in1=lnp[b2][:], op0=ALU.mult, op1=ALU.subtract),
                         reads=[("psb", zb), ("lnp", b2)], writes=[("LK", b2)])
                    if mkt is not None:
                        S.op("pool", lambda: P_.tensor_tensor(out=LK[b2][:], in0=LK[b2][:], in1=mk[mkt][:], op=ALU.mult),
                             reads=[("LK", b2), "mk_" + mkt], writes=[("LK", b2)])

                def sb_mid(i):
                    kb = kbs[i]
                    g = git + i
                    b2 = g % 2
                    lb = 5 if g % 2 == 0 else 7
                    pt = PT[g % 4]
                    first = (i == 0)
                    mkt = sb_mask(kb)
                    S.op("pe", lambda: T_.matmul(psb[lb][:, 0:128], lhsT=cs["ugt"][:], rhs=LK[b2][:], start=True, stop=first),
                         reads=["c_ugt", ("LK", b2)], writes=[("psb", lb)])
                    if not first:
                        S.op("pe", lambda: T_.matmul(psb[lb][:, 0:128], lhsT=cs["ones"][:], rhs=Acc[:], start=False, stop=True),
                             reads=["c_ones", "Acc"], writes=[("psb", lb)])
                    if kb > 0:
                        if first:
                            S.op("pool", lambda: P_.tensor_copy(out=Acc[:], in_=LK[b2][:]), reads=[("LK", b2)], writes=["Acc"])
                        else:
                            S.op("pool", lambda: P_.tensor_tensor(out=Acc[:], in0=Acc[:], in1=LK[b2][:], op=ALU.add), reads=[("LK", b2), "Acc"], writes=["Acc"])
                    S.op("dve", lambda: V.tensor_tensor(out=arg[b2][:], in0=psb[lb][:, 0:128], in1=lnp[b2][:], op=ALU.subtract),
                         reads=[("psb", lb), ("lnp", b2)], writes=[("arg", b2)])
                    S.op("act", lambda: A_.activation(out=pt[:], in_=arg[b2][:], func=AF.Exp), reads=[("arg", b2)], writes=[("PT", g % 4)])
                    if mkt is not None:
                        S.op("pool", lambda: P_.tensor_tensor(out=pt[:], in0=pt[:], in1=mk[mkt][:], op=ALU.mult),
                             reads=[("PT", g % 4), "mk_" + mkt], writes=[("PT", g % 4)])

                def sb_av(i):
                    kb = kbs[i]
                    g = git + i
                    pt = PT[g % 4]
                    S.op("pe", lambda: T_.matmul(psb[ab][:, 0:128], lhsT=pt[:], rhs=vA[:, kb, 0:128], start=(i == 0), stop=(kb == 0)),
                         reads=[("PT", g % 4), ("vA", kb // 4)], writes=[("psb", ab)])

                n_it = len(kbs)
                for i in range(n_it):
                    sb_front(i)
                    if i >= 1:
                        sb_mid(i - 1)
                    if i >= 2:
                        sb_av(i - 2)
                    if i == 1 and pend[0] is not None:
                        pend[0]()
                        pend[0] = None
                sb_mid(n_it - 1)
                if n_it >= 2:
                    sb_av(n_it - 2)
                sb_av(n_it - 1)
                git += len(kbs)
                S.op("dve", lambda: V.tensor_copy(out=obf[:], in_=psb[ab][:, 0:128]), reads=[("psb", ab)], writes=["obf"])
                def tr_b(h=h, j=j):
                    ptb = ps_bf(6)
                    S.op("pe", lambda: T_.transpose(ptb[:, 0:128], obf[:], identb[:]), reads=["obf", "identb"], writes=[("psb", 6)])
                    S.op("act", lambda: A_.copy(out=obT[:, h, j * 128:(j + 1) * 128], in_=ptb[:, 0:128]), reads=[("psb", 6)], writes=[("obT", h)])
                pend[0] = tr_b
            pend[0]()
            pend[0] = None
        if dbg:
            for nm, tt in (("oaT", oaT), ("obT", obT)):
                d = dbg_tensor(nm, [128, NH * NOWN])
                tmp = sb(st, "dtmp_" + nm, [128, NOWN], F32)
                for h in range(NH):
                    S.op("dve", lambda h=h, tt=tt, tmp=tmp: V.tensor_copy(out=tmp[:], in_=tt[:, h, :]), reads=[(nm, h)], writes=["dbg_" + nm])
                    S.dma("sp", lambda h=h, d=d, tmp=tmp: nc.sync.dma_start(out=d[:, h * NOWN:(h + 1) * NOWN], in_=tmp[:]), reads=["dbg_" + nm])


def merge_phase(nc, S, sb, psb, W, hT_own, oaT, obT, mergedT):
    V, A_, P_, T_ = nc.vector, nc.scalar, nc.gpsimd, nc.tensor
    with ExitStack() as st:
        wpa = [sb(st, "wpa%d" % i, [128, 8, 128], BF16) for i in range(2)]
        wpb = [sb(st, "wpb%d" % i, [128, 8, 128], BF16) for i in range(2)]
        wga = [sb(st, "wga%d" % i, [128, 16, 128], BF16) for i in range(2)]
        wgb = [sb(st, "wgb%d" % i, [128, 16, 128], BF16) for i in range(2)]
        sg = [[sb(st, "sg%d_%d" % (i, j), [128, 512], F32) for j in range(2)] for i in range(2)]
        tt = [[sb(st, "tt%d_%d" % (i, j), [128, 512], F32) for j in range(2)] for i in range(2)]
        wpa_r = W["w_proj_a"].rearrange("(k p) n -> p k n", p=128)
        wpb_r = W["w_proj_b"].rearrange("(k p) n -> p k n", p=128)
        w_in_r = W["w_in"].rearrange("(c p) n -> p c n", p=128)
        it = 0
        for m in range(16):
            b = m % 2
            cs_ = slice(m * 128, (m + 1) * 128)
            S.dma("pool", lambda: nc.gpsimd.dma_start(out=wpa[b][:], in_=wpa_r[:, :, cs_]), writes=[("wpa", b)])
            S.dma("pool", lambda: nc.gpsimd.dma_start(out=wpb[b][:], in_=wpb_r[:, :, cs_]), writes=[("wpb", b)])
            for hh in range(2):
                S.dma("pool", lambda: nc.gpsimd.dma_start(out=wga[b][:, hh * 8:(hh + 1) * 8, :], in_=w_in_r[:, hh * 8:(hh + 1) * 8, 6144 + m * 128:6144 + (m + 1) * 128]),
                      writes=[("wga", b, hh)])
                S.dma("pool", lambda: nc.gpsimd.dma_start(out=wgb[b][:, hh * 8:(hh + 1) * 8, :], in_=w_in_r[:, hh * 8:(hh + 1) * 8, 8192 + m * 128:8192 + (m + 1) * 128]),
                      writes=[("wgb", b, hh)])
            for th in range(2):
                base = 4 * (it % 2)
                q = it % 2
                it += 1
                tok = slice(th * 512, (th + 1) * 512)
                for k in range(8):
                    S.op("pe", lambda: T_.matmul(psb[base][:, :], lhsT=wpa[b][:, k, :], rhs=oaT[:, k, tok], start=(k == 0), stop=(k == 7)),
                         reads=[("wpa", b)], writes=[("psb", base)])
                for k in range(8):
                    S.op("pe", lambda: T_.matmul(psb[base + 1][:, :], lhsT=wpb[b][:, k, :], rhs=obT[:, k, tok], start=(k == 0), stop=(k == 7)),
                         reads=[("wpb", b)], writes=[("psb", base + 1)])
                for c in range(16):
                    S.op("pe", lambda: T_.matmul(psb[base + 2][:, :], lhsT=wga[b][:, c, :], rhs=hT_own[:, c, tok], start=(c == 0), stop=(c == 15)),
                         reads=[("wga", b, c // 8)], writes=[("psb", base + 2)])
                for c in range(16):
                    S.op("pe", lambda: T_.matmul(psb[base + 3][:, :], lhsT=wgb[b][:, c, :], rhs=hT_own[:, c, tok], start=(c == 0), stop=(c == 15)),
                         reads=[("wgb", b, c // 8)], writes=[("psb", base + 3)])
                S.op("act", lambda: A_.activation(out=sg[q][0][:], in_=psb[base + 2][:, :], func=AF.Sigmoid), reads=[("psb", base + 2)], writes=[("sg", q, 0)])
                S.op("act", lambda: A_.activation(out=sg[q][1][:], in_=psb[base + 3][:, :], func=AF.Sigmoid), reads=[("psb", base + 3)], writes=[("sg", q, 1)])
                S.op("dve", lambda: V.tensor_tensor(out=tt[q][0][:], in0=psb[base][:, :], in1=sg[q][0][:], op=ALU.mult),
                     reads=[("psb", base), ("sg", q, 0)], writes=[("tt", q, 0)])
                S.op("dve", lambda: V.tensor_tensor(out=tt[q][1][:], in0=psb[base + 1][:, :], in1=sg[q][1][:], op=ALU.mult),
                     reads=[("psb", base + 1), ("sg", q, 1)], writes=[("tt", q, 1)])
                S.op("pool", lambda: P_.tensor_tensor(out=mergedT[:, m, tok], in0=tt[q][0][:], in1=tt[q][1][:], op=ALU.add),
                     reads=[("tt", q, 0), ("tt", q, 1)], writes=[("mergedT", m, th)])
        S.barrier()


def resid_phase(nc, S, sb, psb, W, mergedT, x_own, x1_d, mod_d):
    V, A_, P_, T_ = nc.vector, nc.scalar, nc.gpsimd, nc.tensor
    with ExitStack() as st:
        wo = [sb(st, "wo%d" % i, [128, 16, 512], BF16) for i in range(2)]
        gm_bc = sb(st, "gm_bc", [128, D], F32)
        xo = [sb(st, "xo%d" % i, [128, 512], F32) for i in range(2)]
        tmp = [sb(st, "rtmp%d" % i, [128, 512], F32) for i in range(2)]
        x1c = [sb(st, "x1c%d" % i, [128, 512], F32) for i in range(2)]
        S.dma("sp", lambda: nc.sync.dma_start(out=gm_bc[:], in_=bcast_rows(mod_d[2:3, :])), writes=["gm_bc"])
        w_out_r = W["w_out"].rearrange("(c p) n -> p c n", p=128)
        it = 0
        for n in range(4):
            b = n % 2
            ns = slice(n * 512, (n + 1) * 512)
            for q in range(4):
                S.dma("pool", lambda: nc.gpsimd.dma_start(out=wo[b][:, q * 4:(q + 1) * 4, :], in_=w_out_r[:, q * 4:(q + 1) * 4, ns]), writes=[("wo", b, q)])
            for t in range(8):
                pb = it % 4
                i2 = it % 2
                it += 1
                ts_ = slice(t * 128, (t + 1) * 128)
                S.dma("sp", lambda: nc.sync.dma_start(out=xo[i2][:], in_=x_own[ts_, ns]), writes=[("xo", i2)])
                for c in range(16):
                    S.op("pe", lambda: T_.matmul(psb[pb][:, :], lhsT=mergedT[:, c, ts_], rhs=wo[b][:, c, :], start=(c == 0), stop=(c == 15)),
                         reads=[("wo", b, c // 4)], writes=[("psb", pb)])
                S.op("dve", lambda: V.tensor_tensor(out=tmp[i2][:], in0=psb[pb][:, :], in1=gm_bc[:, ns], op=ALU.mult),
                     reads=[("psb", pb), "gm_bc"], writes=[("rtmp", i2)])
                S.op("pool", lambda: P_.tensor_tensor(out=x1c[i2][:], in0=tmp[i2][:], in1=xo[i2][:], op=ALU.add),
                     reads=[("rtmp", i2), ("xo", i2)], writes=[("x1c", i2)])
                S.dma("sp", lambda: nc.sync.dma_start(out=x1_d[ts_, ns], in_=x1c[i2][:]), reads=[("x1c", i2)], writes=[("x1d", t, n)])
        S.barrier()


def norm_router_phase(nc, S, sb, psb, W, cs, x1_d, h2T, Gt, Gf_col, shf_col, eps_t):
    V, A_, P_, T_ = nc.vector, nc.scalar, nc.gpsimd, nc.tensor
    BIG = 30000.0
    with ExitStack() as st:
        x1t = [sb(st, "x1t%d" % i, [128, D], F32) for i in range(2)]
        xn = [sb(st, "xn%d" % i, [128, D], F32) for i in range(2)]
        hf = [sb(st, "hf%d" % i, [128, 16, 128], F32) for i in range(2)]
        junk = sb(st, "njunk", [128, D], BF16)
        ss = [sb(st, "nss%d" % i, [128, 1], F32) for i in range(2)]
        rs = [sb(st, "nrs%d" % i, [128, 1], F32) for i in range(2)]
        wr = sb(st, "wr", [128, 16, 72], F32)
        brt = sb(st, "brt", [128, 72], F32)
        lg = sb(st, "lg", [128, 72], F32)
        sm = {n: sb(st, "r_" + n, [128, 1], F32) for n in ("gmax", "ngmax", "gsum", "pg", "m1", "m2", "dd", "rr", "den", "p1", "c1", "c2")}
        ohg = sb(st, "ohg", [128, 8], F32)
        pen = sb(st, "pen", [128, 8], F32)
        gjunk = sb(st, "gjunk", [128, 8], F32)
        em = sb(st, "em", [128, 64], F32)
        em2 = sb(st, "em2", [128, 64], F32)
        mask1 = sb(st, "mask1", [128, 64], F32)
        mask2 = sb(st, "mask2", [128, 64], F32)
        w_rg_r = W["w_rg"].rearrange("(c p) n -> p c n", p=128)
        w_re_r = W["w_re"].rearrange("(c p) n -> p c n", p=128)
        for hh in range(2):
            S.dma("sp", lambda: nc.sync.dma_start(out=wr[:, hh * 8:(hh + 1) * 8, 0:8], in_=w_rg_r[:, hh * 8:(hh + 1) * 8, :]), writes=[("wr", 0, hh)])
            S.dma("sp", lambda: nc.sync.dma_start(out=wr[:, hh * 8:(hh + 1) * 8, 8:72], in_=w_re_r[:, hh * 8:(hh + 1) * 8, :]), writes=[("wr", 1, hh)])
        S.dma("sp", lambda: nc.sync.dma_start(out=brt[:, 0:8], in_=bcast_rows(W["b_rg"][0:1, :])), writes=[("brt", 0)])
        S.dma("sp", lambda: nc.sync.dma_start(out=brt[:, 8:72], in_=bcast_rows(W["b_re"][0:1, :])), writes=[("brt", 1)])
        wr_keys = [("wr", 0, 0), ("wr", 0, 1), ("wr", 1, 0), ("wr", 1, 1)]
        for t in range(8):
            b = t % 2
            ts_ = slice(t * 128, (t + 1) * 128)
            for hh in range(2):
                S.dma("sp", lambda: nc.sync.dma_start(out=x1t[b][:, hh * 1024:(hh + 1) * 1024], in_=x1_d[ts_, hh * 1024:(hh + 1) * 1024]), writes=[("x1t", b, hh)])
            S.op("act", lambda: A_.activation(out=junk[:], in_=x1t[b][:], func=AF.Square, accum_out=ss[b][:]),
                 reads=[("x1t", b, 0), ("x1t", b, 1)], writes=[("nss", b), "njunk"])
            S.op("act", lambda: A_.activation(out=rs[b][:], in_=ss[b][:], func=AF.Sqrt, bias=eps_t[:], scale=1.0 / D),
                 reads=[("nss", b), "eps"], writes=[("nrs", b)])
            S.op("dve", lambda: V.reciprocal(out=rs[b][:], in_=rs[b][:]), reads=[("nrs", b)], writes=[("nrs", b)])
            S.op("dve", lambda: V.tensor_scalar(out=xn[b][:], in0=x1t[b][:], scalar1=rs[b][:, 0:1], scalar2=None, op0=ALU.mult),
                 reads=[("x1t", b, 0), ("x1t", b, 1), ("nrs", b)], writes=[("xn", b)])
            for g in range(4):
                for k in range(4):
                    c = g * 4 + k
                    S.op("pe", lambda: T_.matmul(psb[g][:, k * 128:(k + 1) * 128], lhsT=xn[b][:, c * 128:(c + 1) * 128], rhs=cs["ident"][:], start=True, stop=True),
                         reads=[("xn", b), "c_ident"], writes=[("psb", g)])
                for k in range(4):
                    c = g * 4 + k
                    S.op("act", lambda: A_.activation(out=hf[b][:, c, :], in_=psb[g][:, k * 128:(k + 1) * 128], func=AF.Identity,
                                                      bias=shf_col[:, c:c + 1], scale=Gf_col[:, c:c + 1]),
                         reads=[("psb", g), "shf_col", "Gf_col"], writes=[("hf", b, g)])
            S.op("pool", lambda: P_.tensor_copy(out=h2T[:, :, ts_], in_=hf[b][:, :, :]), reads=[("hf", b, g) for g in range(4)], writes=[("h2T", t)])
            rb = 4 + b
            for c in range(16):
                S.op("pe", lambda: T_.matmul(psb[rb][:, 0:72], lhsT=hf[b][:, c, :], rhs=wr[:, c, :], start=(c == 0), stop=(c == 15)),
                     reads=[("hf", b, c // 4)] + wr_keys, writes=[("psb", rb)])
            S.op("dve", lambda: V.tensor_tensor(out=lg[:], in0=psb[rb][:, 0:72], in1=brt[:], op=ALU.add), reads=[("psb", rb), ("brt", 0), ("brt", 1)], writes=["lg"])
            S.op("dve", lambda: V.tensor_reduce(out=sm["gmax"][:], in_=lg[:, 0:8], axis=AX.X, op=ALU.max), reads=["lg"], writes=["gmax"])
            S.op("dve", lambda: V.tensor_scalar(out=ohg[:], in0=lg[:, 0:8], scalar1=sm["gmax"][:, 0:1], scalar2=None, op0=ALU.is_ge), reads=["lg", "gmax"], writes=["ohg"])
            S.op("dve", lambda: V.tensor_scalar(out=sm["ngmax"][:], in0=sm["gmax"][:], scalar1=-1.0, scalar2=None, op0=ALU.mult), reads=["gmax"], writes=["ngmax"])
            S.op("act", lambda: A_.activation(out=gjunk[:], in_=lg[:, 0:8], func=AF.Exp, bias=sm["ngmax"][:, 0:1], scale=1.0, accum_out=sm["gsum"][:]),
                 reads=["lg", "ngmax"], writes=["gsum", "gjunk"])
            S.op("dve", lambda: V.reciprocal(out=sm["pg"][:], in_=sm["gsum"][:]), reads=["gsum"], writes=["pg"])
            S.op("dve", lambda: V.tensor_scalar(out=pen[:], in0=ohg[:], scalar1=BIG, scalar2=-BIG, op0=ALU.mult, op1=ALU.add), reads=["ohg"], writes=["pen"])
            for g in range(8):
                S.op("dve", lambda: V.tensor_scalar(out=em[:, g * 8:(g + 1) * 8], in0=lg[:, 8 + g * 8:16 + g * 8], scalar1=pen[:, g:g + 1], scalar2=None, op0=ALU.add),
                     reads=["lg", "pen"], writes=[("em", g)])
            emk = [("em", g) for g in range(8)]
            S.op("dve", lambda: V.tensor_reduce(out=sm["m1"][:], in_=em[:], axis=AX.X, op=ALU.max), reads=emk, writes=["m1"])
            S.op("dve", lambda: V.tensor_scalar(out=mask1[:], in0=em[:], scalar1=sm["m1"][:, 0:1], scalar2=None, op0=ALU.is_ge), reads=emk + ["m1"], writes=["mask1"])
            S.op("dve", lambda: V.scalar_tensor_tensor(out=em2[:], in0=mask1[:], scalar=-BIG, in1=em[:], op0=ALU.mult, op1=ALU.add), reads=emk + ["mask1"], writes=["em2"])
            S.op("dve", lambda: V.tensor_reduce(out=sm["m2"][:], in_=em2[:], axis=AX.X, op=ALU.max), reads=["em2"], writes=["m2"])
            S.op("dve", lambda: V.tensor_scalar(out=mask2[:], in0=em2[:], scalar1=sm["m2"][:, 0:1], scalar2=None, op0=ALU.is_ge), reads=["em2", "m2"], writes=["mask2"])
            S.op("dve", lambda: V.tensor_tensor(out=sm["dd"][:], in0=sm["m2"][:], in1=sm["m1"][:], op=ALU.subtract), reads=["m1", "m2"], writes=["dd"])
            S.op("act", lambda: A_.activation(out=sm["rr"][:], in_=sm["dd"][:], func=AF.Exp), reads=["dd"], writes=["rr"])
            S.op("dve", lambda: V.tensor_scalar(out=sm["den"][:], in0=sm["rr"][:], scalar1=1.0, scalar2=None, op0=ALU.add), reads=["rr"], writes=["den"])
            S.op("dve", lambda: V.reciprocal(out=sm["p1"][:], in_=sm["den"][:]), reads=["den"], writes=["p1"])
            S.op("dve", lambda: V.tensor_tensor(out=sm["c1"][:], in0=sm["p1"][:], in1=sm["pg"][:], op=ALU.mult), reads=["p1", "pg"], writes=["c1"])
            S.op("dve", lambda: V.tensor_tensor(out=sm["c2"][:], in0=sm["c1"][:], in1=sm["rr"][:], op=ALU.mult), reads=["c1", "rr"], writes=["c2"])
            S.op("dve", lambda: V.tensor_scalar(out=Gt[:, t, :], in0=mask1[:], scalar1=sm["c1"][:, 0:1], scalar2=None, op0=ALU.mult), reads=["mask1", "c1"], writes=[("Gt", t)])
            S.op("dve", lambda: V.scalar_tensor_tensor(out=Gt[:, t, :], in0=mask2[:], scalar=sm["c2"][:, 0:1], in1=Gt[:, t, :], op0=ALU.mult, op1=ALU.add),
                 reads=["mask2", "c2", ("Gt", t)], writes=[("Gt", t)])
        S.barrier()


def moe_weight_bufs(nc, stack, side=None):
    kw = {"side": side} if side else {}
    wg = [stack.enter_context(nc.sbuf_tensor("wg%d" % i, [128, 16, 128], BF16, **kw)) for i in range(MOE_NWB)]
    wu = [stack.enter_context(nc.sbuf_tensor("wu%d" % i, [128, 16, 128], BF16, **kw)) for i in range(MOE_NWB)]
    wd = stack.enter_context(nc.sbuf_tensor("wd", [128, 8, D], BF16, **kw))
    return wg, wu, wd


def moe_issue_unit(nc, S, W, wg, wu, u):
    e, f = u // 8, u % 8
    wb = u % MOE_NWB
    rows_g = W["w_eg"][e * D:(e + 1) * D, :].rearrange("(c p) n -> p c n", p=128)
    rows_u = W["w_eu"][e * D:(e + 1) * D, :].rearrange("(c p) n -> p c n", p=128)
    fs = slice(f * 128, (f + 1) * 128)
    for hh in range(2):
        S.dma("pool", lambda: nc.gpsimd.dma_start(out=wg[wb][:, hh * 8:(hh + 1) * 8, :], in_=rows_g[:, hh * 8:(hh + 1) * 8, fs]), writes=[("wg", wb, hh)])
    for hh in range(2):
        S.dma("pool", lambda: nc.gpsimd.dma_start(out=wu[wb][:, hh * 8:(hh + 1) * 8, :], in_=rows_u[:, hh * 8:(hh + 1) * 8, fs]), writes=[("wu", wb, hh)])


def moe_issue_wd(nc, S, W, wd, e):
    rows_d = W["w_ed"][e * 1024:(e + 1) * 1024, :].rearrange("(f p) n -> p f n", p=128)
    for f2 in range(8):
        S.dma("pool", lambda: nc.gpsimd.dma_start(out=wd[:, f2, :], in_=rows_d[:, f2, :]), writes=[("wd", f2)])


def moe_prefetch(nc, S, W, wg, wu, wd):
    for u in range(MOE_NWB - 1):
        moe_issue_unit(nc, S, W, wg, wu, u)
    moe_issue_wd(nc, S, W, wd, 0)


def moe_phase(nc, S, sb, psb, W, h2T, Gt, acc, NE=64, bufs=None, prefetched=False):
    V, A_, P_, T_ = nc.vector, nc.scalar, nc.gpsimd, nc.tensor
    NWB = MOE_NWB
    NU = NE * 8
    with ExitStack() as st:
        if bufs is None:
            wg, wu, wd = moe_weight_bufs(nc, st)
        else:
            wg, wu, wd = bufs
        actT = sb(st, "actT", [128, 8, NOWN], BF16)
        sl = [sb(st, "sl%d" % i, [128, 512], F32) for i in range(2)]
        SBUF_LOG.append(("moe", nc.sbuf_bytes_remaining))
        for t in range(8):
            S.op("dve", lambda: V.memset(acc[:, t, :], 0.0), writes=[("acc", t, n) for n in range(4)])

        def issue_unit(u):
            moe_issue_unit(nc, S, W, wg, wu, u)

        def issue_wd(e):
            moe_issue_wd(nc, S, W, wd, e)

        if not prefetched:
            moe_prefetch(nc, S, W, wg, wu, wd)
        it = 0
        dn = 0
        for u in range(NU):
            e, f = u // 8, u % 8
            wb = u % NWB
            if u + NWB - 1 < NU:
                issue_unit(u + NWB - 1)
            for half in range(2):
                ab = it % 2
                it += 1
                tok = slice(half * 512, (half + 1) * 512)
                for c in range(16):
                    S.op("pe", lambda: T_.matmul(psb[ab][:, :], lhsT=wg[wb][:, c, :], rhs=h2T[:, c, tok], start=(c == 0), stop=(c == 15)),
                         reads=[("wg", wb, c // 8)], writes=[("psb", ab)])
                for c in range(16):
                    S.op("pe", lambda: T_.matmul(psb[2 + ab][:, :], lhsT=wu[wb][:, c, :], rhs=h2T[:, c, tok], start=(c == 0), stop=(c == 15)),
                         reads=[("wu", wb, c // 8)], writes=[("psb", 2 + ab)])
                S.op("act", lambda: A_.activation(out=sl[ab][:], in_=psb[ab][:, :], func=AF.Silu), reads=[("psb", ab)], writes=[("sl", ab)])
                S.op("dve", lambda: V.tensor_tensor(out=actT[:, f, tok], in0=psb[2 + ab][:, :], in1=sl[ab][:], op=ALU.mult),
                     reads=[("psb", 2 + ab), ("sl", ab)], writes=[("actT", f, half)])
            if f == 7:
                for t in range(8):
                    ts_ = slice(t * 128, (t + 1) * 128)
                    for n in range(4):
                        db = 4 + dn % 4
                        dn += 1
                        ns = slice(n * 512, (n + 1) * 512)
                        for f2 in range(8):
                            S.op("pe", lambda: T_.matmul(psb[db][:, :], lhsT=actT[:, f2, ts_], rhs=wd[:, f2, ns], start=(f2 == 0), stop=(f2 == 7)),
                                 reads=[("actT", f2, t // 4), ("wd", f2)], writes=[("psb", db)])
                        S.op("dve", lambda: V.scalar_tensor_tensor(out=acc[:, t, ns], in0=psb[db][:, :], scalar=Gt[:, t, e:e + 1], in1=acc[:, t, ns], op0=ALU.mult, op1=ALU.add),
                             reads=[("psb", db), ("acc", t, n)], writes=[("acc", t, n)])
                if e + 1 < NE:
                    issue_wd(e + 1)
        S.barrier()


def final_phase(nc, S, sb, W, acc, x1_d, mod_d, out_own, eps_t):
    V, A_, P_ = nc.vector, nc.scalar, nc.gpsimd
    with ExitStack() as st:
        gf_bc = sb(st, "gf_bc", [128, D], F32)
        gfin_bc = sb(st, "gfin_bc", [128, D], F32)
        x1t = [sb(st, "fx1t%d" % i, [128, D], F32) for i in range(2)]
        ot = [sb(st, "fot%d" % i, [128, D], F32) for i in range(2)]
        junk = sb(st, "fjunk", [128, D], BF16)
        ss = [sb(st, "fss%d" % i, [128, 1], F32) for i in range(2)]
        rs = [sb(st, "frs%d" % i, [128, 1], F32) for i in range(2)]
        S.dma("sp", lambda: nc.sync.dma_start(out=gf_bc[:], in_=bcast_rows(mod_d[5:6, :])), writes=["gf_bc"])
        S.dma("sp", lambda: nc.sync.dma_start(out=gfin_bc[:], in_=bcast_rows(W["g_final"][0:1, :])), writes=["gfin_bc"])
        for t in range(8):
            b = t % 2
            ts_ = slice(t * 128, (t + 1) * 128)
            for hh in range(2):
                S.dma("sp", lambda: nc.sync.dma_start(out=x1t[b][:, hh * 1024:(hh + 1) * 1024], in_=x1_d[ts_, hh * 1024:(hh + 1) * 1024]), writes=[("fx1t", b, hh)])
            S.op("dve", lambda: V.tensor_tensor(out=ot[b][:], in0=acc[:, t, :], in1=gf_bc[:], op=ALU.mult), reads=["gf_bc"], writes=[("fot", b)])
            S.op("pool", lambda: P_.tensor_tensor(out=ot[b][:], in0=ot[b][:], in1=x1t[b][:], op=ALU.add),
                 reads=[("fot", b), ("fx1t", b, 0), ("fx1t", b, 1)], writes=[("fot", b)])
            S.op("act", lambda: A_.activation(out=junk[:], in_=ot[b][:], func=AF.Square, accum_out=ss[b][:]), reads=[("fot", b)], writes=[("fss", b), "fjunk"])
            S.op("act", lambda: A_.activation(out=rs[b][:], in_=ss[b][:], func=AF.Sqrt, bias=eps_t[:], scale=1.0 / D), reads=[("fss", b), "eps"], writes=[("frs", b)])
            S.op("dve", lambda: V.reciprocal(out=rs[b][:], in_=rs[b][:]), reads=[("frs", b)], writes=[("frs", b)])
            S.op("dve", lambda: V.scalar_tensor_tensor(out=ot[b][:], in0=ot[b][:], scalar=rs[b][:, 0:1], in1=gfin_bc[:], op0=ALU.mult, op1=ALU.mult),
                 reads=[("fot", b), ("frs", b), "gfin_bc"], writes=[("fot", b)])
            for hh in range(2):
                S.dma("sp", lambda: nc.sync.dma_start(out=out_own[ts_, hh * 1024:(hh + 1) * 1024], in_=ot[b][:, hh * 1024:(hh + 1) * 1024]), reads=[("fot", b)], writes=[("out", t, hh)])


def own_qblocks(half):
    qb = []
    for p in range(4):
        qb.append(2 * p + half)
        qb.append(15 - 2 * p - half)
    return qb


def own_token_index(half):
    return np.concatenate([np.arange(q * 128, (q + 1) * 128) for q in own_qblocks(half)])


def col_layout(v):
    return np.ascontiguousarray(np.asarray(v, np.float32).reshape(-1, 128).T)


def make_shared(inp, stage):
    f = lambda a: np.ascontiguousarray(np.asarray(a, np.float32))
    sh = {
        "rel_bias_table": f(inp["rel_bias_table"]), "w_ada": f(inp["w_ada"][0]), "b_ada": f(inp["b_ada"][0]).reshape(1, -1),
        "g_mix_col": col_layout(inp["g_mix"][0]), "w_in": f(inp["w_in"][0]),
        "lq1": f(inp["lambda_q1"][0]).reshape(1, -1), "lk1": f(inp["lambda_k1"][0]).reshape(1, -1),
        "lq2": f(inp["lambda_q2"][0]).reshape(1, -1), "lk2": f(inp["lambda_k2"][0]).reshape(1, -1),
        "g_subln": f(inp["g_subln"][0]).reshape(1, -1), "w_proj_a": f(inp["w_proj_a"][0]), "w_proj_b": f(inp["w_proj_b"][0]),
        "w_out": f(inp["w_out"][0]), "g_ffn_col": col_layout(inp["g_ffn"][0]),
        "w_rg": f(inp["w_router_group"][0]), "b_rg": f(inp["b_router_group"][0]).reshape(1, -1),
        "w_re": f(inp["w_router_expert"][0]), "b_re": f(inp["b_router_expert"][0]).reshape(1, -1),
        "g_final": f(inp["g_final"]).reshape(1, -1),
    }
    if stage >= 7:
        sh["w_eg"] = f(inp["w_expert_gate"][0]).reshape(64 * D, 1024)
        sh["w_eu"] = f(inp["w_expert_up"][0]).reshape(64 * D, 1024)
        sh["w_ed"] = f(inp["w_expert_down"][0]).reshape(64 * 1024, D)
    for k, v in host_consts().items():
        sh["c_" + k] = v
    return sh


def make_core_map(inp, shared, core):
    b, half = core // 2, core % 2
    xb = np.asarray(inp["x"][b], np.float32)
    m = dict(shared)
    m["x_all"] = np.ascontiguousarray(xb)
    m["x_own"] = np.ascontiguousarray(xb[own_token_index(half)])
    m["c_col"] = col_layout(inp["c"][b])
    m["halfv"] = np.full((128, 1), float(half), np.float32)
    return m


def kernel(**inputs):
    nc, _ = build(stage=99, dbg=False)
    shared = make_shared(inputs, 99)
    in_maps = [make_core_map(inputs, shared, c) for c in range(8)]
    res = run_bass_kernel_spmd(nc, in_maps, core_ids=list(range(8)))
    out = np.zeros((4, SEQ, D), np.float32)
    for c in range(8):
        out[c // 2, own_token_index(c % 2)] = res.results[c]["out_own"]
    return out
```

```python
import math
from contextlib import ExitStack

import numpy as np
import concourse.bass as bass
import concourse.mybir as mybir
from concourse.bass_utils import run_bass_kernel_spmd

F32 = mybir.dt.float32
BF16 = mybir.dt.bfloat16
I32 = mybir.dt.int32
U32 = mybir.dt.uint32
AF = mybir.ActivationFunctionType
ALU = mybir.AluOpType
AX = mybir.AxisListType

D = 2048
SEQ = 2048
NOWN = 1024
NH = 8
NBLK = 80
EPS = 1e-6
LAM_INIT = 0.8 - 0.6 * math.exp(0.0)
NEG = -30000.0
SBUF_LOG = []
MOE_NWB = 4


class Sched:
    def __init__(self, nc, stack, n_dma_sems=32):
        self.nc = nc
        self.eng = {"pe": nc.tensor, "act": nc.scalar, "dve": nc.vector,
                    "pool": nc.gpsimd, "sp": nc.sync}
        self.sem = {e: stack.enter_context(nc.semaphore("s_" + e)) for e in self.eng}
        self.cnt = {e: 0 for e in self.eng}
        self.dsem = [stack.enter_context(nc.semaphore("d%d" % i)) for i in range(n_dma_sems)]
        self.dcnt = [0] * n_dma_sems
        self.dnext = 0
        self.seen = {e: {} for e in self.eng}
        self.lastw = {}
        self.reads = {}
        self.semobj = {}
        for e, s in self.sem.items():
            self.semobj[("e", e)] = s
        for i, s in enumerate(self.dsem):
            self.semobj[("d", i)] = s

    def _wait(self, e, ev):
        sid, val, src = ev
        if src == "pe" and e == "pe":
            return
        if self.seen[e].get(sid, 0) >= val:
            return
        self.seen[e][sid] = val
        self.eng[e].wait_ge(self.semobj[sid], val)

    def _deps(self, e, reads, writes, extra):
        evs = []
        for k in reads:
            if k in self.lastw:
                evs.append(self.lastw[k])
        for k in writes:
            if k in self.lastw:
                evs.append(self.lastw[k])
            evs.extend(self.reads.get(k, []))
        evs.extend(extra)
        best = {}
        for ev in evs:
            sid, val, src = ev
            if src == "pe" and e == "pe":
                continue
            if sid not in best or best[sid][1] < val:
                best[sid] = ev
        for ev in best.values():
            self._wait(e, ev)

    def _record(self, ev, reads, writes):
        for k in reads:
            lst = self.reads.setdefault(k, [])
            lst.append(ev)
        for k in writes:
            self.lastw[k] = ev
            self.reads[k] = []

    def op(self, e, fn, reads=(), writes=(), extra=()):
        self._deps(e, reads, writes, extra)
        ins = fn()
        self.cnt[e] += 1
        ins.then_inc(self.sem[e], 1)
        ev = (("e", e), self.cnt[e], e)
        self._record(ev, reads, writes)
        return ev

    def dma(self, q, fn, reads=(), writes=(), extra=()):
        i = self.dnext
        self.dnext = (self.dnext + 1) % len(self.dsem)
        sid = ("d", i)
        if self.dcnt[i] > 0:
            self._wait(q, (sid, self.dcnt[i], "dma"))
        self._deps(q, reads, writes, extra)
        ins = fn()
        self.dcnt[i] += 16
        ins.then_inc(self.dsem[i], 16)
        ev = (sid, self.dcnt[i], "dma")
        self._record(ev, reads, writes)
        return ev

    def all_events(self):
        evs = []
        for i, c in enumerate(self.dcnt):
            if c:
                evs.append((("d", i), c, "dma"))
        for en in self.eng:
            if self.cnt[en]:
                evs.append((("e", en), self.cnt[en], "x"))
        return evs

    def barrier(self):
        evs = self.all_events()
        for e in self.eng:
            for ev in evs:
                self._wait(e, ev)
        self.lastw = {}
        self.reads = {}

    def finish(self, e="sp"):
        for ev in self.all_events():
            self._wait(e, ev)


def bcast_rows(ap, nparts=128):
    n = ap.shape[-1]
    return bass.AP(ap.tensor, ap.offset, [[0, nparts], [1, n]])


def rel_bucket_np(n):
    n = np.maximum(n, 0)
    nf = np.maximum(n, 1).astype(np.float32)
    large = 16 + (np.log(nf / np.float32(16)) / np.float32(math.log(128 / 16)) * np.float32(16)).astype(np.int32)
    large = np.minimum(large, 31)
    return np.where(n < 16, n, large)


def host_consts():
    i = np.arange(128)
    c = {}
    c["ident"] = np.eye(128, dtype=np.float32)
    c["antiI"] = np.eye(128, dtype=np.float32)[::-1].copy()
    c["tri"] = (i[:, None] < i[None, :]).astype(np.float32)
    c["ugt"] = (i[:, None] > i[None, :]).astype(np.float32)
    c["ones"] = np.ones((128, 128), np.float32)
    n = np.arange(640) - 256
    oh = np.zeros((32, 640), np.float32)
    valid = (n >= 0) & (n < 256)
    bk = rel_bucket_np(np.clip(n, 0, None))
    oh[bk[valid], np.nonzero(valid)[0]] = 1.0
    oh[31, valid] -= 1.0
    c["ohb"] = oh
    c["negrow"] = np.where(n < 0, NEG, 0.0).astype(np.float32)[None, :]
    c["iota128"] = np.tile(np.arange(128, dtype=np.float32)[None, :], (128, 1))
    c["thr"] = np.tile((128.0 * np.arange(16, dtype=np.float32))[None, :], (128, 1))
    c["pcol"] = np.arange(128, dtype=np.float32)[:, None].copy()
    return c


CONST_SHAPES = {"ident": [128, 128], "antiI": [128, 128], "tri": [128, 128], "ugt": [128, 128],
                "ones": [128, 128], "ohb": [32, 640], "negrow": [1, 640], "iota128": [128, 128],
                "thr": [128, 16], "pcol": [128, 1]}

WEIGHT_SHAPES = {
    "rel_bias_table": [32, 8], "w_ada": [D, 6 * D], "b_ada": [1, 6 * D], "g_mix_col": [128, 16],
    "w_in": [D, 10240], "lq1": [1, 64], "lk1": [1, 64], "lq2": [1, 64], "lk2": [1, 64],
    "g_subln": [1, 128], "w_proj_a": [1024, D], "w_proj_b": [1024, D], "w_out": [D, D],
    "g_ffn_col": [128, 16], "w_rg": [D, 8], "b_rg": [1, 8], "w_re": [D, 64], "b_re": [1, 64],
    "w_eg": [64 * D, 1024], "w_eu": [64 * D, 1024], "w_ed": [64 * 1024, D], "g_final": [1, D],
}


def build(stage=99, dbg=False):
    nc = bass.Bass("TRN2", target_bir_lowering=False)
    din = {}

    def dram_in(name, shape, dt=F32):
        din[name] = nc.dram_tensor(name, list(shape), dt, kind="ExternalInput").ap()
        return din[name]

    x_all = dram_in("x_all", [SEQ, D])
    x_own = dram_in("x_own", [NOWN, D])
    c_col = dram_in("c_col", [128, 16])
    halfv = dram_in("halfv", [128, 1])
    W = {k: dram_in(k, s) for k, s in WEIGHT_SHAPES.items() if stage >= 7 or k not in ("w_eg", "w_eu", "w_ed")}
    C = {k: dram_in("c_" + k, s) for k, s in CONST_SHAPES.items()}
    out_own = nc.dram_tensor("out_own", [NOWN, D], F32, kind="ExternalOutput").ap()
    dbg_out = {}

    def dbg_tensor(name, shape):
        dbg_out[name] = nc.dram_tensor("dbg_" + name, list(shape), F32, kind="ExternalOutput").ap()
        return dbg_out[name]

    tv_d = nc.dram_tensor("tv_scratch", [8, 640], F32, kind="Internal")
    mod_d = nc.dram_tensor("mod_scratch", [6, D], F32, kind="Internal").ap()
    x1_d = nc.dram_tensor("dbg_x1" if dbg else "x1_scratch", [NOWN, D], F32, kind="ExternalOutput" if dbg else "Internal").ap()

    with ExitStack() as st0:
        S = Sched(nc, st0)
        V, A_, P_, T_ = nc.vector, nc.scalar, nc.gpsimd, nc.tensor

        def sb(stack, name, shape, dt):
            return stack.enter_context(nc.sbuf_tensor(name, list(shape), dt))

        psb = [st0.enter_context(nc.psum_tensor("psb%d" % i, [128, 512], F32)) for i in range(8)]

        def ps_bf(i):
            return psb[i][:].bitcast(BF16)

        cs = {}
        for k in ("ident", "antiI", "tri", "ugt", "ones"):
            cs[k] = sb(st0, "k_" + k, [128, 128], F32)
            S.dma("sp", lambda k=k: nc.sync.dma_start(out=cs[k][:], in_=C[k][:, :]), writes=["c_" + k])
        identb = sb(st0, "identb", [128, 128], BF16)
        S.op("dve", lambda: V.tensor_copy(out=identb[:], in_=cs["ident"][:]), reads=["c_ident"], writes=["identb"])
        half_t = sb(st0, "half_t", [128, 1], F32)
        S.dma("sp", lambda: nc.sync.dma_start(out=half_t[:], in_=halfv[:, :]), writes=["half"])

        Gm_col = sb(st0, "Gm_col", [128, 16], F32)
        shm_col = sb(st0, "shm_col", [128, 16], F32)
        Gf_col = sb(st0, "Gf_col", [128, 16], F32)
        shf_col = sb(st0, "shf_col", [128, 16], F32)

        with ExitStack() as st:
            ccol = sb(st, "ccol", [128, 16], F32)
            scol = sb(st, "scol", [128, 16], F32)
            gmix = sb(st, "gmix", [128, 16], F32)
            S.dma("sp", lambda: nc.sync.dma_start(out=ccol[:], in_=c_col[:, :]), writes=["ccol"])
            S.dma("sp", lambda: nc.sync.dma_start(out=gmix[:], in_=W["g_mix_col"][:, :]), writes=["gmix"])
            gffn = sb(st, "gffn", [128, 16], F32)
            S.dma("sp", lambda: nc.sync.dma_start(out=gffn[:], in_=W["g_ffn_col"][:, :]), writes=["gffn"])
            S.op("act", lambda: A_.activation(out=scol[:], in_=ccol[:], func=AF.Silu), reads=["ccol"], writes=["scol"])
            wa = [sb(st, "wa%d" % i, [128, 16, 512], F32) for i in range(2)]
            brow = [sb(st, "brow%d" % i, [1, 512], F32) for i in range(2)]
            mrow = [sb(st, "mrow%d" % i, [1, 512], F32) for i in range(2)]
            one11 = sb(st, "one11", [1, 1], F32)
            S.op("dve", lambda: V.memset(one11[:], 1.0), writes=["one11"])
            w_ada_r = W["w_ada"].rearrange("(c p) n -> p c n", p=128)
            for j in range(24):
                m, jj = j // 4, j % 4
                b = j % 2
                for hh in range(2):
                    S.dma("sp", lambda b=b, j=j, hh=hh: nc.sync.dma_start(
                        out=wa[b][:, hh * 8:(hh + 1) * 8, :], in_=w_ada_r[:, hh * 8:(hh + 1) * 8, j * 512:(j + 1) * 512]),
                        writes=[("wa", b, hh)])
                S.dma("sp", lambda b=b, j=j: nc.sync.dma_start(out=brow[b][:], in_=W["b_ada"][0:1, j * 512:(j + 1) * 512]), writes=[("brow", b)])
                pb = psb[b]
                for c in range(16):
                    S.op("pe", lambda c=c, b=b, pb=pb: T_.matmul(pb[0:1, :], lhsT=scol[:, c:c + 1], rhs=wa[b][:, c, :], start=(c == 0), stop=(c == 15)),
                         reads=["scol", ("wa", b, c // 8)], writes=[("psb", b)])
                S.op("dve", lambda b=b, pb=pb: V.tensor_tensor(out=mrow[b][:], in0=pb[0:1, :], in1=brow[b][:], op=ALU.add),
                     reads=[("psb", b), ("brow", b)], writes=[("mrow", b)])
                if m in (0, 1, 3, 4):
                    pc = psb[2 + b]
                    for q in range(4):
                        S.op("pe", lambda q=q, b=b, pc=pc: T_.matmul(pc[:, q:q + 1], lhsT=mrow[b][0:1, q * 128:(q + 1) * 128], rhs=one11[0:1, 0:1], start=True, stop=True),
                             reads=[("mrow", b), "one11"], writes=[("psb", 2 + b)])
                    dst = {0: shm_col, 1: Gm_col, 3: shf_col, 4: Gf_col}[m]
                    key = {0: "shm_col", 1: "Gm_col", 3: "shf_col", 4: "Gf_col"}[m]
                    S.op("dve", lambda b=b, pc=pc, dst=dst, jj=jj: V.tensor_copy(out=dst[:, jj * 4:(jj + 1) * 4], in_=pc[:, 0:4]),
                         reads=[("psb", 2 + b)], writes=[key])
                S.dma("sp", lambda b=b, m=m, jj=jj: nc.sync.dma_start(out=mod_d[m:m + 1, jj * 512:(jj + 1) * 512], in_=mrow[b][:]),
                      reads=[("mrow", b)], writes=[("mod_d", j)])
            S.op("dve", lambda: V.scalar_tensor_tensor(out=Gm_col[:], in0=Gm_col[:], scalar=1.0, in1=gmix[:], op0=ALU.add, op1=ALU.mult),
                 reads=["Gm_col", "gmix"], writes=["Gm_col"])
            S.op("dve", lambda: V.scalar_tensor_tensor(out=Gf_col[:], in0=Gf_col[:], scalar=1.0, in1=gffn[:], op0=ALU.add, op1=ALU.mult),
                 reads=["Gf_col", "gffn"], writes=["Gf_col"])
            if dbg:
                d = dbg_tensor("Gm_col", [128, 16]); S.dma("sp", lambda: nc.sync.dma_start(out=d[:, :], in_=Gm_col[:]), reads=["Gm_col"])
                d2 = dbg_tensor("shm_col", [128, 16]); S.dma("sp", lambda: nc.sync.dma_start(out=d2[:, :], in_=shm_col[:]), reads=["shm_col"])
            S.barrier()
        if stage <= 0:
            S.finish("sp")
            return nc, dbg_out

        stR = ExitStack()
        stA = ExitStack()
        eps_t = sb(st0, "eps_t", [128, 1], F32)
        S.op("dve", lambda: V.memset(eps_t[:], EPS), writes=["eps"])
        hT_own = sb(stA, "hT_own", [128, 16, NOWN], BF16)
        oaT = sb(stA, "oaT", [128, NH, NOWN], BF16)
        obT = sb(stA, "obT", [128, NH, NOWN], BF16)

        def norm_tile_to_hT(stk_bufs, src_ap, dstT, col0, tag):
            xt, xb, ss, rs, junk = stk_bufs
            for hh in range(2):
                S.dma("sp", lambda hh=hh: nc.sync.dma_start(out=xt[:, hh * 1024:(hh + 1) * 1024], in_=src_ap[:, hh * 1024:(hh + 1) * 1024]), writes=[(tag, "xt", hh)])
            S.op("act", lambda: A_.activation(out=junk[:], in_=xt[:], func=AF.Square, accum_out=ss[:]),
                 reads=[(tag, "xt", 0), (tag, "xt", 1)], writes=[(tag, "ss"), (tag, "junk")])
            S.op("act", lambda: A_.activation(out=rs[:], in_=ss[:], func=AF.Sqrt, bias=eps_t[:], scale=1.0 / D),
                 reads=[(tag, "ss"), "eps"], writes=[(tag, "rs")])
            S.op("dve", lambda: V.reciprocal(out=rs[:], in_=rs[:]), reads=[(tag, "rs")], writes=[(tag, "rs")])
            S.op("dve", lambda: V.tensor_scalar(out=xb[:], in0=xt[:], scalar1=rs[:, 0:1], scalar2=None, op0=ALU.mult),
                 reads=[(tag, "xt", 0), (tag, "xt", 1), (tag, "rs")], writes=[(tag, "xb")])
            for g in range(2):
                pt = ps_bf(6 + g)
                for c8 in range(8):
                    c = g * 8 + c8
                    S.op("pe", lambda c=c, c8=c8, pt=pt: T_.transpose(pt[:, c8 * 128:(c8 + 1) * 128], xb[:, c * 128:(c + 1) * 128], identb[:]),
                         reads=[(tag, "xb"), "identb"], writes=[("psb", 6 + g)])
                for c8 in range(8):
                    c = g * 8 + c8
                    S.op("act", lambda c=c, c8=c8, pt=pt: A_.activation(out=dstT[:, c, col0:col0 + 128], in_=pt[:, c8 * 128:(c8 + 1) * 128],
                                                                      func=AF.Identity, bias=shm_col[:, c:c + 1], scale=Gm_col[:, c:c + 1]),
                         reads=[("psb", 6 + g), "shm_col", "Gm_col"], writes=[(tag, "hT", col0 // 512)])

        with ExitStack() as stB:
            hT_all = sb(stB, "hT_all", [128, 16, SEQ], BF16)
            with ExitStack() as st:
                bufs = []
                for i in range(2):
                    bufs.append((sb(st, "xt%d" % i, [128, D], F32), sb(st, "xb%d" % i, [128, D], BF16),
                                 sb(st, "ss%d" % i, [128, 1], F32), sb(st, "rs%d" % i, [128, 1], F32),
                                 sb(st, "junk%d" % i, [128, D], BF16)))
                for t in range(16):
                    norm_tile_to_hT(bufs[t % 2], x_all[t * 128:(t + 1) * 128, :], hT_all, t * 128, ("n", t % 2))
                S.barrier()
                for t in range(8):
                    norm_tile_to_hT(bufs[t % 2], x_own[t * 128:(t + 1) * 128, :], hT_own, t * 128, ("n", t % 2))
                S.barrier()
            attention_phase(nc, S, stB, sb, psb, ps_bf, cs, identb, half_t, W, C, tv_d, hT_all, hT_own, oaT, obT, False, dbg_tensor)
            S.barrier()
        mergedT = stR.enter_context(nc.sbuf_tensor("mergedT", [128, 16, NOWN], BF16, side="right"))
        merge_phase(nc, S, sb, psb, W, hT_own, oaT, obT, mergedT)
        S.barrier()
        stA.close()
        h2T = sb(st0, "h2T", [128, 16, NOWN], BF16)
        Gt = sb(st0, "Gt", [128, 8, 64], F32)
        resid_phase(nc, S, sb, psb, W, mergedT, x_own, x1_d, mod_d)
        S.barrier()
        stR.close()
        stW = ExitStack()
        moe_bufs = None
        if stage > 5:
            moe_bufs = moe_weight_bufs(nc, stW, side="right")
            moe_prefetch(nc, S, W, *moe_bufs)
        norm_router_phase(nc, S, sb, psb, W, cs, x1_d, h2T, Gt, Gf_col, shf_col, eps_t)
        S.barrier()
        if dbg:
            d = dbg_tensor("Gt", [128, 8 * 64])
            S.dma("sp", lambda: nc.sync.dma_start(out=d[:, :], in_=Gt[:].rearrange("p a b -> p (a b)")))
            d2 = dbg_tensor("h2T", [128, 16 * NOWN])
            with ExitStack() as st:
                tmpf = sb(st, "dbgtmp", [128, NOWN], F32)
                for c in range(16):
                    S.op("dve", lambda c=c: V.tensor_copy(out=tmpf[:], in_=h2T[:, c, :]), writes=["dbgtmp"])
                    S.dma("sp", lambda c=c: nc.sync.dma_start(out=d2[:, c * NOWN:(c + 1) * NOWN], in_=tmpf[:]), reads=["dbgtmp"])
                S.barrier()
        if stage <= 4:
            S.finish("sp")
            return nc, dbg_out
        acc = sb(st0, "acc", [128, 8, D], F32)
        if stage == 5:
            for t in range(8):
                S.op("dve", lambda: V.memset(acc[:, t, :], 0.0), writes=[("acc", t)])
        else:
            moe_phase(nc, S, sb, psb, W, h2T, Gt, acc, bufs=moe_bufs, prefetched=True)
        S.barrier()
        stW.close()
        final_phase(nc, S, sb, W, acc, x1_d, mod_d, out_own, eps_t)
        S.finish("sp")
    return nc, dbg_out


def attention_phase(nc, S, stB, sb, psb, ps_bf, cs, identb, half_t, W, C, tv_d, hT_all, hT_own, oaT, obT, dbg, dbg_tensor):
    V, A_, P_, T_ = nc.vector, nc.scalar, nc.gpsimd, nc.tensor
    with ExitStack() as st:
        tab = sb(st, "tab", [32, 8], F32)
        ohb = sb(st, "ohb", [32, 640], F32)
        negrow = sb(st, "negrow", [1, 640], F32)
        tvs = sb(st, "tvs", [8, 640], F32)
        b31 = sb(st, "b31", [128, 8], F32)
        S.dma("sp", lambda: nc.sync.dma_start(out=tab[:], in_=W["rel_bias_table"][:, :]), writes=["tab"])
        S.dma("sp", lambda: nc.sync.dma_start(out=ohb[:], in_=C["ohb"][:, :]), writes=["ohb"])
        S.dma("sp", lambda: nc.sync.dma_start(out=negrow[:], in_=C["negrow"][:, :]), writes=["negrow"])
        S.dma("sp", lambda: nc.sync.dma_start(out=b31[:], in_=bcast_rows(W["rel_bias_table"][31:32, :])), writes=["b31"])
        for half in range(2):
            pb = psb[half]
            S.op("pe", lambda half=half, pb=pb: T_.matmul(pb[0:8, 0:320], lhsT=tab[:, :], rhs=ohb[:, half * 320:(half + 1) * 320], start=True, stop=False),
                 reads=["tab", "ohb"], writes=[("psb", half)])
            S.op("pe", lambda half=half, pb=pb: T_.matmul(pb[0:8, 0:320], lhsT=cs["ones"][0:1, 0:8], rhs=negrow[0:1, half * 320:(half + 1) * 320], start=False, stop=True),
                 reads=["c_ones", "negrow"], writes=[("psb", half)])
            S.op("act", lambda half=half, pb=pb: A_.mul(out=tvs[:, half * 320:(half + 1) * 320], in_=pb[0:8, 0:320], mul=8.0),
                 reads=[("psb", half)], writes=["tvs"])
        S.dma("sp", lambda: nc.sync.dma_start(out=tv_d.ap()[:, :], in_=tvs[:]), reads=["tvs"], writes=["tv_d"])

        lam4 = sb(st, "lam4", [128, 4, 64], F32)
        for i, nm in enumerate(("lq1", "lk1", "lq2", "lk2")):
            S.dma("sp", lambda i=i, nm=nm: nc.sync.dma_start(out=lam4[:, i, :], in_=W[nm][0:1, :].partition_broadcast(128)), writes=[("lam4", i)])
        lsum = sb(st, "lsum", [128, 2], F32)
        ljunk = sb(st, "ljunk", [128, 64], F32)
        for i in range(2):
            S.op("dve", lambda i=i: V.tensor_tensor(out=ljunk[:], in0=lam4[:, 2 * i, :], in1=lam4[:, 2 * i + 1, :], op=ALU.mult),
                 reads=[("lam4", 2 * i), ("lam4", 2 * i + 1)], writes=["ljunk"])
            S.op("dve", lambda i=i: V.tensor_reduce(out=lsum[:, i:i + 1], in_=ljunk[:], axis=AX.X, op=ALU.add),
                 reads=["ljunk"], writes=[("lsum", i)])
        S.op("act", lambda: A_.activation(out=lsum[:], in_=lsum[:], func=AF.Exp), reads=[("lsum", 0), ("lsum", 1)], writes=["lsume"])
        nlam = sb(st, "nlam", [128, 1], F32)
        S.op("dve", lambda: V.tensor_tensor(out=nlam[:], in0=lsum[:, 1:2], in1=lsum[:, 0:1], op=ALU.subtract), reads=["lsume"], writes=["nlam"])
        S.op("dve", lambda: V.tensor_scalar(out=nlam[:], in0=nlam[:], scalar1=-LAM_INIT, scalar2=None, op0=ALU.add), reads=["nlam"], writes=["nlam"])
        gs8 = sb(st, "gs8", [128, 128], F32)
        S.dma("sp", lambda: nc.sync.dma_start(out=gs8[:], in_=bcast_rows(W["g_subln"][0:1, :])), writes=["gs8"])
        S.op("dve", lambda: V.tensor_scalar(out=gs8[:], in0=gs8[:], scalar1=(1.0 - LAM_INIT), scalar2=None, op0=ALU.mult), reads=["gs8"], writes=["gs8"])
        eps_t = sb(st, "eps_t2", [128, 1], F32)
        S.op("dve", lambda: V.memset(eps_t[:], EPS), writes=["eps2"])

        tri, ones = cs["tri"], cs["ones"]
        omt = sb(st, "omt", [128, 128], F32)
        S.op("dve", lambda: V.tensor_tensor(out=omt[:], in0=ones[:], in1=tri[:], op=ALU.subtract), reads=["c_ones", "c_tri"], writes=["omt"])
        mk = {n: sb(st, "mk_" + n, [128, 128], F32) for n in ("XB", "XC", "YB", "YC")}
        omh = sb(st, "omh", [128, 1], F32)
        S.op("dve", lambda: V.tensor_scalar(out=omh[:], in0=half_t[:], scalar1=-1.0, scalar2=1.0, op0=ALU.mult, op1=ALU.add), reads=["half"], writes=["omh"])
        S.op("dve", lambda: V.scalar_tensor_tensor(out=mk["XB"][:], in0=omt[:], scalar=half_t[:, 0:1], in1=tri[:], op0=ALU.mult, op1=ALU.add),
             reads=["omt", "half", "c_tri"], writes=["mk_XB"])
        S.op("dve", lambda: V.tensor_scalar(out=mk["XC"][:], in0=tri[:], scalar1=half_t[:, 0:1], scalar2=None, op0=ALU.mult), reads=["c_tri", "half"], writes=["mk_XC"])
        S.op("dve", lambda: V.scalar_tensor_tensor(out=mk["YB"][:], in0=omt[:], scalar=omh[:, 0:1], in1=tri[:], op0=ALU.mult, op1=ALU.add),
             reads=["omt", "omh", "c_tri"], writes=["mk_YB"])
        S.op("dve", lambda: V.tensor_scalar(out=mk["YC"][:], in0=tri[:], scalar1=omh[:, 0:1], scalar2=None, op0=ALU.mult), reads=["c_tri", "omh"], writes=["mk_YC"])

        wq = [sb(st, "wq%d" % i, [128, 16, 128], BF16) for i in range(3)]
        qT = sb(st, "qT", [128, NOWN], BF16)
        kT = sb(st, "kT", [128, SEQ], BF16)
        vA = sb(st, "vA", [128, 16, 130], BF16)
        Ht = [sb(st, "Ht%d" % i, [128, 128], F32) for i in range(4)]
        slot = {n: sb(st, "slot" + n, [128, 128], F32) for n in ("XA", "XB", "XC", "YA", "YB", "YC")}
        hd = sb(st, "hd", [128, 128], F32)
        PT = [sb(st, "PT%d" % i, [128, 128], BF16) for i in range(4)]
        e_t = [sb(st, "e_t%d" % i, [128, 128], F32) for i in range(2)]
        lnp = [sb(st, "lnp%d" % i, [128, 128], F32) for i in range(2)]
        LK = [sb(st, "LK%d" % i, [128, 128], F32) for i in range(2)]
        arg = [sb(st, "arg%d" % i, [128, 128], F32) for i in range(2)]
        Acc = sb(st, "Acc", [128, 128], F32)
        o1 = sb(st, "o1", [128, 128], F32)
        o2 = sb(st, "o2", [128, 128], F32)
        obf = sb(st, "obf", [128, 128], BF16)
        rr = sb(st, "rr", [128, 4], F32)
        sjunk = sb(st, "sjunk", [128, 128], F32)
        S.op("dve", lambda: V.memset(vA[:], 1.0), writes=["vA_init"])

        w_in_r = W["w_in"].rearrange("(c p) n -> p c n", p=128)
        SC_A = 64 ** -0.5
        SC_B = 128 ** -0.5

        def own_tile_info(j):
            p = j // 2
            if j % 2 == 0:
                return 2 * p + 2, "X", (2 * p - 1, 2 * p, 2 * p + 1)
            return 16 - 2 * p, "Y", (13 - 2 * p, 14 - 2 * p, 15 - 2 * p)

        def project_head(h, colbase, with_ones):
            for i, off in enumerate((0, 1024, 2048)):
                c0 = colbase + off + h * 128
                for hh in range(2):
                    S.dma("pool", lambda i=i, c0=c0, hh=hh: nc.gpsimd.dma_start(out=wq[i][:, hh * 8:(hh + 1) * 8, :], in_=w_in_r[:, hh * 8:(hh + 1) * 8, c0:c0 + 128]),
                          writes=[("wq", i, hh)])
            n = 0
            for ch in range(2):
                pb = psb[n % 2]; n += 1
                for c in range(16):
                    S.op("pe", lambda c=c, ch=ch, pb=pb: T_.matmul(pb[:, :], lhsT=wq[0][:, c, :], rhs=hT_own[:, c, ch * 512:(ch + 1) * 512], start=(c == 0), stop=(c == 15)),
                         reads=[("wq", 0, c // 8)], writes=[("psb", (n - 1) % 2)])
                S.op("act", lambda ch=ch, pb=pb: A_.copy(out=qT[:, ch * 512:(ch + 1) * 512], in_=pb[:, :]), reads=[("psb", (n - 1) % 2)], writes=[("qT", ch)])
            for ch in range(4):
                pb = psb[n % 2]; n += 1
                for c in range(16):
                    S.op("pe", lambda c=c, ch=ch, pb=pb: T_.matmul(pb[:, :], lhsT=wq[1][:, c, :], rhs=hT_all[:, c, ch * 512:(ch + 1) * 512], start=(c == 0), stop=(c == 15)),
                         reads=[("wq", 1, c // 8)], writes=[("psb", (n - 1) % 2)])
                S.op("dve", lambda ch=ch, pb=pb: V.tensor_copy(out=kT[:, ch * 512:(ch + 1) * 512], in_=pb[:, :]), reads=[("psb", (n - 1) % 2)], writes=[("kT", ch)])
            for kb4 in range(4):
                pb = psb[n % 2]; n += 1
                for k4 in range(4):
                    kb = kb4 * 4 + k4
                    for c in range(16):
                        S.op("pe", lambda c=c, kb=kb, k4=k4, pb=pb: T_.matmul(pb[:, k4 * 128:(k4 + 1) * 128], lhsT=hT_all[:, c, kb * 128:(kb + 1) * 128], rhs=wq[2][:, c, :], start=(c == 0), stop=(c == 15)),
                             reads=[("wq", 2, c // 8)], writes=[("psb", (n - 1) % 2)])
                eng = "act" if kb4 % 2 == 0 else "dve"
                if eng == "act":
                    S.op("act", lambda kb4=kb4, pb=pb: A_.copy(out=vA[:, kb4 * 4:(kb4 + 1) * 4, 0:128], in_=pb[:, :].rearrange("p (a b) -> p a b", a=4)),
                         reads=[("psb", (n - 1) % 2), "vA_init"], writes=[("vA", kb4)])
                else:
                    S.op("dve", lambda kb4=kb4, pb=pb: V.tensor_copy(out=vA[:, kb4 * 4:(kb4 + 1) * 4, 0:128], in_=pb[:, :].rearrange("p (a b) -> p a b", a=4)),
                         reads=[("psb", (n - 1) % 2), "vA_init"], writes=[("vA", kb4)])

        def qk_keys(j, kb):
            return [("qT", j // 4), ("kT", kb // 4)]

        pend = [None]
        pendB = [None]
        for h in range(NH):
            project_head(h, 0, True)
            for i, dl in enumerate((-128, 0, 128, 256)):
                src = bass.AP(tv_d, h * 640 + 129 + dl, [[1, 128], [1, 128]])
                S.dma("sp", lambda i=i, src=src: nc.sync.dma_start(out=Ht[i][:], in_=src), reads=["tv_d"], writes=[("Ht", i)])
            for nm, lo, hi in (("XA", 2, 3), ("XB", 1, 2), ("XC", 0, 1), ("YA", 3, 2), ("YB", 2, 1), ("YC", 1, 0)):
                S.op("dve", lambda lo=lo, hi=hi: V.tensor_tensor(out=hd[:], in0=Ht[hi][:], in1=Ht[lo][:], op=ALU.subtract),
                     reads=[("Ht", lo), ("Ht", hi)], writes=["hd"])
                S.op("dve", lambda nm=nm, lo=lo: V.scalar_tensor_tensor(out=slot[nm][:], in0=hd[:], scalar=half_t[:, 0:1], in1=Ht[lo][:], op0=ALU.mult, op1=ALU.add),
                     reads=["hd", "half", ("Ht", lo)], writes=[("slot", nm)])
            git = 0
            for j in range(8):
                L, sset, slots = own_tile_info(j)
                obase = 4 if j % 2 == 0 else 0
                steps = [(m, kb) for m in range(2) for kb in range(L)]

                def da_front(i):
                    m, kb = steps[i]
                    rows = slice(64 * m, 64 * m + 64)
                    g = git + i
                    sbk = 2 + (g % 2)
                    pt = PT[g % 4]
                    sl = None
                    if kb in slots:
                        sl = sset + "ABC"[slots.index(kb)]
                    S.op("pe", lambda: T_.matmul(psb[sbk][:, 0:128], lhsT=kT[rows, kb * 128:(kb + 1) * 128], rhs=qT[rows, j * 128:(j + 1) * 128], start=True, stop=(sl is None)),
                         reads=qk_keys(j, kb), writes=[("psb", sbk)])
                    if sl is not None:
                        S.op("pe", lambda: T_.matmul(psb[sbk][:, 0:128], lhsT=cs["antiI"][:], rhs=slot[sl][:], start=False, stop=True),
                             reads=["c_antiI", ("slot", sl)], writes=[("psb", sbk)])
                    S.op("act", lambda: A_.activation(out=pt[:], in_=psb[sbk][:, 0:128], func=AF.Exp, bias=b31[:, h:h + 1], scale=SC_A),
                         reads=[("psb", sbk), "b31"], writes=[("PT", g % 4)])

                def da_back(i):
                    m, kb = steps[i]
                    g = git + i
                    ob = obase + m
                    pt = PT[g % 4]
                    S.op("pe", lambda: T_.matmul(psb[ob][:, 0:130], lhsT=pt[:], rhs=vA[:, kb, :], start=(kb == 0), stop=(kb == L - 1)),
                         reads=[("PT", g % 4), ("vA", kb // 4)], writes=[("psb", ob)])

                for i in range(len(steps)):
                    da_front(i)
                    if i >= 1:
                        da_back(i - 1)
                    if i == 2 and pendB[0] is not None:
                        pendB[0]()
                        pendB[0] = None
                    if i == min(5, len(steps) - 1) and pend[0] is not None:
                        pend[0]()
                        pend[0] = None
                da_back(len(steps) - 1)
                git += len(steps)
                o1b, o2b = obase, obase + 1
                S.op("dve", lambda: V.reciprocal(out=rr[:, 0:1], in_=psb[o1b][:, 128:129]), reads=[("psb", o1b)], writes=["rr0"])
                S.op("dve", lambda: V.reciprocal(out=rr[:, 1:2], in_=psb[o2b][:, 128:129]), reads=[("psb", o2b)], writes=["rr1"])
                S.op("dve", lambda: V.tensor_tensor(out=rr[:, 1:2], in0=rr[:, 1:2], in1=nlam[:], op=ALU.mult), reads=["rr1", "nlam"], writes=["rr1"])
                S.op("dve", lambda: V.tensor_scalar(out=o1[:], in0=psb[o1b][:, 0:128], scalar1=rr[:, 0:1], scalar2=None, op0=ALU.mult), reads=[("psb", o1b), "rr0"], writes=["o1"])
                S.op("dve", lambda: V.scalar_tensor_tensor(out=o2[:], in0=psb[o2b][:, 0:128], scalar=rr[:, 1:2], in1=o1[:], op0=ALU.mult, op1=ALU.add),
                     reads=[("psb", o2b), "rr1", "o1"], writes=["o2"])
                S.op("dve", lambda: V.tensor_tensor(out=sjunk[:], in0=o2[:], in1=o2[:], op=ALU.mult), reads=["o2"], writes=["sjunk"])
                S.op("dve", lambda: V.tensor_reduce(out=rr[:, 2:3], in_=sjunk[:], axis=AX.X, op=ALU.add), reads=["sjunk"], writes=["rr2"])

                def ln_a():
                    S.op("act", lambda: A_.activation(out=rr[:, 3:4], in_=rr[:, 2:3], func=AF.Ln, bias=eps_t[:], scale=1.0 / 128), reads=["rr2", "eps2"], writes=["rr3"])
                    S.op("act", lambda: A_.activation(out=rr[:, 2:3], in_=rr[:, 3:4], func=AF.Exp, scale=-0.5), reads=["rr3"], writes=["rr2"])
                    S.op("dve", lambda: V.scalar_tensor_tensor(out=obf[:], in0=o2[:], scalar=rr[:, 2:3], in1=gs8[:], op0=ALU.mult, op1=ALU.mult),
                         reads=["o2", "rr2", "gs8"], writes=["obf"])
                pendB[0] = ln_a

                def tr_a(h=h, j=j):
                    ptb = ps_bf(6)
                    S.op("pe", lambda: T_.transpose(ptb[:, 0:128], obf[:], identb[:]), reads=["obf", "identb"], writes=[("psb", 6)])
                    S.op("act", lambda: A_.copy(out=oaT[:, h, j * 128:(j + 1) * 128], in_=ptb[:, 0:128]), reads=[("psb", 6)], writes=[("oaT", h)])
                pend[0] = tr_a
            pendB[0]()
            pendB[0] = None
            pend[0]()
            pend[0] = None

        for h in range(NH):
            project_head(h, 3072, False)
            git = 0
            for j in range(8):
                L, sset, slots = own_tile_info(j)
                kbs = list(range(L - 1, -1, -1))
                ab = 4 if j % 2 == 0 else 0

                def sb_mask(kb):
                    if kb in slots:
                        nm = sset + "ABC"[slots.index(kb)]
                        if nm[1] != "A":
                            return nm
                    return None

                def sb_front(i):
                    kb = kbs[i]
                    g = git + i
                    zb = 2 + (g % 2)
                    b2 = g % 2
                    mkt = sb_mask(kb)
                    S.op("pe", lambda: T_.matmul(psb[zb][:, 0:128], lhsT=kT[:, kb * 128:(kb + 1) * 128], rhs=qT[:, j * 128:(j + 1) * 128], start=True, stop=True),
                         reads=qk_keys(j, kb), writes=[("psb", zb)])
                    S.op("act", lambda: A_.activation(out=e_t[b2][:], in_=psb[zb][:, 0:128], func=AF.Exp, scale=-SC_B), reads=[("psb", zb)], writes=[("e_t", b2)])
                    S.op("act", lambda: A_.activation(out=lnp[b2][:], in_=e_t[b2][:], func=AF.Ln, bias=1.0, scale=1.0), reads=[("e_t", b2)], writes=[("lnp", b2)])
                    S.op("dve", lambda: V.scalar_tensor_tensor(out=LK[b2][:], in0=psb[zb][:, 0:128], scalar=-SC_B, in1=lnp[b2][:], op0=ALU.mult, op1=ALU.subtract),
                         reads=[("psb", zb), ("lnp", b2)], writes=[("LK", b2)])
                    if mkt is not None:
                        S.op("pool", lambda: P_.tensor_tensor(out=LK[b2][:], in0=LK[b2][:], in1=mk[mkt][:], op=ALU.mult),
                             reads=[("LK", b2), "mk_" + mkt], writes=[("LK", b2)])

                def sb_mid(i):
                    kb = kbs[i]
                    g = git + i
                    b2 = g % 2
                    lb = 5 if g % 2 == 0 else 7
                    pt = PT[g % 4]
                    first = (i == 0)
                    mkt = sb_mask(kb)
                    S.op("pe", lambda: T_.matmul(psb[lb][:, 0:128], lhsT=cs["ugt"][:], rhs=LK[b2][:], start=True, stop=first),
                         reads=["c_ugt", ("LK", b2)], writes=[("psb", lb)])
                    if not first:
                        S.op("pe", lambda: T_.matmul(psb[lb][:, 0:128], lhsT=cs["ones"][:], rhs=Acc[:], start=False, stop=True),
                             reads=["c_ones", "Acc"], writes=[("psb", lb)])
                    if kb > 0:
                        if first:
                            S.op("pool", lambda: P_.tensor_copy(out=Acc[:], in_=LK[b2][:]), reads=[("LK", b2)], writes=["Acc"])
                        else:
                            S.op("pool", lambda: P_.tensor_tensor(out=Acc[:], in0=Acc[:], in1=LK[b2][:], op=ALU.add), reads=[("LK", b2), "Acc"], writes=["Acc"])
                    S.op("dve", lambda: V.tensor_tensor(out=arg[b2][:], in0=psb[lb][:, 0:128], in1=lnp[b2][:], op=ALU.subtract),
                         reads=[("psb", lb), ("lnp", b2)], writes=[("arg", b2)])
                    S.op("act", lambda: A_.activation(out=pt[:], in_=arg[b2][:], func=AF.Exp), reads=[("arg", b2)], writes=[("PT", g % 4)])
                    if mkt is not None:
                        S.op("pool", lambda: P_.tensor_tensor(out=pt[:], in0=pt[:], in1=mk[mkt][:], op=ALU.mult),
                             reads=[("PT", g % 4), "mk_" + mkt], writes=[("PT", g % 4)])

                def sb_av(i):
                    kb = kbs[i]
                    g = git + i
                    pt = PT[g % 4]
                    S.op("pe", lambda: T_.matmul(psb[ab][:, 0:128], lhsT=pt[:], rhs=vA[:, kb, 0:128], start=(i == 0), stop=(kb == 0)),
                         reads=[("PT", g % 4), ("vA", kb // 4)], writes=[("psb", ab)])

                n_it = len(kbs)
                for i in range(n_it):
                    sb_front(i)
                    if i >= 1:
                        sb_mid(i - 1)
                    if i >= 2:
                        sb_av(i - 2)
                    if i == 1 and pend[0] is not None:
                        pend[0]()
                        pend[0] = None
                sb_mid(n_it - 1)
                if n_it >= 2:
                    sb_av(n_it - 2)
                sb_av(n_it - 1)
                git += len(kbs)
                S.op("dve", lambda: V.tensor_copy(out=obf[:], in_=psb[ab][:, 0:128]), reads=[("psb", ab)], writes=["obf"])
                def tr_b(h=h, j=j):
                    ptb = ps_bf(6)
                    S.op("pe", lambda: T_.transpose(ptb[:, 0:128], obf[:], identb[:]), reads=["obf", "identb"], writes=[("psb", 6)])
                    S.op("act", lambda: A_.copy(out=obT[:, h, j * 128:(j + 1) * 128], in_=ptb[:, 0:128]), reads=[("psb", 6)], writes=[("obT", h)])
                pend[0] = tr_b
            pend[0]()
            pend[0] = None
        if dbg:
            for nm, tt in (("oaT", oaT), ("obT", obT)):
                d = dbg_tensor(nm, [128, NH * NOWN])
                tmp = sb(st, "dtmp_" + nm, [128, NOWN], F32)
                for h in range(NH):
                    S.op("dve", lambda h=h, tt=tt, tmp=tmp: V.tensor_copy(out=tmp[:], in_=tt[:, h, :]), reads=[(nm, h)], writes=["dbg_" + nm])
                    S.dma("sp", lambda h=h, d=d, tmp=tmp: nc.sync.dma_start(out=d[:, h * NOWN:(h + 1) * NOWN], in_=tmp[:]), reads=["dbg_" + nm])


def merge_phase(nc, S, sb, psb, W, hT_own, oaT, obT, mergedT):
    V, A_, P_, T_ = nc.vector, nc.scalar, nc.gpsimd, nc.tensor
    with ExitStack() as st:
        wpa = [sb(st, "wpa%d" % i, [128, 8, 128], BF16) for i in range(2)]
        wpb = [sb(st, "wpb%d" % i, [128, 8, 128], BF16) for i in range(2)]
        wga = [sb(st, "wga%d" % i, [128, 16, 128], BF16) for i in range(2)]
        wgb = [sb(st, "wgb%d" % i, [128, 16, 128], BF16) for i in range(2)]
        sg = [[sb(st, "sg%d_%d" % (i, j), [128, 512], F32) for j in range(2)] for i in range(2)]
        tt = [[sb(st, "tt%d_%d" % (i, j), [128, 512], F32) for j in range(2)] for i in range(2)]
        wpa_r = W["w_proj_a"].rearrange("(k p) n -> p k n", p=128)
        wpb_r = W["w_proj_b"].rearrange("(k p) n -> p k n", p=128)
        w_in_r = W["w_in"].rearrange("(c p) n -> p c n", p=128)
        it = 0
        for m in range(16):
            b = m % 2
            cs_ = slice(m * 128, (m + 1) * 128)
            S.dma("pool", lambda: nc.gpsimd.dma_start(out=wpa[b][:], in_=wpa_r[:, :, cs_]), writes=[("wpa", b)])
            S.dma("pool", lambda: nc.gpsimd.dma_start(out=wpb[b][:], in_=wpb_r[:, :, cs_]), writes=[("wpb", b)])
            for hh in range(2):
                S.dma("pool", lambda: nc.gpsimd.dma_start(out=wga[b][:, hh * 8:(hh + 1) * 8, :], in_=w_in_r[:, hh * 8:(hh + 1) * 8, 6144 + m * 128:6144 + (m + 1) * 128]),
                      writes=[("wga", b, hh)])
                S.dma("pool", lambda: nc.gpsimd.dma_start(out=wgb[b][:, hh * 8:(hh + 1) * 8, :], in_=w_in_r[:, hh * 8:(hh + 1) * 8, 8192 + m * 128:8192 + (m + 1) * 128]),
                      writes=[("wgb", b, hh)])
            for th in range(2):
                base = 4 * (it % 2)
                q = it % 2
                it += 1
                tok = slice(th * 512, (th + 1) * 512)
                for k in range(8):
                    S.op("pe", lambda: T_.matmul(psb[base][:, :], lhsT=wpa[b][:, k, :], rhs=oaT[:, k, tok], start=(k == 0), stop=(k == 7)),
                         reads=[("wpa", b)], writes=[("psb", base)])
                for k in range(8):
                    S.op("pe", lambda: T_.matmul(psb[base + 1][:, :], lhsT=wpb[b][:, k, :], rhs=obT[:, k, tok], start=(k == 0), stop=(k == 7)),
                         reads=[("wpb", b)], writes=[("psb", base + 1)])
                for c in range(16):
                    S.op("pe", lambda: T_.matmul(psb[base + 2][:, :], lhsT=wga[b][:, c, :], rhs=hT_own[:, c, tok], start=(c == 0), stop=(c == 15)),
                         reads=[("wga", b, c // 8)], writes=[("psb", base + 2)])
                for c in range(16):
                    S.op("pe", lambda: T_.matmul(psb[base + 3][:, :], lhsT=wgb[b][:, c, :], rhs=hT_own[:, c, tok], start=(c == 0), stop=(c == 15)),
                         reads=[("wgb", b, c // 8)], writes=[("psb", base + 3)])
                S.op("act", lambda: A_.activation(out=sg[q][0][:], in_=psb[base + 2][:, :], func=AF.Sigmoid), reads=[("psb", base + 2)], writes=[("sg", q, 0)])
                S.op("act", lambda: A_.activation(out=sg[q][1][:], in_=psb[base + 3][:, :], func=AF.Sigmoid), reads=[("psb", base + 3)], writes=[("sg", q, 1)])
                S.op("dve", lambda: V.tensor_tensor(out=tt[q][0][:], in0=psb[base][:, :], in1=sg[q][0][:], op=ALU.mult),
                     reads=[("psb", base), ("sg", q, 0)], writes=[("tt", q, 0)])
                S.op("dve", lambda: V.tensor_tensor(out=tt[q][1][:], in0=psb[base + 1][:, :], in1=sg[q][1][:], op=ALU.mult),
                     reads=[("psb", base + 1), ("sg", q, 1)], writes=[("tt", q, 1)])
                S.op("pool", lambda: P_.tensor_tensor(out=mergedT[:, m, tok], in0=tt[q][0][:], in1=tt[q][1][:], op=ALU.add),
                     reads=[("tt", q, 0), ("tt", q, 1)], writes=[("mergedT", m, th)])
        S.barrier()


def resid_phase(nc, S, sb, psb, W, mergedT, x_own, x1_d, mod_d):
    V, A_, P_, T_ = nc.vector, nc.scalar, nc.gpsimd, nc.tensor
    with ExitStack() as st:
        wo = [sb(st, "wo%d" % i, [128, 16, 512], BF16) for i in range(2)]
        gm_bc = sb(st, "gm_bc", [128, D], F32)
        xo = [sb(st, "xo%d" % i, [128, 512], F32) for i in range(2)]
        tmp = [sb(st, "rtmp%d" % i, [128, 512], F32) for i in range(2)]
        x1c = [sb(st, "x1c%d" % i, [128, 512], F32) for i in range(2)]
        S.dma("sp", lambda: nc.sync.dma_start(out=gm_bc[:], in_=bcast_rows(mod_d[2:3, :])), writes=["gm_bc"])
        w_out_r = W["w_out"].rearrange("(c p) n -> p c n", p=128)
        it = 0
        for n in range(4):
            b = n % 2
            ns = slice(n * 512, (n + 1) * 512)
            for q in range(4):
                S.dma("pool", lambda: nc.gpsimd.dma_start(out=wo[b][:, q * 4:(q + 1) * 4, :], in_=w_out_r[:, q * 4:(q + 1) * 4, ns]), writes=[("wo", b, q)])
            for t in range(8):
                pb = it % 4
                i2 = it % 2
                it += 1
                ts_ = slice(t * 128, (t + 1) * 128)
                S.dma("sp", lambda: nc.sync.dma_start(out=xo[i2][:], in_=x_own[ts_, ns]), writes=[("xo", i2)])
                for c in range(16):
                    S.op("pe", lambda: T_.matmul(psb[pb][:, :], lhsT=mergedT[:, c, ts_], rhs=wo[b][:, c, :], start=(c == 0), stop=(c == 15)),
                         reads=[("wo", b, c // 4)], writes=[("psb", pb)])
                S.op("dve", lambda: V.tensor_tensor(out=tmp[i2][:], in0=psb[pb][:, :], in1=gm_bc[:, ns], op=ALU.mult),
                     reads=[("psb", pb), "gm_bc"], writes=[("rtmp", i2)])
                S.op("pool", lambda: P_.tensor_tensor(out=x1c[i2][:], in0=tmp[i2][:], in1=xo[i2][:], op=ALU.add),
                     reads=[("rtmp", i2), ("xo", i2)], writes=[("x1c", i2)])
                S.dma("sp", lambda: nc.sync.dma_start(out=x1_d[ts_, ns], in_=x1c[i2][:]), reads=[("x1c", i2)], writes=[("x1d", t, n)])
        S.barrier()


def norm_router_phase(nc, S, sb, psb, W, cs, x1_d, h2T, Gt, Gf_col, shf_col, eps_t):
    V, A_, P_, T_ = nc.vector, nc.scalar, nc.gpsimd, nc.tensor
    BIG = 30000.0
    with ExitStack() as st:
        x1t = [sb(st, "x1t%d" % i, [128, D], F32) for i in range(2)]
        xn = [sb(st, "xn%d" % i, [128, D], F32) for i in range(2)]
        hf = [sb(st, "hf%d" % i, [128, 16, 128], F32) for i in range(2)]
        junk = sb(st, "njunk", [128, D], BF16)
        ss = [sb(st, "nss%d" % i, [128, 1], F32) for i in range(2)]
        rs = [sb(st, "nrs%d" % i, [128, 1], F32) for i in range(2)]
        wr = sb(st, "wr", [128, 16, 72], F32)
        brt = sb(st, "brt", [128, 72], F32)
        lg = sb(st, "lg", [128, 72], F32)
        sm = {n: sb(st, "r_" + n, [128, 1], F32) for n in ("gmax", "ngmax", "gsum", "pg", "m1", "m2", "dd", "rr", "den", "p1", "c1", "c2")}
        ohg = sb(st, "ohg", [128, 8], F32)
        pen = sb(st, "pen", [128, 8], F32)
        gjunk = sb(st, "gjunk", [128, 8], F32)
        em = sb(st, "em", [128, 64], F32)
        em2 = sb(st, "em2", [128, 64], F32)
        mask1 = sb(st, "mask1", [128, 64], F32)
        mask2 = sb(st, "mask2", [128, 64], F32)
        w_rg_r = W["w_rg"].rearrange("(c p) n -> p c n", p=128)
        w_re_r = W["w_re"].rearrange("(c p) n -> p c n", p=128)
        for hh in range(2):
            S.dma("sp", lambda: nc.sync.dma_start(out=wr[:, hh * 8:(hh + 1) * 8, 0:8], in_=w_rg_r[:, hh * 8:(hh + 1) * 8, :]), writes=[("wr", 0, hh)])
            S.dma("sp", lambda: nc.sync.dma_start(out=wr[:, hh * 8:(hh + 1) * 8, 8:72], in_=w_re_r[:, hh * 8:(hh + 1) * 8, :]), writes=[("wr", 1, hh)])
        S.dma("sp", lambda: nc.sync.dma_start(out=brt[:, 0:8], in_=bcast_rows(W["b_rg"][0:1, :])), writes=[("brt", 0)])
        S.dma("sp", lambda: nc.sync.dma_start(out=brt[:, 8:72], in_=bcast_rows(W["b_re"][0:1, :])), writes=[("brt", 1)])
        wr_keys = [("wr", 0, 0), ("wr", 0, 1), ("wr", 1, 0), ("wr", 1, 1)]
        for t in range(8):
            b = t % 2
            ts_ = slice(t * 128, (t + 1) * 128)
            for hh in range(2):
                S.dma("sp", lambda: nc.sync.dma_start(out=x1t[b][:, hh * 1024:(hh + 1) * 1024], in_=x1_d[ts_, hh * 1024:(hh + 1) * 1024]), writes=[("x1t", b, hh)])
            S.op("act", lambda: A_.activation(out=junk[:], in_=x1t[b][:], func=AF.Square, accum_out=ss[b][:]),
                 reads=[("x1t", b, 0), ("x1t", b, 1)], writes=[("nss", b), "njunk"])
            S.op("act", lambda: A_.activation(out=rs[b][:], in_=ss[b][:], func=AF.Sqrt, bias=eps_t[:], scale=1.0 / D),
                 reads=[("nss", b), "eps"], writes=[("nrs", b)])
            S.op("dve", lambda: V.reciprocal(out=rs[b][:], in_=rs[b][:]), reads=[("nrs", b)], writes=[("nrs", b)])
            S.op("dve", lambda: V.tensor_scalar(out=xn[b][:], in0=x1t[b][:], scalar1=rs[b][:, 0:1], scalar2=None, op0=ALU.mult),
                 reads=[("x1t", b, 0), ("x1t", b, 1), ("nrs", b)], writes=[("xn", b)])
            for g in range(4):
                for k in range(4):
                    c = g * 4 + k
                    S.op("pe", lambda: T_.matmul(psb[g][:, k * 128:(k + 1) * 128], lhsT=xn[b][:, c * 128:(c + 1) * 128], rhs=cs["ident"][:], start=True, stop=True),
                         reads=[("xn", b), "c_ident"], writes=[("psb", g)])
                for k in range(4):
                    c = g * 4 + k
                    S.op("act", lambda: A_.activation(out=hf[b][:, c, :], in_=psb[g][:, k * 128:(k + 1) * 128], func=AF.Identity,
                                                      bias=shf_col[:, c:c + 1], scale=Gf_col[:, c:c + 1]),
                         reads=[("psb", g), "shf_col", "Gf_col"], writes=[("hf", b, g)])
            S.op("pool", lambda: P_.tensor_copy(out=h2T[:, :, ts_], in_=hf[b][:, :, :]), reads=[("hf", b, g) for g in range(4)], writes=[("h2T", t)])
            rb = 4 + b
            for c in range(16):
                S.op("pe", lambda: T_.matmul(psb[rb][:, 0:72], lhsT=hf[b][:, c, :], rhs=wr[:, c, :], start=(c == 0), stop=(c == 15)),
                     reads=[("hf", b, c // 4)] + wr_keys, writes=[("psb", rb)])
            S.op("dve", lambda: V.tensor_tensor(out=lg[:], in0=psb[rb][:, 0:72], in1=brt[:], op=ALU.add), reads=[("psb", rb), ("brt", 0), ("brt", 1)], writes=["lg"])
            S.op("dve", lambda: V.tensor_reduce(out=sm["gmax"][:], in_=lg[:, 0:8], axis=AX.X, op=ALU.max), reads=["lg"], writes=["gmax"])
            S.op("dve", lambda: V.tensor_scalar(out=ohg[:], in0=lg[:, 0:8], scalar1=sm["gmax"][:, 0:1], scalar2=None, op0=ALU.is_ge), reads=["lg", "gmax"], writes=["ohg"])
            S.op("dve", lambda: V.tensor_scalar(out=sm["ngmax"][:], in0=sm["gmax"][:], scalar1=-1.0, scalar2=None, op0=ALU.mult), reads=["gmax"], writes=["ngmax"])
            S.op("act", lambda: A_.activation(out=gjunk[:], in_=lg[:, 0:8], func=AF.Exp, bias=sm["ngmax"][:, 0:1], scale=1.0, accum_out=sm["gsum"][:]),
                 reads=["lg", "ngmax"], writes=["gsum", "gjunk"])
            S.op("dve", lambda: V.reciprocal(out=sm["pg"][:], in_=sm["gsum"][:]), reads=["gsum"], writes=["pg"])
            S.op("dve", lambda: V.tensor_scalar(out=pen[:], in0=ohg[:], scalar1=BIG, scalar2=-BIG, op0=ALU.mult, op1=ALU.add), reads=["ohg"], writes=["pen"])
            for g in range(8):
                S.op("dve", lambda: V.tensor_scalar(out=em[:, g * 8:(g + 1) * 8], in0=lg[:, 8 + g * 8:16 + g * 8], scalar1=pen[:, g:g + 1], scalar2=None, op0=ALU.add),
                     reads=["lg", "pen"], writes=[("em", g)])
            emk = [("em", g) for g in range(8)]
            S.op("dve", lambda: V.tensor_reduce(out=sm["m1"][:], in_=em[:], axis=AX.X, op=ALU.max), reads=emk, writes=["m1"])
            S.op("dve", lambda: V.tensor_scalar(out=mask1[:], in0=em[:], scalar1=sm["m1"][:, 0:1], scalar2=None, op0=ALU.is_ge), reads=emk + ["m1"], writes=["mask1"])
            S.op("dve", lambda: V.scalar_tensor_tensor(out=em2[:], in0=mask1[:], scalar=-BIG, in1=em[:], op0=ALU.mult, op1=ALU.add), reads=emk + ["mask1"], writes=["em2"])
            S.op("dve", lambda: V.tensor_reduce(out=sm["m2"][:], in_=em2[:], axis=AX.X, op=ALU.max), reads=["em2"], writes=["m2"])
            S.op("dve", lambda: V.tensor_scalar(out=mask2[:], in0=em2[:], scalar1=sm["m2"][:, 0:1], scalar2=None, op0=ALU.is_ge), reads=["em2", "m2"], writes=["mask2"])
            S.op("dve", lambda: V.tensor_tensor(out=sm["dd"][:], in0=sm["m2"][:], in1=sm["m1"][:], op=ALU.subtract), reads=["m1", "m2"], writes=["dd"])
            S.op("act", lambda: A_.activation(out=sm["rr"][:], in_=sm["dd"][:], func=AF.Exp), reads=["dd"], writes=["rr"])
            S.op("dve", lambda: V.tensor_scalar(out=sm["den"][:], in0=sm["rr"][:], scalar1=1.0, scalar2=None, op0=ALU.add), reads=["rr"], writes=["den"])
            S.op("dve", lambda: V.reciprocal(out=sm["p1"][:], in_=sm["den"][:]), reads=["den"], writes=["p1"])
            S.op("dve", lambda: V.tensor_tensor(out=sm["c1"][:], in0=sm["p1"][:], in1=sm["pg"][:], op=ALU.mult), reads=["p1", "pg"], writes=["c1"])
            S.op("dve", lambda: V.tensor_tensor(out=sm["c2"][:], in0=sm["c1"][:], in1=sm["rr"][:], op=ALU.mult), reads=["c1", "rr"], writes=["c2"])
            S.op("dve", lambda: V.tensor_scalar(out=Gt[:, t, :], in0=mask1[:], scalar1=sm["c1"][:, 0:1], scalar2=None, op0=ALU.mult), reads=["mask1", "c1"], writes=[("Gt", t)])
            S.op("dve", lambda: V.scalar_tensor_tensor(out=Gt[:, t, :], in0=mask2[:], scalar=sm["c2"][:, 0:1], in1=Gt[:, t, :], op0=ALU.mult, op1=ALU.add),
                 reads=["mask2", "c2", ("Gt", t)], writes=[("Gt", t)])
        S.barrier()


def moe_weight_bufs(nc, stack, side=None):
    kw = {"side": side} if side else {}
    wg = [stack.enter_context(nc.sbuf_tensor("wg%d" % i, [128, 16, 128], BF16, **kw)) for i in range(MOE_NWB)]
    wu = [stack.enter_context(nc.sbuf_tensor("wu%d" % i, [128, 16, 128], BF16, **kw)) for i in range(MOE_NWB)]
    wd = stack.enter_context(nc.sbuf_tensor("wd", [128, 8, D], BF16, **kw))
    return wg, wu, wd


def moe_issue_unit(nc, S, W, wg, wu, u):
    e, f = u // 8, u % 8
    wb = u % MOE_NWB
    rows_g = W["w_eg"][e * D:(e + 1) * D, :].rearrange("(c p) n -> p c n", p=128)
    rows_u = W["w_eu"][e * D:(e + 1) * D, :].rearrange("(c p) n -> p c n", p=128)
    fs = slice(f * 128, (f + 1) * 128)
    for hh in range(2):
        S.dma("pool", lambda: nc.gpsimd.dma_start(out=wg[wb][:, hh * 8:(hh + 1) * 8, :], in_=rows_g[:, hh * 8:(hh + 1) * 8, fs]), writes=[("wg", wb, hh)])
    for hh in range(2):
        S.dma("pool", lambda: nc.gpsimd.dma_start(out=wu[wb][:, hh * 8:(hh + 1) * 8, :], in_=rows_u[:, hh * 8:(hh + 1) * 8, fs]), writes=[("wu", wb, hh)])


def moe_issue_wd(nc, S, W, wd, e):
    rows_d = W["w_ed"][e * 1024:(e + 1) * 1024, :].rearrange("(f p) n -> p f n", p=128)
    for f2 in range(8):
        S.dma("pool", lambda: nc.gpsimd.dma_start(out=wd[:, f2, :], in_=rows_d[:, f2, :]), writes=[("wd", f2)])


def moe_prefetch(nc, S, W, wg, wu, wd):
    for u in range(MOE_NWB - 1):
        moe_issue_unit(nc, S, W, wg, wu, u)
    moe_issue_wd(nc, S, W, wd, 0)


def moe_phase(nc, S, sb, psb, W, h2T, Gt, acc, NE=64, bufs=None, prefetched=False):
    V, A_, P_, T_ = nc.vector, nc.scalar, nc.gpsimd, nc.tensor
    NWB = MOE_NWB
    NU = NE * 8
    with ExitStack() as st:
        if bufs is None:
            wg, wu, wd = moe_weight_bufs(nc, st)
        else:
            wg, wu, wd = bufs
        actT = sb(st, "actT", [128, 8, NOWN], BF16)
        sl = [sb(st, "sl%d" % i, [128, 512], F32) for i in range(2)]
        SBUF_LOG.append(("moe", nc.sbuf_bytes_remaining))
        for t in range(8):
            S.op("dve", lambda: V.memset(acc[:, t, :], 0.0), writes=[("acc", t, n) for n in range(4)])

        def issue_unit(u):
            moe_issue_unit(nc, S, W, wg, wu, u)

        def issue_wd(e):
            moe_issue_wd(nc, S, W, wd, e)

        if not prefetched:
            moe_prefetch(nc, S, W, wg, wu, wd)
        it = 0
        dn = 0
        for u in range(NU):
            e, f = u // 8, u % 8
            wb = u % NWB
            if u + NWB - 1 < NU:
                issue_unit(u + NWB - 1)
            for half in range(2):
                ab = it % 2
                it += 1
                tok = slice(half * 512, (half + 1) * 512)
                for c in range(16):
                    S.op("pe", lambda: T_.matmul(psb[ab][:, :], lhsT=wg[wb][:, c, :], rhs=h2T[:, c, tok], start=(c == 0), stop=(c == 15)),
                         reads=[("wg", wb, c // 8)], writes=[("psb", ab)])
                for c in range(16):
                    S.op("pe", lambda: T_.matmul(psb[2 + ab][:, :], lhsT=wu[wb][:, c, :], rhs=h2T[:, c, tok], start=(c == 0), stop=(c == 15)),
                         reads=[("wu", wb, c // 8)], writes=[("psb", 2 + ab)])
                S.op("act", lambda: A_.activation(out=sl[ab][:], in_=psb[ab][:, :], func=AF.Silu), reads=[("psb", ab)], writes=[("sl", ab)])
                S.op("dve", lambda: V.tensor_tensor(out=actT[:, f, tok], in0=psb[2 + ab][:, :], in1=sl[ab][:], op=ALU.mult),
                     reads=[("psb", 2 + ab), ("sl", ab)], writes=[("actT", f, half)])
            if f == 7:
                for t in range(8):
                    ts_ = slice(t * 128, (t + 1) * 128)
                    for n in range(4):
                        db = 4 + dn % 4
                        dn += 1
                        ns = slice(n * 512, (n + 1) * 512)
                        for f2 in range(8):
                            S.op("pe", lambda: T_.matmul(psb[db][:, :], lhsT=actT[:, f2, ts_], rhs=wd[:, f2, ns], start=(f2 == 0), stop=(f2 == 7)),
                                 reads=[("actT", f2, t // 4), ("wd", f2)], writes=[("psb", db)])
                        S.op("dve", lambda: V.scalar_tensor_tensor(out=acc[:, t, ns], in0=psb[db][:, :], scalar=Gt[:, t, e:e + 1], in1=acc[:, t, ns], op0=ALU.mult, op1=ALU.add),
                             reads=[("psb", db), ("acc", t, n)], writes=[("acc", t, n)])
                if e + 1 < NE:
                    issue_wd(e + 1)
        S.barrier()


def final_phase(nc, S, sb, W, acc, x1_d, mod_d, out_own, eps_t):
    V, A_, P_ = nc.vector, nc.scalar, nc.gpsimd
    with ExitStack() as st:
        gf_bc = sb(st, "gf_bc", [128, D], F32)
        gfin_bc = sb(st, "gfin_bc", [128, D], F32)
        x1t = [sb(st, "fx1t%d" % i, [128, D], F32) for i in range(2)]
        ot = [sb(st, "fot%d" % i, [128, D], F32) for i in range(2)]
        junk = sb(st, "fjunk", [128, D], BF16)
        ss = [sb(st, "fss%d" % i, [128, 1], F32) for i in range(2)]
        rs = [sb(st, "frs%d" % i, [128, 1], F32) for i in range(2)]
        S.dma("sp", lambda: nc.sync.dma_start(out=gf_bc[:], in_=bcast_rows(mod_d[5:6, :])), writes=["gf_bc"])
        S.dma("sp", lambda: nc.sync.dma_start(out=gfin_bc[:], in_=bcast_rows(W["g_final"][0:1, :])), writes=["gfin_bc"])
        for t in range(8):
            b = t % 2
            ts_ = slice(t * 128, (t + 1) * 128)
            for hh in range(2):
                S.dma("sp", lambda: nc.sync.dma_start(out=x1t[b][:, hh * 1024:(hh + 1) * 1024], in_=x1_d[ts_, hh * 1024:(hh + 1) * 1024]), writes=[("fx1t", b, hh)])
            S.op("dve", lambda: V.tensor_tensor(out=ot[b][:], in0=acc[:, t, :], in1=gf_bc[:], op=ALU.mult), reads=["gf_bc"], writes=[("fot", b)])
            S.op("pool", lambda: P_.tensor_tensor(out=ot[b][:], in0=ot[b][:], in1=x1t[b][:], op=ALU.add),
                 reads=[("fot", b), ("fx1t", b, 0), ("fx1t", b, 1)], writes=[("fot", b)])
            S.op("act", lambda: A_.activation(out=junk[:], in_=ot[b][:], func=AF.Square, accum_out=ss[b][:]), reads=[("fot", b)], writes=[("fss", b), "fjunk"])
            S.op("act", lambda: A_.activation(out=rs[b][:], in_=ss[b][:], func=AF.Sqrt, bias=eps_t[:], scale=1.0 / D), reads=[("fss", b), "eps"], writes=[("frs", b)])
            S.op("dve", lambda: V.reciprocal(out=rs[b][:], in_=rs[b][:]), reads=[("frs", b)], writes=[("frs", b)])
            S.op("dve", lambda: V.scalar_tensor_tensor(out=ot[b][:], in0=ot[b][:], scalar=rs[b][:, 0:1], in1=gfin_bc[:], op0=ALU.mult, op1=ALU.mult),
                 reads=[("fot", b), ("frs", b), "gfin_bc"], writes=[("fot", b)])
            for hh in range(2):
                S.dma("sp", lambda: nc.sync.dma_start(out=out_own[ts_, hh * 1024:(hh + 1) * 1024], in_=ot[b][:, hh * 1024:(hh + 1) * 1024]), reads=[("fot", b)], writes=[("out", t, hh)])


def own_qblocks(half):
    qb = []
    for p in range(4):
        qb.append(2 * p + half)
        qb.append(15 - 2 * p - half)
    return qb


def own_token_index(half):
    return np.concatenate([np.arange(q * 128, (q + 1) * 128) for q in own_qblocks(half)])


def col_layout(v):
    return np.ascontiguousarray(np.asarray(v, np.float32).reshape(-1, 128).T)


def make_shared(inp, stage):
    f = lambda a: np.ascontiguousarray(np.asarray(a, np.float32))
    sh = {
        "rel_bias_table": f(inp["rel_bias_table"]), "w_ada": f(inp["w_ada"][0]), "b_ada": f(inp["b_ada"][0]).reshape(1, -1),
        "g_mix_col": col_layout(inp["g_mix"][0]), "w_in": f(inp["w_in"][0]),
        "lq1": f(inp["lambda_q1"][0]).reshape(1, -1), "lk1": f(inp["lambda_k1"][0]).reshape(1, -1),
        "lq2": f(inp["lambda_q2"][0]).reshape(1, -1), "lk2": f(inp["lambda_k2"][0]).reshape(1, -1),
        "g_subln": f(inp["g_subln"][0]).reshape(1, -1), "w_proj_a": f(inp["w_proj_a"][0]), "w_proj_b": f(inp["w_proj_b"][0]),
        "w_out": f(inp["w_out"][0]), "g_ffn_col": col_layout(inp["g_ffn"][0]),
        "w_rg": f(inp["w_router_group"][0]), "b_rg": f(inp["b_router_group"][0]).reshape(1, -1),
        "w_re": f(inp["w_router_expert"][0]), "b_re": f(inp["b_router_expert"][0]).reshape(1, -1),
        "g_final": f(inp["g_final"]).reshape(1, -1),
    }
    if stage >= 7:
        sh["w_eg"] = f(inp["w_expert_gate"][0]).reshape(64 * D, 1024)
        sh["w_eu"] = f(inp["w_expert_up"][0]).reshape(64 * D, 1024)
        sh["w_ed"] = f(inp["w_expert_down"][0]).reshape(64 * 1024, D)
    for k, v in host_consts().items():
        sh["c_" + k] = v
    return sh


def make_core_map(inp, shared, core):
    b, half = core // 2, core % 2
    xb = np.asarray(inp["x"][b], np.float32)
    m = dict(shared)
    m["x_all"] = np.ascontiguousarray(xb)
    m["x_own"] = np.ascontiguousarray(xb[own_token_index(half)])
    m["c_col"] = col_layout(inp["c"][b])
    m["halfv"] = np.full((128, 1), float(half), np.float32)
    return m


def kernel(**inputs):
    nc, _ = build(stage=99, dbg=False)
    shared = make_shared(inputs, 99)
    in_maps = [make_core_map(inputs, shared, c) for c in range(8)]
    res = run_bass_kernel_spmd(nc, in_maps, core_ids=list(range(8)))
    out = np.zeros((4, SEQ, D), np.float32)
    for c in range(8):
        out[c // 2, own_token_index(c % 2)] = res.results[c]["out_own"]
    return out
```

```python
import math
from contextlib import ExitStack

import numpy as np
import concourse.bass as bass
import concourse.mybir as mybir
from concourse.bass_utils import run_bass_kernel_spmd

F32 = mybir.dt.float32
BF16 = mybir.dt.bfloat16
I32 = mybir.dt.int32
U32 = mybir.dt.uint32
AF = mybir.ActivationFunctionType
ALU = mybir.AluOpType
AX = mybir.AxisListType

D = 2048
SEQ = 2048
NOWN = 1024
NH = 8
NBLK = 80
EPS = 1e-6
LAM_INIT = 0.8 - 0.6 * math.exp(0.0)
NEG = -30000.0
SBUF_LOG = []
MOE_NWB = 4


class Sched:
    def __init__(self, nc, stack, n_dma_sems=32):
        self.nc = nc
        self.eng = {"pe": nc.tensor, "act": nc.scalar, "dve": nc.vector,
                    "pool": nc.gpsimd, "sp": nc.sync}
        self.sem = {e: stack.enter_context(nc.semaphore("s_" + e)) for e in self.eng}
        self.cnt = {e: 0 for e in self.eng}
        self.dsem = [stack.enter_context(nc.semaphore("d%d" % i)) for i in range(n_dma_sems)]
        self.dcnt = [0] * n_dma_sems
        self.dnext = 0
        self.seen = {e: {} for e in self.eng}
        self.lastw = {}
        self.reads = {}
        self.semobj = {}
        for e, s in self.sem.items():
            self.semobj[("e", e)] = s
        for i, s in enumerate(self.dsem):
            self.semobj[("d", i)] = s

    def _wait(self, e, ev):
        sid, val, src = ev
        if src == "pe" and e == "pe":
            return
        if self.seen[e].get(sid, 0) >= val:
            return
        self.seen[e][sid] = val
        self.eng[e].wait_ge(self.semobj[sid], val)

    def _deps(self, e, reads, writes, extra):
        evs = []
        for k in reads:
            if k in self.lastw:
                evs.append(self.lastw[k])
        for k in writes:
            if k in self.lastw:
                evs.append(self.lastw[k])
            evs.extend(self.reads.get(k, []))
        evs.extend(extra)
        best = {}
        for ev in evs:
            sid, val, src = ev
            if src == "pe" and e == "pe":
                continue
            if sid not in best or best[sid][1] < val:
                best[sid] = ev
        for ev in best.values():
            self._wait(e, ev)

    def _record(self, ev, reads, writes):
        for k in reads:
            lst = self.reads.setdefault(k, [])
            lst.append(ev)
        for k in writes:
            self.lastw[k] = ev
            self.reads[k] = []

    def op(self, e, fn, reads=(), writes=(), extra=()):
        self._deps(e, reads, writes, extra)
        ins = fn()
        self.cnt[e] += 1
        ins.then_inc(self.sem[e], 1)
        ev = (("e", e), self.cnt[e], e)
        self._record(ev, reads, writes)
        return ev

    def dma(self, q, fn, reads=(), writes=(), extra=()):
        i = self.dnext
        self.dnext = (self.dnext + 1) % len(self.dsem)
        sid = ("d", i)
        if self.dcnt[i] > 0:
            self._wait(q, (sid, self.dcnt[i], "dma"))
        self._deps(q, reads, writes, extra)
        ins = fn()
        self.dcnt[i] += 16
        ins.then_inc(self.dsem[i], 16)
        ev = (sid, self.dcnt[i], "dma")
        self._record(ev, reads, writes)
        return ev

    def all_events(self):
        evs = []
        for i, c in enumerate(self.dcnt):
            if c:
                evs.append((("d", i), c, "dma"))
        for en in self.eng:
            if self.cnt[en]:
                evs.append((("e", en), self.cnt[en], "x"))
        return evs

    def barrier(self):
        evs = self.all_events()
        for e in self.eng:
            for ev in evs:
                self._wait(e, ev)
        self.lastw = {}
        self.reads = {}

    def finish(self, e="sp"):
        for ev in self.all_events():
            self._wait(e, ev)


def bcast_rows(ap, nparts=128):
    n = ap.shape[-1]
    return bass.AP(ap.tensor, ap.offset, [[0, nparts], [1, n]])


def rel_bucket_np(n):
    n = np.maximum(n, 0)
    nf = np.maximum(n, 1).astype(np.float32)
    large = 16 + (np.log(nf / np.float32(16)) / np.float32(math.log(128 / 16)) * np.float32(16)).astype(np.int32)
    large = np.minimum(large, 31)
    return np.where(n < 16, n, large)


def host_consts():
    i = np.arange(128)
    c = {}
    c["ident"] = np.eye(128, dtype=np.float32)
    c["antiI"] = np.eye(128, dtype=np.float32)[::-1].copy()
    c["tri"] = (i[:, None] < i[None, :]).astype(np.float32)
    c["ugt"] = (i[:, None] > i[None, :]).astype(np.float32)
    c["ones"] = np.ones((128, 128), np.float32)
    n = np.arange(640) - 256
    oh = np.zeros((32, 640), np.float32)
    valid = (n >= 0) & (n < 256)
    bk = rel_bucket_np(np.clip(n, 0, None))
    oh[bk[valid], np.nonzero(valid)[0]] = 1.0
    oh[31, valid] -= 1.0
    c["ohb"] = oh
    c["negrow"] = np.where(n < 0, NEG, 0.0).astype(np.float32)[None, :]
    c["iota128"] = np.tile(np.arange(128, dtype=np.float32)[None, :], (128, 1))
    c["thr"] = np.tile((128.0 * np.arange(16, dtype=np.float32))[None, :], (128, 1))
    c["pcol"] = np.arange(128, dtype=np.float32)[:, None].copy()
    return c


CONST_SHAPES = {"ident": [128, 128], "antiI": [128, 128], "tri": [128, 128], "ugt": [128, 128],
                "ones": [128, 128], "ohb": [32, 640], "negrow": [1, 640], "iota128": [128, 128],
                "thr": [128, 16], "pcol": [128, 1]}

WEIGHT_SHAPES = {
    "rel_bias_table": [32, 8], "w_ada": [D, 6 * D], "b_ada": [1, 6 * D], "g_mix_col": [128, 16],
    "w_in": [D, 10240], "lq1": [1, 64], "lk1": [1, 64], "lq2": [1, 64], "lk2": [1, 64],
    "g_subln": [1, 128], "w_proj_a": [1024, D], "w_proj_b": [1024, D], "w_out": [D, D],
    "g_ffn_col": [128, 16], "w_rg": [D, 8], "b_rg": [1, 8], "w_re": [D, 64], "b_re": [1, 64],
    "w_eg": [64 * D, 1024], "w_eu": [64 * D, 1024], "w_ed": [64 * 1024, D], "g_final": [1, D],
}


def build(stage=99, dbg=False):
    nc = bass.Bass("TRN2", target_bir_lowering=False)
    din = {}

    def dram_in(name, shape, dt=F32):
        din[name] = nc.dram_tensor(name, list(shape), dt, kind="ExternalInput").ap()
        return din[name]

    x_all = dram_in("x_all", [SEQ, D])
    x_own = dram_in("x_own", [NOWN, D])
    c_col = dram_in("c_col", [128, 16])
    halfv = dram_in("halfv", [128, 1])
    W = {k: dram_in(k, s) for k, s in WEIGHT_SHAPES.items() if stage >= 7 or k not in ("w_eg", "w_eu", "w_ed")}
    C = {k: dram_in("c_" + k, s) for k, s in CONST_SHAPES.items()}
    out_own = nc.dram_tensor("out_own", [NOWN, D], F32, kind="ExternalOutput").ap()
    dbg_out = {}

    def dbg_tensor(name, shape):
        dbg_out[name] = nc.dram_tensor("dbg_" + name, list(shape), F32, kind="ExternalOutput").ap()
        return dbg_out[name]

    tv_d = nc.dram_tensor("tv_scratch", [8, 640], F32, kind="Internal")
    mod_d = nc.dram_tensor("mod_scratch", [6, D], F32, kind="Internal").ap()
    x1_d = nc.dram_tensor("dbg_x1" if dbg else "x1_scratch", [NOWN, D], F32, kind="ExternalOutput" if dbg else "Internal").ap()

    with ExitStack() as st0:
        S = Sched(nc, st0)
        V, A_, P_, T_ = nc.vector, nc.scalar, nc.gpsimd, nc.tensor

        def sb(stack, name, shape, dt):
            return stack.enter_context(nc.sbuf_tensor(name, list(shape), dt))

        psb = [st0.enter_context(nc.psum_tensor("psb%d" % i, [128, 512], F32)) for i in range(8)]

        def ps_bf(i):
            return psb[i][:].bitcast(BF16)

        cs = {}
        for k in ("ident", "antiI", "tri", "ugt", "ones"):
            cs[k] = sb(st0, "k_" + k, [128, 128], F32)
            S.dma("sp", lambda k=k: nc.sync.dma_start(out=cs[k][:], in_=C[k][:, :]), writes=["c_" + k])
        identb = sb(st0, "identb", [128, 128], BF16)
        S.op("dve", lambda: V.tensor_copy(out=identb[:], in_=cs["ident"][:]), reads=["c_ident"], writes=["identb"])
        half_t = sb(st0, "half_t", [128, 1], F32)
        S.dma("sp", lambda: nc.sync.dma_start(out=half_t[:], in_=halfv[:, :]), writes=["half"])

        Gm_col = sb(st0, "Gm_col", [128, 16], F32)
        shm_col = sb(st0, "shm_col", [128, 16], F32)
        Gf_col = sb(st0, "Gf_col", [128, 16], F32)
        shf_col = sb(st0, "shf_col", [128, 16], F32)

        with ExitStack() as st:
            ccol = sb(st, "ccol", [128, 16], F32)
            scol = sb(st, "scol", [128, 16], F32)
            gmix = sb(st, "gmix", [128, 16], F32)
            S.dma("sp", lambda: nc.sync.dma_start(out=ccol[:], in_=c_col[:, :]), writes=["ccol"])
            S.dma("sp", lambda: nc.sync.dma_start(out=gmix[:], in_=W["g_mix_col"][:, :]), writes=["gmix"])
            gffn = sb(st, "gffn", [128, 16], F32)
            S.dma("sp", lambda: nc.sync.dma_start(out=gffn[:], in_=W["g_ffn_col"][:, :]), writes=["gffn"])
            S.op("act", lambda: A_.activation(out=scol[:], in_=ccol[:], func=AF.Silu), reads=["ccol"], writes=["scol"])
            wa = [sb(st, "wa%d" % i, [128, 16, 512], F32) for i in range(2)]
            brow = [sb(st, "brow%d" % i, [1, 512], F32) for i in range(2)]
            mrow = [sb(st, "mrow%d" % i, [1, 512], F32) for i in range(2)]
            one11 = sb(st, "one11", [1, 1], F32)
            S.op("dve", lambda: V.memset(one11[:], 1.0), writes=["one11"])
            w_ada_r = W["w_ada"].rearrange("(c p) n -> p c n", p=128)
            for j in range(24):
                m, jj = j // 4, j % 4
                b = j % 2
                for hh in range(2):
                    S.dma("sp", lambda b=b, j=j, hh=hh: nc.sync.dma_start(
                        out=wa[b][:, hh * 8:(hh + 1) * 8, :], in_=w_ada_r[:, hh * 8:(hh + 1) * 8, j * 512:(j + 1) * 512]),
                        writes=[("wa", b, hh)])
                S.dma("sp", lambda b=b, j=j: nc.sync.dma_start(out=brow[b][:], in_=W["b_ada"][0:1, j * 512:(j + 1) * 512]), writes=[("brow", b)])
                pb = psb[b]
                for c in range(16):
                    S.op("pe", lambda c=c, b=b, pb=pb: T_.matmul(pb[0:1, :], lhsT=scol[:, c:c + 1], rhs=wa[b][:, c, :], start=(c == 0), stop=(c == 15)),
                         reads=["scol", ("wa", b, c // 8)], writes=[("psb", b)])
                S.op("dve", lambda b=b, pb=pb: V.tensor_tensor(out=mrow[b][:], in0=pb[0:1, :], in1=brow[b][:], op=ALU.add),
                     reads=[("psb", b), ("brow", b)], writes=[("mrow", b)])
                if m in (0, 1, 3, 4):
                    pc = psb[2 + b]
                    for q in range(4):
                        S.op("pe", lambda q=q, b=b, pc=pc: T_.matmul(pc[:, q:q + 1], lhsT=mrow[b][0:1, q * 128:(q + 1) * 128], rhs=one11[0:1, 0:1], start=True, stop=True),
                             reads=[("mrow", b), "one11"], writes=[("psb", 2 + b)])
                    dst = {0: shm_col, 1: Gm_col, 3: shf_col, 4: Gf_col}[m]
                    key = {0: "shm_col", 1: "Gm_col", 3: "shf_col", 4: "Gf_col"}[m]
                    S.op("dve", lambda b=b, pc=pc, dst=dst, jj=jj: V.tensor_copy(out=dst[:, jj * 4:(jj + 1) * 4], in_=pc[:, 0:4]),
                         reads=[("psb", 2 + b)], writes=[key])
                S.dma("sp", lambda b=b, m=m, jj=jj: nc.sync.dma_start(out=mod_d[m:m + 1, jj * 512:(jj + 1) * 512], in_=mrow[b][:]),
                      reads=[("mrow", b)], writes=[("mod_d", j)])
            S.op("dve", lambda: V.scalar_tensor_tensor(out=Gm_col[:], in0=Gm_col[:], scalar=1.0, in1=gmix[:], op0=ALU.add, op1=ALU.mult),
                 reads=["Gm_col", "gmix"], writes=["Gm_col"])
            S.op("dve", lambda: V.scalar_tensor_tensor(out=Gf_col[:], in0=Gf_col[:], scalar=1.0, in1=gffn[:], op0=ALU.add, op1=ALU.mult),
                 reads=["Gf_col", "gffn"], writes=["Gf_col"])
            if dbg:
                d = dbg_tensor("Gm_col", [128, 16]); S.dma("sp", lambda: nc.sync.dma_start(out=d[:, :], in_=Gm_col[:]), reads=["Gm_col"])
                d2 = dbg_tensor("shm_col", [128, 16]); S.dma("sp", lambda: nc.sync.dma_start(out=d2[:, :], in_=shm_col[:]), reads=["shm_col"])
            S.barrier()
        if stage <= 0:
            S.finish("sp")
            return nc, dbg_out

        stR = ExitStack()
        stA = ExitStack()
        eps_t = sb(st0, "eps_t", [128, 1], F32)
        S.op("dve", lambda: V.memset(eps_t[:], EPS), writes=["eps"])
        hT_own = sb(stA, "hT_own", [128, 16, NOWN], BF16)
        oaT = sb(stA, "oaT", [128, NH, NOWN], BF16)
        obT = sb(stA, "obT", [128, NH, NOWN], BF16)

        def norm_tile_to_hT(stk_bufs, src_ap, dstT, col0, tag):
            xt, xb, ss, rs, junk = stk_bufs
            for hh in range(2):
                S.dma("sp", lambda hh=hh: nc.sync.dma_start(out=xt[:, hh * 1024:(hh + 1) * 1024], in_=src_ap[:, hh * 1024:(hh + 1) * 1024]), writes=[(tag, "xt", hh)])
            S.op("act", lambda: A_.activation(out=junk[:], in_=xt[:], func=AF.Square, accum_out=ss[:]),
                 reads=[(tag, "xt", 0), (tag, "xt", 1)], writes=[(tag, "ss"), (tag, "junk")])
            S.op("act", lambda: A_.activation(out=rs[:], in_=ss[:], func=AF.Sqrt, bias=eps_t[:], scale=1.0 / D),
                 reads=[(tag, "ss"), "eps"], writes=[(tag, "rs")])
            S.op("dve", lambda: V.reciprocal(out=rs[:], in_=rs[:]), reads=[(tag, "rs")], writes=[(tag, "rs")])
            S.op("dve", lambda: V.tensor_scalar(out=xb[:], in0=xt[:], scalar1=rs[:, 0:1], scalar2=None, op0=ALU.mult),
                 reads=[(tag, "xt", 0), (tag, "xt", 1), (tag, "rs")], writes=[(tag, "xb")])
            for g in range(2):
                pt = ps_bf(6 + g)
                for c8 in range(8):
                    c = g * 8 + c8
                    S.op("pe", lambda c=c, c8=c8, pt=pt: T_.transpose(pt[:, c8 * 128:(c8 + 1) * 128], xb[:, c * 128:(c + 1) * 128], identb[:]),
                         reads=[(tag, "xb"), "identb"], writes=[("psb", 6 + g)])
                for c8 in range(8):
                    c = g * 8 + c8
                    S.op("act", lambda c=c, c8=c8, pt=pt: A_.activation(out=dstT[:, c, col0:col0 + 128], in_=pt[:, c8 * 128:(c8 + 1) * 128],
                                                                      func=AF.Identity, bias=shm_col[:, c:c + 1], scale=Gm_col[:, c:c + 1]),
                         reads=[("psb", 6 + g), "shm_col", "Gm_col"], writes=[(tag, "hT", col0 // 512)])

        with ExitStack() as stB:
            hT_all = sb(stB, "hT_all", [128, 16, SEQ], BF16)
            with ExitStack() as st:
                bufs = []
                for i in range(2):
                    bufs.append((sb(st, "xt%d" % i, [128, D], F32), sb(st, "xb%d" % i, [128, D], BF16),
                                 sb(st, "ss%d" % i, [128, 1], F32), sb(st, "rs%d" % i, [128, 1], F32),
                                 sb(st, "junk%d" % i, [128, D], BF16)))
                for t in range(16):
                    norm_tile_to_hT(bufs[t % 2], x_all[t * 128:(t + 1) * 128, :], hT_all, t * 128, ("n", t % 2))
                S.barrier()
                for t in range(8):
                    norm_tile_to_hT(bufs[t % 2], x_own[t * 128:(t + 1) * 128, :], hT_own, t * 128, ("n", t % 2))
                S.barrier()
            attention_phase(nc, S, stB, sb, psb, ps_bf, cs, identb, half_t, W, C, tv_d, hT_all, hT_own, oaT, obT, False, dbg_tensor)
            S.barrier()
        mergedT = stR.enter_context(nc.sbuf_tensor("mergedT", [128, 16, NOWN], BF16, side="right"))
        merge_phase(nc, S, sb, psb, W, hT_own, oaT, obT, mergedT)
        S.barrier()
        stA.close()
        h2T = sb(st0, "h2T", [128, 16, NOWN], BF16)
        Gt = sb(st0, "Gt", [128, 8, 64], F32)
        resid_phase(nc, S, sb, psb, W, mergedT, x_own, x1_d, mod_d)
        S.barrier()
        stR.close()
        stW = ExitStack()
        moe_bufs = None
        if stage > 5:
            moe_bufs = moe_weight_bufs(nc, stW, side="right")
            moe_prefetch(nc, S, W, *moe_bufs)
        norm_router_phase(nc, S, sb, psb, W, cs, x1_d, h2T, Gt, Gf_col, shf_col, eps_t)
        S.barrier()
        if dbg:
            d = dbg_tensor("Gt", [128, 8 * 64])
            S.dma("sp", lambda: nc.sync.dma_start(out=d[:, :], in_=Gt[:].rearrange("p a b -> p (a b)")))
            d2 = dbg_tensor("h2T", [128, 16 * NOWN])
            with ExitStack() as st:
                tmpf = sb(st, "dbgtmp", [128, NOWN], F32)
                for c in range(16):
                    S.op("dve", lambda c=c: V.tensor_copy(out=tmpf[:], in_=h2T[:, c, :]), writes=["dbgtmp"])
                    S.dma("sp", lambda c=c: nc.sync.dma_start(out=d2[:, c * NOWN:(c + 1) * NOWN], in_=tmpf[:]), reads=["dbgtmp"])
                S.barrier()
        if stage <= 4:
            S.finish("sp")
            return nc, dbg_out
        acc = sb(st0, "acc", [128, 8, D], F32)
        if stage == 5:
            for t in range(8):
                S.op("dve", lambda: V.memset(acc[:, t, :], 0.0), writes=[("acc", t)])
        else:
            moe_phase(nc, S, sb, psb, W, h2T, Gt, acc, bufs=moe_bufs, prefetched=True)
        S.barrier()
        stW.close()
        final_phase(nc, S, sb, W, acc, x1_d, mod_d, out_own, eps_t)
        S.finish("sp")
    return nc, dbg_out


def attention_phase(nc, S, stB, sb, psb, ps_bf, cs, identb, half_t, W, C, tv_d, hT_all, hT_own, oaT, obT, dbg, dbg_tensor):
    V, A_, P_, T_ = nc.vector, nc.scalar, nc.gpsimd, nc.tensor
    with ExitStack() as st:
        tab = sb(st, "tab", [32, 8], F32)
        ohb = sb(st, "ohb", [32, 640], F32)
        negrow = sb(st, "negrow", [1, 640], F32)
        tvs = sb(st, "tvs", [8, 640], F32)
        b31 = sb(st, "b31", [128, 8], F32)
        S.dma("sp", lambda: nc.sync.dma_start(out=tab[:], in_=W["rel_bias_table"][:, :]), writes=["tab"])
        S.dma("sp", lambda: nc.sync.dma_start(out=ohb[:], in_=C["ohb"][:, :]), writes=["ohb"])
        S.dma("sp", lambda: nc.sync.dma_start(out=negrow[:], in_=C["negrow"][:, :]), writes=["negrow"])
        S.dma("sp", lambda: nc.sync.dma_start(out=b31[:], in_=bcast_rows(W["rel_bias_table"][31:32, :])), writes=["b31"])
        for half in range(2):
            pb = psb[half]
            S.op("pe", lambda half=half, pb=pb: T_.matmul(pb[0:8, 0:320], lhsT=tab[:, :], rhs=ohb[:, half * 320:(half + 1) * 320], start=True, stop=False),
                 reads=["tab", "ohb"], writes=[("psb", half)])
            S.op("pe", lambda half=half, pb=pb: T_.matmul(pb[0:8, 0:320], lhsT=cs["ones"][0:1, 0:8], rhs=negrow[0:1, half * 320:(half + 1) * 320], start=False, stop=True),
                 reads=["c_ones", "negrow"], writes=[("psb", half)])
            S.op("act", lambda half=half, pb=pb: A_.mul(out=tvs[:, half * 320:(half + 1) * 320], in_=pb[0:8, 0:320], mul=8.0),
                 reads=[("psb", half)], writes=["tvs"])
        S.dma("sp", lambda: nc.sync.dma_start(out=tv_d.ap()[:, :], in_=tvs[:]), reads=["tvs"], writes=["tv_d"])

        lam4 = sb(st, "lam4", [128, 4, 64], F32)
        for i, nm in enumerate(("lq1", "lk1", "lq2", "lk2")):
            S.dma("sp", lambda i=i, nm=nm: nc.sync.dma_start(out=lam4[:, i, :], in_=W[nm][0:1, :].partition_broadcast(128)), writes=[("lam4", i)])
        lsum = sb(st, "lsum", [128, 2], F32)
        ljunk = sb(st, "ljunk", [128, 64], F32)
        for i in range(2):
            S.op("dve", lambda i=i: V.tensor_tensor(out=ljunk[:], in0=lam4[:, 2 * i, :], in1=lam4[:, 2 * i + 1, :], op=ALU.mult),
                 reads=[("lam4", 2 * i), ("lam4", 2 * i + 1)], writes=["ljunk"])
            S.op("dve", lambda i=i: V.tensor_reduce(out=lsum[:, i:i + 1], in_=ljunk[:], axis=AX.X, op=ALU.add),
                 reads=["ljunk"], writes=[("lsum", i)])
        S.op("act", lambda: A_.activation(out=lsum[:], in_=lsum[:], func=AF.Exp), reads=[("lsum", 0), ("lsum", 1)], writes=["lsume"])
        nlam = sb(st, "nlam", [128, 1], F32)
        S.op("dve", lambda: V.tensor_tensor(out=nlam[:], in0=lsum[:, 1:2], in1=lsum[:, 0:1], op=ALU.subtract), reads=["lsume"], writes=["nlam"])
        S.op("dve", lambda: V.tensor_scalar(out=nlam[:], in0=nlam[:], scalar1=-LAM_INIT, scalar2=None, op0=ALU.add), reads=["nlam"], writes=["nlam"])
        gs8 = sb(st, "gs8", [128, 128], F32)
        S.dma("sp", lambda: nc.sync.dma_start(out=gs8[:], in_=bcast_rows(W["g_subln"][0:1, :])), writes=["gs8"])
        S.op("dve", lambda: V.tensor_scalar(out=gs8[:], in0=gs8[:], scalar1=(1.0 - LAM_INIT), scalar2=None, op0=ALU.mult), reads=["gs8"], writes=["gs8"])
        eps_t = sb(st, "eps_t2", [128, 1], F32)
        S.op("dve", lambda: V.memset(eps_t[:], EPS), writes=["eps2"])

        tri, ones = cs["tri"], cs["ones"]
        omt = sb(st, "omt", [128, 128], F32)
        S.op("dve", lambda: V.tensor_tensor(out=omt[:], in0=ones[:], in1=tri[:], op=ALU.subtract), reads=["c_ones", "c_tri"], writes=["omt"])
        mk = {n: sb(st, "mk_" + n, [128, 128], F32) for n in ("XB", "XC", "YB", "YC")}
        omh = sb(st, "omh", [128, 1], F32)
        S.op("dve", lambda: V.tensor_scalar(out=omh[:], in0=half_t[:], scalar1=-1.0, scalar2=1.0, op0=ALU.mult, op1=ALU.add), reads=["half"], writes=["omh"])
        S.op("dve", lambda: V.scalar_tensor_tensor(out=mk["XB"][:], in0=omt[:], scalar=half_t[:, 0:1], in1=tri[:], op0=ALU.mult, op1=ALU.add),
             reads=["omt", "half", "c_tri"], writes=["mk_XB"])
        S.op("dve", lambda: V.tensor_scalar(out=mk["XC"][:], in0=tri[:], scalar1=half_t[:, 0:1], scalar2=None, op0=ALU.mult), reads=["c_tri", "half"], writes=["mk_XC"])
        S.op("dve", lambda: V.scalar_tensor_tensor(out=mk["YB"][:], in0=omt[:], scalar=omh[:, 0:1], in1=tri[:], op0=ALU.mult, op1=ALU.add),
             reads=["omt", "omh", "c_tri"], writes=["mk_YB"])
        S.op("dve", lambda: V.tensor_scalar(out=mk["YC"][:], in0=tri[:], scalar1=omh[:, 0:1], scalar2=None, op0=ALU.mult), reads=["c_tri", "omh"], writes=["mk_YC"])

        wq = [sb(st, "wq%d" % i, [128, 16, 128], BF16) for i in range(3)]
        qT = sb(st, "qT", [128, NOWN], BF16)
        kT = sb(st, "kT", [128, SEQ], BF16)
        vA = sb(st, "vA", [128, 16, 130], BF16)
        Ht = [sb(st, "Ht%d" % i, [128, 128], F32) for i in range(4)]
        slot = {n: sb(st, "slot" + n, [128, 128], F32) for n in ("XA", "XB", "XC", "YA", "YB", "YC")}
        hd = sb(st, "hd", [128, 128], F32)
        PT = [sb(st, "PT%d" % i, [128, 128], BF16) for i in range(4)]
        e_t = [sb(st, "e_t%d" % i, [128, 128], F32) for i in range(2)]
        lnp = [sb(st, "lnp%d" % i, [128, 128], F32) for i in range(2)]
        LK = [sb(st, "LK%d" % i, [128, 128], F32) for i in range(2)]
        arg = [sb(st, "arg%d" % i, [128, 128], F32) for i in range(2)]
        Acc = sb(st, "Acc", [128, 128], F32)
        o1 = sb(st, "o1", [128, 128], F32)
        o2 = sb(st, "o2", [128, 128], F32)
        obf = sb(st, "obf", [128, 128], BF16)
        rr = sb(st, "rr", [128, 4], F32)
        sjunk = sb(st, "sjunk", [128, 128], F32)
        S.op("dve", lambda: V.memset(vA[:], 1.0), writes=["vA_init"])

        w_in_r = W["w_in"].rearrange("(c p) n -> p c n", p=128)
        SC_A = 64 ** -0.5
        SC_B = 128 ** -0.5

        def own_tile_info(j):
            p = j // 2
            if j % 2 == 0:
                return 2 * p + 2, "X", (2 * p - 1, 2 * p, 2 * p + 1)
            return 16 - 2 * p, "Y", (13 - 2 * p, 14 - 2 * p, 15 - 2 * p)

        def project_head(h, colbase, with_ones):
            for i, off in enumerate((0, 1024, 2048)):
                c0 = colbase + off + h * 128
                for hh in range(2):
                    S.dma("pool", lambda i=i, c0=c0, hh=hh: nc.gpsimd.dma_start(out=wq[i][:, hh * 8:(hh + 1) * 8, :], in_=w_in_r[:, hh * 8:(hh + 1) * 8, c0:c0 + 128]),
                          writes=[("wq", i, hh)])
            n = 0
            for ch in range(2):
                pb = psb[n % 2]; n += 1
                for c in range(16):
                    S.op("pe", lambda c=c, ch=ch, pb=pb: T_.matmul(pb[:, :], lhsT=wq[0][:, c, :], rhs=hT_own[:, c, ch * 512:(ch + 1) * 512], start=(c == 0), stop=(c == 15)),
                         reads=[("wq", 0, c // 8)], writes=[("psb", (n - 1) % 2)])
                S.op("act", lambda ch=ch, pb=pb: A_.copy(out=qT[:, ch * 512:(ch + 1) * 512], in_=pb[:, :]), reads=[("psb", (n - 1) % 2)], writes=[("qT", ch)])
            for ch in range(4):
                pb = psb[n % 2]; n += 1
                for c in range(16):
                    S.op("pe", lambda c=c, ch=ch, pb=pb: T_.matmul(pb[:, :], lhsT=wq[1][:, c, :], rhs=hT_all[:, c, ch * 512:(ch + 1) * 512], start=(c == 0), stop=(c == 15)),
                         reads=[("wq", 1, c // 8)], writes=[("psb", (n - 1) % 2)])
                S.op("dve", lambda ch=ch, pb=pb: V.tensor_copy(out=kT[:, ch * 512:(ch + 1) * 512], in_=pb[:, :]), reads=[("psb", (n - 1) % 2)], writes=[("kT", ch)])
            for kb4 in range(4):
                pb = psb[n % 2]; n += 1
                for k4 in range(4):
                    kb = kb4 * 4 + k4
                    for c in range(16):
                        S.op("pe", lambda c=c, kb=kb, k4=k4, pb=pb: T_.matmul(pb[:, k4 * 128:(k4 + 1) * 128], lhsT=hT_all[:, c, kb * 128:(kb + 1) * 128], rhs=wq[2][:, c, :], start=(c == 0), stop=(c == 15)),
                             reads=[("wq", 2, c // 8)], writes=[("psb", (n - 1) % 2)])
                eng = "act" if kb4 % 2 == 0 else "dve"
                if eng == "act":
                    S.op("act", lambda kb4=kb4, pb=pb: A_.copy(out=vA[:, kb4 * 4:(kb4 + 1) * 4, 0:128], in_=pb[:, :].rearrange("p (a b) -> p a b", a=4)),
                         reads=[("psb", (n - 1) % 2), "vA_init"], writes=[("vA", kb4)])
                else:
                    S.op("dve", lambda kb4=kb4, pb=pb: V.tensor_copy(out=vA[:, kb4 * 4:(kb4 + 1) * 4, 0:128], in_=pb[:, :].rearrange("p (a b) -> p a b", a=4)),
                         reads=[("psb", (n - 1) % 2), "vA_init"], writes=[("vA", kb4)])

        def qk_keys(j, kb):
            return [("qT", j // 4), ("kT", kb // 4)]

        pend = [None]
        pendB = [None]
        for h in range(NH):
            project_head(h, 0, True)
            for i, dl in enumerate((-128, 0, 128, 256)):
                src = bass.AP(tv_d, h * 640 + 129 + dl, [[1, 128], [1, 128]])
                S.dma("sp", lambda i=i, src=src: nc.sync.dma_start(out=Ht[i][:], in_=src), reads=["tv_d"], writes=[("Ht", i)])
            for nm, lo, hi in (("XA", 2, 3), ("XB", 1, 2), ("XC", 0, 1), ("YA", 3, 2), ("YB", 2, 1), ("YC", 1, 0)):
                S.op("dve", lambda lo=lo, hi=hi: V.tensor_tensor(out=hd[:], in0=Ht[hi][:], in1=Ht[lo][:], op=ALU.subtract),
                     reads=[("Ht", lo), ("Ht", hi)], writes=["hd"])
                S.op("dve", lambda nm=nm, lo=lo: V.scalar_tensor_tensor(out=slot[nm][:], in0=hd[:], scalar=half_t[:, 0:1], in1=Ht[lo][:], op0=ALU.mult, op1=ALU.add),
                     reads=["hd", "half", ("Ht", lo)], writes=[("slot", nm)])
            git = 0
            for j in range(8):
                L, sset, slots = own_tile_info(j)
                obase = 4 if j % 2 == 0 else 0
                steps = [(m, kb) for m in range(2) for kb in range(L)]

                def da_front(i):
                    m, kb = steps[i]
                    rows = slice(64 * m, 64 * m + 64)
                    g = git + i
                    sbk = 2 + (g % 2)
                    pt = PT[g % 4]
                    sl = None
                    if kb in slots:
                        sl = sset + "ABC"[slots.index(kb)]
                    S.op("pe", lambda: T_.matmul(psb[sbk][:, 0:128], lhsT=kT[rows, kb * 128:(kb + 1) * 128], rhs=qT[rows, j * 128:(j + 1) * 128], start=True, stop=(sl is None)),
                         reads=qk_keys(j, kb), writes=[("psb", sbk)])
                    if sl is not None:
                        S.op("pe", lambda: T_.matmul(psb[sbk][:, 0:128], lhsT=cs["antiI"][:], rhs=slot[sl][:], start=False, stop=True),
                             reads=["c_antiI", ("slot", sl)], writes=[("psb", sbk)])
                    S.op("act", lambda: A_.activation(out=pt[:], in_=psb[sbk][:, 0:128], func=AF.Exp, bias=b31[:, h:h + 1], scale=SC_A),
                         reads=[("psb", sbk), "b31"], writes=[("PT", g % 4)])

                def da_back(i):
                    m, kb = steps[i]
                    g = git + i
                    ob = obase + m
                    pt = PT[g % 4]
                    S.op("pe", lambda: T_.matmul(psb[ob][:, 0:130], lhsT=pt[:], rhs=vA[:, kb, :], start=(kb == 0), stop=(kb == L - 1)),
                         reads=[("PT", g % 4), ("vA", kb // 4)], writes=[("psb", ob)])

                for i in range(len(steps)):
                    da_front(i)
                    if i >= 1:
                        da_back(i - 1)
                    if i == 2 and pendB[0] is not None:
                        pendB[0]()
                        pendB[0] = None
                    if i == min(5, len(steps) - 1) and pend[0] is not None:
                        pend[0]()
                        pend[0] = None
                da_back(len(steps) - 1)
                git += len(steps)
                o1b, o2b = obase, obase + 1
                S.op("dve", lambda: V.reciprocal(out=rr[:, 0:1], in_=psb[o1b][:, 128:129]), reads=[("psb", o1b)], writes=["rr0"])
                S.op("dve", lambda: V.reciprocal(out=rr[:, 1:2], in_=psb[o2b][:, 128:129]), reads=[("psb", o2b)], writes=["rr1"])
                S.op("dve", lambda: V.tensor_tensor(out=rr[:, 1:2], in0=rr[:, 1:2], in1=nlam[:], op=ALU.mult), reads=["rr1", "nlam"], writes=["rr1"])
                S.op("dve", lambda: V.tensor_scalar(out=o1[:], in0=psb[o1b][:, 0:128], scalar1=rr[:, 0:1], scalar2=None, op0=ALU.mult), reads=[("psb", o1b), "rr0"], writes=["o1"])
                S.op("dve", lambda: V.scalar_tensor_tensor(out=o2[:], in0=psb[o2b][:, 0:128], scalar=rr[:, 1:2], in1=o1[:], op0=ALU.mult, op1=ALU.add),
                     reads=[("psb", o2b), "rr1", "o1"], writes=["o2"])
                S.op("dve", lambda: V.tensor_tensor(out=sjunk[:], in0=o2[:], in1=o2[:], op=ALU.mult), reads=["o2"], writes=["sjunk"])
                S.op("dve", lambda: V.tensor_reduce(out=rr[:, 2:3], in_=sjunk[:], axis=AX.X, op=ALU.add), reads=["sjunk"], writes=["rr2"])

                def ln_a():
                    S.op("act", lambda: A_.activation(out=rr[:, 3:4], in_=rr[:, 2:3], func=AF.Ln, bias=eps_t[:], scale=1.0 / 128), reads=["rr2", "eps2"], writes=["rr3"])
                    S.op("act", lambda: A_.activation(out=rr[:, 2:3], in_=rr[:, 3:4], func=AF.Exp, scale=-0.5), reads=["rr3"], writes=["rr2"])
                    S.op("dve", lambda: V.scalar_tensor_tensor(out=obf[:], in0=o2[:], scalar=rr[:, 2:3], in1=gs8[:], op0=ALU.mult, op1=ALU.mult),
                         reads=["o2", "rr2", "gs8"], writes=["obf"])
                pendB[0] = ln_a

                def tr_a(h=h, j=j):
                    ptb = ps_bf(6)
                    S.op("pe", lambda: T_.transpose(ptb[:, 0:128], obf[:], identb[:]), reads=["obf", "identb"], writes=[("psb", 6)])
                    S.op("act", lambda: A_.copy(out=oaT[:, h, j * 128:(j + 1) * 128], in_=ptb[:, 0:128]), reads=[("psb", 6)], writes=[("oaT", h)])
                pend[0] = tr_a
            pendB[0]()
            pendB[0] = None
            pend[0]()
            pend[0] = None

        for h in range(NH):
            project_head(h, 3072, False)
            git = 0
            for j in range(8):
                L, sset, slots = own_tile_info(j)
                kbs = list(range(L - 1, -1, -1))
                ab = 4 if j % 2 == 0 else 0

                def sb_mask(kb):
                    if kb in slots:
                        nm = sset + "ABC"[slots.index(kb)]
                        if nm[1] != "A":
                            return nm
                    return None

                def sb_front(i):
                    kb = kbs[i]
                    g = git + i
                    zb = 2 + (g % 2)
                    b2 = g % 2
                    mkt = sb_mask(kb)
                    S.op("pe", lambda: T_.matmul(psb[zb][:, 0:128], lhsT=kT[:, kb * 128:(kb + 1) * 128], rhs=qT[:, j * 128:(j + 1) * 128], start=True, stop=True),
                         reads=qk_keys(j, kb), writes=[("psb", zb)])
                    S.op("act", lambda: A_.activation(out=e_t[b2][:], in_=psb[zb][:, 0:128], func=AF.Exp, scale=-SC_B), reads=[("psb", zb)], writes=[("e_t", b2)])
                    S.op("act", lambda: A_.activation(out=lnp[b2][:], in_=e_t[b2][:], func=AF.Ln, bias=1.0, scale=1.0), reads=[("e_t", b2)], writes=[("lnp", b2)])
                    S.op("dve", lambda: V.scalar_tensor_tensor(out=LK[b2][:], in0=psb[zb][:, 0:128], scalar=-SC_B, in1=lnp[b2][:], op0=ALU.mult, op1=ALU.subtract),
                         reads=[("psb", zb), ("lnp", b2)], writes=[("LK", b2)])
                    if mkt is not None:
                        S.op("pool", lambda: P_.tensor_tensor(out=LK[b2][:], in0=LK[b2][:], in1=mk[mkt][:], op=ALU.mult),
                             reads=[("LK", b2), "mk_" + mkt], writes=[("LK", b2)])

                def sb_mid(i):
                    kb = kbs[i]
                    g = git + i
                    b2 = g % 2
                    lb = 5 if g % 2 == 0 else 7
                    pt = PT[g % 4]
                    first = (i == 0)
                    mkt = sb_mask(kb)
                    S.op("pe", lambda: T_.matmul(psb[lb][:, 0:128], lhsT=cs["ugt"][:], rhs=LK[b2][:], start=True, stop=first),
                         reads=["c_ugt", ("LK", b2)], writes=[("psb", lb)])
                    if not first:
                        S.op("pe", lambda: T_.matmul(psb[lb][:, 0:128], lhsT=cs["ones"][:], rhs=Acc[:], start=False, stop=True),
                             reads=["c_ones", "Acc"], writes=[("psb", lb)])
                    if kb > 0:
                        if first:
                            S.op("pool", lambda: P_.tensor_copy(out=Acc[:], in_=LK[b2][:]), reads=[("LK", b2)], writes=["Acc"])
                        else:
                            S.op("pool", lambda: P_.tensor_tensor(out=Acc[:], in0=Acc[:], in1=LK[b2][:], op=ALU.add), reads=[("LK", b2), "Acc"], writes=["Acc"])
                    S.op("dve", lambda: V.tensor_tensor(out=arg[b2][:], in0=psb[lb][:, 0:128], in1=lnp[b2][:], op=ALU.subtract),
                         reads=[("psb", lb), ("lnp", b2)], writes=[("arg", b2)])
                    S.op("act", lambda: A_.activation(out=pt[:], in_=arg[b2][:], func=AF.Exp), reads=[("arg", b2)], writes=[("PT", g % 4)])
                    if mkt is not None:
                        S.op("pool", lambda: P_.tensor_tensor(out=pt[:], in0=pt[:], in1=mk[mkt][:], op=ALU.mult),
                             reads=[("PT", g % 4), "mk_" + mkt], writes=[("PT", g % 4)])

                def sb_av(i):
                    kb = kbs[i]
                    g = git + i
                    pt = PT[g % 4]
                    S.op("pe", lambda: T_.matmul(psb[ab][:, 0:128], lhsT=pt[:], rhs=vA[:, kb, 0:128], start=(i == 0), stop=(kb == 0)),
                         reads=[("PT", g % 4), ("vA", kb // 4)], writes=[("psb", ab)])

                n_it = len(kbs)
                for i in range(n_it):
                    sb_front(i)
                    if i >= 1:
                        sb_mid(i - 1)
                    if i >= 2:
                        sb_av(i - 2)
                    if i == 1 and pend[0] is not None:
                        pend[0]()
                        pend[0] = None
                sb_mid(n_it - 1)
                if n_it >= 2:
                    sb_av(n_it - 2)
                sb_av(n_it - 1)
                git += len(kbs)
                S.op("dve", lambda: V.tensor_copy(out=obf[:], in_=psb[ab][:, 0:128]), reads=[("psb", ab)], writes=["obf"])
                def tr_b(h=h, j=j):
                    ptb = ps_bf(6)
                    S.op("pe", lambda: T_.transpose(ptb[:, 0:128], obf[:], identb[:]), reads=["obf", "identb"], writes=[("psb", 6)])
                    S.op("act", lambda: A_.copy(out=obT[:, h, j * 128:(j + 1) * 128], in_=ptb[:, 0:128]), reads=[("psb", 6)], writes=[("obT", h)])
                pend[0] = tr_b
            pend[0]()
            pend[0] = None
        if dbg:
            for nm, tt in (("oaT", oaT), ("obT", obT)):
                d = dbg_tensor(nm, [128, NH * NOWN])
                tmp = sb(st, "dtmp_" + nm, [128, NOWN], F32)
                for h in range(NH):
                    S.op("dve", lambda h=h, tt=tt, tmp=tmp: V.tensor_copy(out=tmp[:], in_=tt[:, h, :]), reads=[(nm, h)], writes=["dbg_" + nm])
                    S.dma("sp", lambda h=h, d=d, tmp=tmp: nc.sync.dma_start(out=d[:, h * NOWN:(h + 1) * NOWN], in_=tmp[:]), reads=["dbg_" + nm])


def merge_phase(nc, S, sb, psb, W, hT_own, oaT, obT, mergedT):
    V, A_, P_, T_ = nc.vector, nc.scalar, nc.gpsimd, nc.tensor
    with ExitStack() as st:
        wpa = [sb(st, "wpa%d" % i, [128, 8, 128], BF16) for i in range(3)]
        wpb = [sb(st, "wpb%d" % i, [128, 8, 128], BF16) for i in range(3)]
        wga = [sb(st, "wga%d" % i, [128, 16, 128], BF16) for i in range(3)]
        wgb = [sb(st, "wgb%d" % i, [128, 16, 128], BF16) for i in range(3)]
        sg = [[sb(st, "sg%d_%d" % (i, j), [128, 512], F32) for j in range(2)] for i in range(2)]
        tt = [[sb(st, "tt%d_%d" % (i, j), [128, 512], F32) for j in range(2)] for i in range(2)]
        wpa_r = W["w_proj_a"].rearrange("(k p) n -> p k n", p=128)
        wpb_r = W["w_proj_b"].rearrange("(k p) n -> p k n", p=128)
        w_in_r = W["w_in"].rearrange("(c p) n -> p c n", p=128)
        it = 0
        for m in range(16):
            b = m % 3
            cs_ = slice(m * 128, (m + 1) * 128)
            S.dma("pool", lambda: nc.gpsimd.dma_start(out=wpa[b][:], in_=wpa_r[:, :, cs_]), writes=[("wpa", b)])
            S.dma("pool", lambda: nc.gpsimd.dma_start(out=wpb[b][:], in_=wpb_r[:, :, cs_]), writes=[("wpb", b)])
            for hh in range(2):
                S.dma("pool", lambda: nc.gpsimd.dma_start(out=wga[b][:, hh * 8:(hh + 1) * 8, :], in_=w_in_r[:, hh * 8:(hh + 1) * 8, 6144 + m * 128:6144 + (m + 1) * 128]),
                      writes=[("wga", b, hh)])
                S.dma("pool", lambda: nc.gpsimd.dma_start(out=wgb[b][:, hh * 8:(hh + 1) * 8, :], in_=w_in_r[:, hh * 8:(hh + 1) * 8, 8192 + m * 128:8192 + (m + 1) * 128]),
                      writes=[("wgb", b, hh)])
            for th in range(2):
                base = 4 * (it % 2)
                q = it % 2
                it += 1
                tok = slice(th * 512, (th + 1) * 512)
                for k in range(8):
                    S.op("pe", lambda: T_.matmul(psb[base][:, :], lhsT=wpa[b][:, k, :], rhs=oaT[:, k, tok], start=(k == 0), stop=(k == 7)),
                         reads=[("wpa", b)], writes=[("psb", base)])
                for k in range(8):
                    S.op("pe", lambda: T_.matmul(psb[base + 1][:, :], lhsT=wpb[b][:, k, :], rhs=obT[:, k, tok], start=(k == 0), stop=(k == 7)),
                         reads=[("wpb", b)], writes=[("psb", base + 1)])
                for c in range(16):
                    S.op("pe", lambda: T_.matmul(psb[base + 2][:, :], lhsT=wga[b][:, c, :], rhs=hT_own[:, c, tok], start=(c == 0), stop=(c == 15)),
                         reads=[("wga", b, c // 8)], writes=[("psb", base + 2)])
                for c in range(16):
                    S.op("pe", lambda: T_.matmul(psb[base + 3][:, :], lhsT=wgb[b][:, c, :], rhs=hT_own[:, c, tok], start=(c == 0), stop=(c == 15)),
                         reads=[("wgb", b, c // 8)], writes=[("psb", base + 3)])
                S.op("act", lambda: A_.activation(out=sg[q][0][:], in_=psb[base + 2][:, :], func=AF.Sigmoid), reads=[("psb", base + 2)], writes=[("sg", q, 0)])
                S.op("act", lambda: A_.activation(out=sg[q][1][:], in_=psb[base + 3][:, :], func=AF.Sigmoid), reads=[("psb", base + 3)], writes=[("sg", q, 1)])
                S.op("dve", lambda: V.tensor_tensor(out=tt[q][0][:], in0=psb[base][:, :], in1=sg[q][0][:], op=ALU.mult),
                     reads=[("psb", base), ("sg", q, 0)], writes=[("tt", q, 0)])
                S.op("dve", lambda: V.tensor_tensor(out=tt[q][1][:], in0=psb[base + 1][:, :], in1=sg[q][1][:], op=ALU.mult),
                     reads=[("psb", base + 1), ("sg", q, 1)], writes=[("tt", q, 1)])
                S.op("pool", lambda: P_.tensor_tensor(out=mergedT[:, m, tok], in0=tt[q][0][:], in1=tt[q][1][:], op=ALU.add),
                     reads=[("tt", q, 0), ("tt", q, 1)], writes=[("mergedT", m, th)])
        S.barrier()


def resid_phase(nc, S, sb, psb, W, mergedT, x_own, x1_d, mod_d):
    V, A_, P_, T_ = nc.vector, nc.scalar, nc.gpsimd, nc.tensor
    with ExitStack() as st:
        wo = [sb(st, "wo%d" % i, [128, 16, 512], BF16) for i in range(2)]
        gm_bc = sb(st, "gm_bc", [128, D], F32)
        xo = [sb(st, "xo%d" % i, [128, 512], F32) for i in range(2)]
        tmp = [sb(st, "rtmp%d" % i, [128, 512], F32) for i in range(2)]
        x1c = [sb(st, "x1c%d" % i, [128, 512], F32) for i in range(2)]
        S.dma("sp", lambda: nc.sync.dma_start(out=gm_bc[:], in_=bcast_rows(mod_d[2:3, :])), writes=["gm_bc"])
        w_out_r = W["w_out"].rearrange("(c p) n -> p c n", p=128)
        it = 0
        for n in range(4):
            b = n % 2
            ns = slice(n * 512, (n + 1) * 512)
            for q in range(4):
                S.dma("pool", lambda: nc.gpsimd.dma_start(out=wo[b][:, q * 4:(q + 1) * 4, :], in_=w_out_r[:, q * 4:(q + 1) * 4, ns]), writes=[("wo", b, q)])
            for t in range(8):
                pb = it % 4
                i2 = it % 2
                it += 1
                ts_ = slice(t * 128, (t + 1) * 128)
                S.dma("sp", lambda: nc.sync.dma_start(out=xo[i2][:], in_=x_own[ts_, ns]), writes=[("xo", i2)])
                for c in range(16):
                    S.op("pe", lambda: T_.matmul(psb[pb][:, :], lhsT=mergedT[:, c, ts_], rhs=wo[b][:, c, :], start=(c == 0), stop=(c == 15)),
                         reads=[("wo", b, c // 4)], writes=[("psb", pb)])
                S.op("dve", lambda: V.tensor_tensor(out=tmp[i2][:], in0=psb[pb][:, :], in1=gm_bc[:, ns], op=ALU.mult),
                     reads=[("psb", pb), "gm_bc"], writes=[("rtmp", i2)])
                S.op("pool", lambda: P_.tensor_tensor(out=x1c[i2][:], in0=tmp[i2][:], in1=xo[i2][:], op=ALU.add),
                     reads=[("rtmp", i2), ("xo", i2)], writes=[("x1c", i2)])
                S.dma("sp", lambda: nc.sync.dma_start(out=x1_d[ts_, ns], in_=x1c[i2][:]), reads=[("x1c", i2)], writes=[("x1d", t, n)])
        S.barrier()


def norm_router_phase(nc, S, sb, psb, W, cs, x1_d, h2T, Gt, Gf_col, shf_col, eps_t):
    V, A_, P_, T_ = nc.vector, nc.scalar, nc.gpsimd, nc.tensor
    BIG = 30000.0
    with ExitStack() as st:
        x1t = [sb(st, "x1t%d" % i, [128, D], F32) for i in range(2)]
        xn = [sb(st, "xn%d" % i, [128, D], F32) for i in range(2)]
        hf = [sb(st, "hf%d" % i, [128, 16, 128], F32) for i in range(2)]
        junk = sb(st, "njunk", [128, D], BF16)
        ss = [sb(st, "nss%d" % i, [128, 1], F32) for i in range(2)]
        rs = [sb(st, "nrs%d" % i, [128, 1], F32) for i in range(2)]
        wr = sb(st, "wr", [128, 16, 72], F32)
        brt = sb(st, "brt", [128, 72], F32)
        lg = sb(st, "lg", [128, 72], F32)
        sm = {n: sb(st, "r_" + n, [128, 1], F32) for n in ("gmax", "ngmax", "gsum", "pg", "m1", "m2", "dd", "rr", "den", "p1", "c1", "c2")}
        ohg = sb(st, "ohg", [128, 8], F32)
        pen = sb(st, "pen", [128, 8], F32)
        gjunk = sb(st, "gjunk", [128, 8], F32)
        em = sb(st, "em", [128, 64], F32)
        em2 = sb(st, "em2", [128, 64], F32)
        mask1 = sb(st, "mask1", [128, 64], F32)
        mask2 = sb(st, "mask2", [128, 64], F32)
        w_rg_r = W["w_rg"].rearrange("(c p) n -> p c n", p=128)
        w_re_r = W["w_re"].rearrange("(c p) n -> p c n", p=128)
        for hh in range(2):
            S.dma("sp", lambda: nc.sync.dma_start(out=wr[:, hh * 8:(hh + 1) * 8, 0:8], in_=w_rg_r[:, hh * 8:(hh + 1) * 8, :]), writes=[("wr", 0, hh)])
            S.dma("sp", lambda: nc.sync.dma_start(out=wr[:, hh * 8:(hh + 1) * 8, 8:72], in_=w_re_r[:, hh * 8:(hh + 1) * 8, :]), writes=[("wr", 1, hh)])
        S.dma("sp", lambda: nc.sync.dma_start(out=brt[:, 0:8], in_=bcast_rows(W["b_rg"][0:1, :])), writes=[("brt", 0)])
        S.dma("sp", lambda: nc.sync.dma_start(out=brt[:, 8:72], in_=bcast_rows(W["b_re"][0:1, :])), writes=[("brt", 1)])
        wr_keys = [("wr", 0, 0), ("wr", 0, 1), ("wr", 1, 0), ("wr", 1, 1)]
        def r_front(t):
            b = t % 2
            ts_ = slice(t * 128, (t + 1) * 128)
            for hh in range(2):
                S.dma("sp", lambda: nc.sync.dma_start(out=x1t[b][:, hh * 1024:(hh + 1) * 1024], in_=x1_d[ts_, hh * 1024:(hh + 1) * 1024]), writes=[("x1t", b, hh)])
            S.op("act", lambda: A_.activation(out=junk[:], in_=x1t[b][:], func=AF.Square, accum_out=ss[b][:]),
                 reads=[("x1t", b, 0), ("x1t", b, 1)], writes=[("nss", b), "njunk"])
            S.op("act", lambda: A_.activation(out=rs[b][:], in_=ss[b][:], func=AF.Sqrt, bias=eps_t[:], scale=1.0 / D),
                 reads=[("nss", b), "eps"], writes=[("nrs", b)])
            S.op("dve", lambda: V.reciprocal(out=rs[b][:], in_=rs[b][:]), reads=[("nrs", b)], writes=[("nrs", b)])
            S.op("dve", lambda: V.tensor_scalar(out=xn[b][:], in0=x1t[b][:], scalar1=rs[b][:, 0:1], scalar2=None, op0=ALU.mult),
                 reads=[("x1t", b, 0), ("x1t", b, 1), ("nrs", b)], writes=[("xn", b)])
            for g in range(4):
                for k in range(4):
                    c = g * 4 + k
                    S.op("pe", lambda: T_.matmul(psb[g][:, k * 128:(k + 1) * 128], lhsT=xn[b][:, c * 128:(c + 1) * 128], rhs=cs["ident"][:], start=True, stop=True),
                         reads=[("xn", b), "c_ident"], writes=[("psb", g)])
                for k in range(4):
                    c = g * 4 + k
                    S.op("act", lambda: A_.activation(out=hf[b][:, c, :], in_=psb[g][:, k * 128:(k + 1) * 128], func=AF.Identity,
                                                      bias=shf_col[:, c:c + 1], scale=Gf_col[:, c:c + 1]),
                         reads=[("psb", g), "shf_col", "Gf_col"], writes=[("hf", b, g)])
            S.op("pool", lambda: P_.tensor_copy(out=h2T[:, :, ts_], in_=hf[b][:, :, :]), reads=[("hf", b, g) for g in range(4)], writes=[("h2T", t)])
            rb = 4 + b
            for c in range(16):
                S.op("pe", lambda: T_.matmul(psb[rb][:, 0:72], lhsT=hf[b][:, c, :], rhs=wr[:, c, :], start=(c == 0), stop=(c == 15)),
                     reads=[("hf", b, c // 4)] + wr_keys, writes=[("psb", rb)])

        def r_chain(t):
            b = t % 2
            rb = 4 + b
            S.op("dve", lambda: V.tensor_tensor(out=lg[:], in0=psb[rb][:, 0:72], in1=brt[:], op=ALU.add), reads=[("psb", rb), ("brt", 0), ("brt", 1)], writes=["lg"])
            S.op("dve", lambda: V.tensor_reduce(out=sm["gmax"][:], in_=lg[:, 0:8], axis=AX.X, op=ALU.max), reads=["lg"], writes=["gmax"])
            S.op("dve", lambda: V.tensor_scalar(out=ohg[:], in0=lg[:, 0:8], scalar1=sm["gmax"][:, 0:1], scalar2=None, op0=ALU.is_ge), reads=["lg", "gmax"], writes=["ohg"])
            S.op("dve", lambda: V.tensor_scalar(out=sm["ngmax"][:], in0=sm["gmax"][:], scalar1=-1.0, scalar2=None, op0=ALU.mult), reads=["gmax"], writes=["ngmax"])
            S.op("act", lambda: A_.activation(out=gjunk[:], in_=lg[:, 0:8], func=AF.Exp, bias=sm["ngmax"][:, 0:1], scale=1.0, accum_out=sm["gsum"][:]),
                 reads=["lg", "ngmax"], writes=["gsum", "gjunk"])
            S.op("dve", lambda: V.reciprocal(out=sm["pg"][:], in_=sm["gsum"][:]), reads=["gsum"], writes=["pg"])
            S.op("dve", lambda: V.tensor_scalar(out=pen[:], in0=ohg[:], scalar1=BIG, scalar2=-BIG, op0=ALU.mult, op1=ALU.add), reads=["ohg"], writes=["pen"])
            for g in range(8):
                S.op("dve", lambda: V.tensor_scalar(out=em[:, g * 8:(g + 1) * 8], in0=lg[:, 8 + g * 8:16 + g * 8], scalar1=pen[:, g:g + 1], scalar2=None, op0=ALU.add),
                     reads=["lg", "pen"], writes=[("em", g)])
            emk = [("em", g) for g in range(8)]
            S.op("dve", lambda: V.tensor_reduce(out=sm["m1"][:], in_=em[:], axis=AX.X, op=ALU.max), reads=emk, writes=["m1"])
            S.op("dve", lambda: V.tensor_scalar(out=mask1[:], in0=em[:], scalar1=sm["m1"][:, 0:1], scalar2=None, op0=ALU.is_ge), reads=emk + ["m1"], writes=["mask1"])
            S.op("dve", lambda: V.scalar_tensor_tensor(out=em2[:], in0=mask1[:], scalar=-BIG, in1=em[:], op0=ALU.mult, op1=ALU.add), reads=emk + ["mask1"], writes=["em2"])
            S.op("dve", lambda: V.tensor_reduce(out=sm["m2"][:], in_=em2[:], axis=AX.X, op=ALU.max), reads=["em2"], writes=["m2"])
            S.op("dve", lambda: V.tensor_scalar(out=mask2[:], in0=em2[:], scalar1=sm["m2"][:, 0:1], scalar2=None, op0=ALU.is_ge), reads=["em2", "m2"], writes=["mask2"])
            S.op("dve", lambda: V.tensor_tensor(out=sm["dd"][:], in0=sm["m2"][:], in1=sm["m1"][:], op=ALU.subtract), reads=["m1", "m2"], writes=["dd"])
            S.op("act", lambda: A_.activation(out=sm["rr"][:], in_=sm["dd"][:], func=AF.Exp), reads=["dd"], writes=["rr"])
            S.op("dve", lambda: V.tensor_scalar(out=sm["den"][:], in0=sm["rr"][:], scalar1=1.0, scalar2=None, op0=ALU.add), reads=["rr"], writes=["den"])
            S.op("dve", lambda: V.reciprocal(out=sm["p1"][:], in_=sm["den"][:]), reads=["den"], writes=["p1"])
            S.op("dve", lambda: V.tensor_tensor(out=sm["c1"][:], in0=sm["p1"][:], in1=sm["pg"][:], op=ALU.mult), reads=["p1", "pg"], writes=["c1"])
            S.op("dve", lambda: V.tensor_tensor(out=sm["c2"][:], in0=sm["c1"][:], in1=sm["rr"][:], op=ALU.mult), reads=["c1", "rr"], writes=["c2"])
            S.op("dve", lambda: V.tensor_scalar(out=Gt[:, t, :], in0=mask1[:], scalar1=sm["c1"][:, 0:1], scalar2=None, op0=ALU.mult), reads=["mask1", "c1"], writes=[("Gt", t)])
            S.op("dve", lambda: V.scalar_tensor_tensor(out=Gt[:, t, :], in0=mask2[:], scalar=sm["c2"][:, 0:1], in1=Gt[:, t, :], op0=ALU.mult, op1=ALU.add),
                 reads=["mask2", "c2", ("Gt", t)], writes=[("Gt", t)])
        for t in range(8):
            r_front(t)
            if t >= 1:
                r_chain(t - 1)
        r_chain(7)
        S.barrier()


def moe_weight_bufs(nc, stack, side=None):
    kw = {"side": side} if side else {}
    wg = [stack.enter_context(nc.sbuf_tensor("wg%d" % i, [128, 16, 128], BF16, **kw)) for i in range(MOE_NWB)]
    wu = [stack.enter_context(nc.sbuf_tensor("wu%d" % i, [128, 16, 128], BF16, **kw)) for i in range(MOE_NWB)]
    wd = stack.enter_context(nc.sbuf_tensor("wd", [128, 8, D], BF16, **kw))
    return wg, wu, wd


def moe_issue_unit(nc, S, W, wg, wu, u):
    e, f = u // 8, u % 8
    wb = u % MOE_NWB
    rows_g = W["w_eg"][e * D:(e + 1) * D, :].rearrange("(c p) n -> p c n", p=128)
    rows_u = W["w_eu"][e * D:(e + 1) * D, :].rearrange("(c p) n -> p c n", p=128)
    fs = slice(f * 128, (f + 1) * 128)
    for hh in range(2):
        S.dma("pool", lambda: nc.gpsimd.dma_start(out=wg[wb][:, hh * 8:(hh + 1) * 8, :], in_=rows_g[:, hh * 8:(hh + 1) * 8, fs]), writes=[("wg", wb, hh)])
    for hh in range(2):
        S.dma("pool", lambda: nc.gpsimd.dma_start(out=wu[wb][:, hh * 8:(hh + 1) * 8, :], in_=rows_u[:, hh * 8:(hh + 1) * 8, fs]), writes=[("wu", wb, hh)])


def moe_issue_wd(nc, S, W, wd, e):
    rows_d = W["w_ed"][e * 1024:(e + 1) * 1024, :].rearrange("(f p) n -> p f n", p=128)
    for f2 in range(8):
        S.dma("pool", lambda: nc.gpsimd.dma_start(out=wd[:, f2, :], in_=rows_d[:, f2, :]), writes=[("wd", f2)])


def moe_prefetch(nc, S, W, wg, wu, wd):
    for u in range(MOE_NWB - 1):
        moe_issue_unit(nc, S, W, wg, wu, u)
    moe_issue_wd(nc, S, W, wd, 0)


def moe_phase(nc, S, sb, psb, W, h2T, Gt, acc, NE=64, bufs=None, prefetched=False):
    V, A_, P_, T_ = nc.vector, nc.scalar, nc.gpsimd, nc.tensor
    NWB = MOE_NWB
    NU = NE * 8
    with ExitStack() as st:
        if bufs is None:
            wg, wu, wd = moe_weight_bufs(nc, st)
        else:
            wg, wu, wd = bufs
        actT = sb(st, "actT", [128, 8, NOWN], BF16)
        sl = [sb(st, "sl%d" % i, [128, 512], F32) for i in range(2)]
        SBUF_LOG.append(("moe", nc.sbuf_bytes_remaining))
        for t in range(8):
            S.op("dve", lambda: V.memset(acc[:, t, :], 0.0), writes=[("acc", t, n) for n in range(4)])

        def issue_unit(u):
            moe_issue_unit(nc, S, W, wg, wu, u)

        def issue_wd(e):
            moe_issue_wd(nc, S, W, wd, e)

        if not prefetched:
            moe_prefetch(nc, S, W, wg, wu, wd)
        it = 0
        dn = 0
        for u in range(NU):
            e, f = u // 8, u % 8
            wb = u % NWB
            if u + NWB - 1 < NU:
                issue_unit(u + NWB - 1)
            for half in range(2):
                ab = it % 2
                it += 1
                tok = slice(half * 512, (half + 1) * 512)
                for c in range(16):
                    S.op("pe", lambda: T_.matmul(psb[ab][:, :], lhsT=wg[wb][:, c, :], rhs=h2T[:, c, tok], start=(c == 0), stop=(c == 15)),
                         reads=[("wg", wb, c // 8)], writes=[("psb", ab)])
                for c in range(16):
                    S.op("pe", lambda: T_.matmul(psb[2 + ab][:, :], lhsT=wu[wb][:, c, :], rhs=h2T[:, c, tok], start=(c == 0), stop=(c == 15)),
                         reads=[("wu", wb, c // 8)], writes=[("psb", 2 + ab)])
                S.op("act", lambda: A_.activation(out=sl[ab][:], in_=psb[ab][:, :], func=AF.Silu), reads=[("psb", ab)], writes=[("sl", ab)])
                S.op("dve", lambda: V.tensor_tensor(out=actT[:, f, tok], in0=psb[2 + ab][:, :], in1=sl[ab][:], op=ALU.mult),
                     reads=[("psb", 2 + ab), ("sl", ab)], writes=[("actT", f, half)])
            if f == 7:
                for t in range(8):
                    ts_ = slice(t * 128, (t + 1) * 128)
                    for n in range(4):
                        db = 4 + dn % 4
                        dn += 1
                        ns = slice(n * 512, (n + 1) * 512)
                        for f2 in range(8):
                            S.op("pe", lambda: T_.matmul(psb[db][:, :], lhsT=actT[:, f2, ts_], rhs=wd[:, f2, ns], start=(f2 == 0), stop=(f2 == 7)),
                                 reads=[("actT", f2, t // 4), ("wd", f2)], writes=[("psb", db)])
                        S.op("dve", lambda: V.scalar_tensor_tensor(out=acc[:, t, ns], in0=psb[db][:, :], scalar=Gt[:, t, e:e + 1], in1=acc[:, t, ns], op0=ALU.mult, op1=ALU.add),
                             reads=[("psb", db), ("acc", t, n)], writes=[("acc", t, n)])
                if e + 1 < NE:
                    issue_wd(e + 1)
        S.barrier()


def final_phase(nc, S, sb, W, acc, x1_d, mod_d, out_own, eps_t):
    V, A_, P_ = nc.vector, nc.scalar, nc.gpsimd
    with ExitStack() as st:
        gf_bc = sb(st, "gf_bc", [128, D], F32)
        gfin_bc = sb(st, "gfin_bc", [128, D], F32)
        x1t = [sb(st, "fx1t%d" % i, [128, D], F32) for i in range(2)]
        ot = [sb(st, "fot%d" % i, [128, D], F32) for i in range(2)]
        junk = sb(st, "fjunk", [128, D], BF16)
        ss = [sb(st, "fss%d" % i, [128, 1], F32) for i in range(2)]
        rs = [sb(st, "frs%d" % i, [128, 1], F32) for i in range(2)]
        S.dma("sp", lambda: nc.sync.dma_start(out=gf_bc[:], in_=bcast_rows(mod_d[5:6, :])), writes=["gf_bc"])
        S.dma("sp", lambda: nc.sync.dma_start(out=gfin_bc[:], in_=bcast_rows(W["g_final"][0:1, :])), writes=["gfin_bc"])
        for t in range(8):
            b = t % 2
            ts_ = slice(t * 128, (t + 1) * 128)
            for hh in range(2):
                S.dma("sp", lambda: nc.sync.dma_start(out=x1t[b][:, hh * 1024:(hh + 1) * 1024], in_=x1_d[ts_, hh * 1024:(hh + 1) * 1024]), writes=[("fx1t", b, hh)])
            S.op("dve", lambda: V.tensor_tensor(out=ot[b][:], in0=acc[:, t, :], in1=gf_bc[:], op=ALU.mult), reads=["gf_bc"], writes=[("fot", b)])
            S.op("pool", lambda: P_.tensor_tensor(out=ot[b][:], in0=ot[b][:], in1=x1t[b][:], op=ALU.add),
                 reads=[("fot", b), ("fx1t", b, 0), ("fx1t", b, 1)], writes=[("fot", b)])
            S.op("act", lambda: A_.activation(out=junk[:], in_=ot[b][:], func=AF.Square, accum_out=ss[b][:]), reads=[("fot", b)], writes=[("fss", b), "fjunk"])
            S.op("act", lambda: A_.activation(out=rs[b][:], in_=ss[b][:], func=AF.Sqrt, bias=eps_t[:], scale=1.0 / D), reads=[("fss", b), "eps"], writes=[("frs", b)])
            S.op("dve", lambda: V.reciprocal(out=rs[b][:], in_=rs[b][:]), reads=[("frs", b)], writes=[("frs", b)])
            S.op("dve", lambda: V.scalar_tensor_tensor(out=ot[b][:], in0=ot[b][:], scalar=rs[b][:, 0:1], in1=gfin_bc[:], op0=ALU.mult, op1=ALU.mult),
                 reads=[("fot", b), ("frs", b), "gfin_bc"], writes=[("fot", b)])
            for hh in range(2):
                S.dma("sp", lambda: nc.sync.dma_start(out=out_own[ts_, hh * 1024:(hh + 1) * 1024], in_=ot[b][:, hh * 1024:(hh + 1) * 1024]), reads=[("fot", b)], writes=[("out", t, hh)])


def own_qblocks(half):
    qb = []
    for p in range(4):
        qb.append(2 * p + half)
        qb.append(15 - 2 * p - half)
    return qb


def own_token_index(half):
    return np.concatenate([np.arange(q * 128, (q + 1) * 128) for q in own_qblocks(half)])


def col_layout(v):
    return np.ascontiguousarray(np.asarray(v, np.float32).reshape(-1, 128).T)


def make_shared(inp, stage):
    f = lambda a: np.ascontiguousarray(np.asarray(a, np.float32))
    sh = {
        "rel_bias_table": f(inp["rel_bias_table"]), "w_ada": f(inp["w_ada"][0]), "b_ada": f(inp["b_ada"][0]).reshape(1, -1),
        "g_mix_col": col_layout(inp["g_mix"][0]), "w_in": f(inp["w_in"][0]),
        "lq1": f(inp["lambda_q1"][0]).reshape(1, -1), "lk1": f(inp["lambda_k1"][0]).reshape(1, -1),
        "lq2": f(inp["lambda_q2"][0]).reshape(1, -1), "lk2": f(inp["lambda_k2"][0]).reshape(1, -1),
        "g_subln": f(inp["g_subln"][0]).reshape(1, -1), "w_proj_a": f(inp["w_proj_a"][0]), "w_proj_b": f(inp["w_proj_b"][0]),
        "w_out": f(inp["w_out"][0]), "g_ffn_col": col_layout(inp["g_ffn"][0]),
        "w_rg": f(inp["w_router_group"][0]), "b_rg": f(inp["b_router_group"][0]).reshape(1, -1),
        "w_re": f(inp["w_router_expert"][0]), "b_re": f(inp["b_router_expert"][0]).reshape(1, -1),
        "g_final": f(inp["g_final"]).reshape(1, -1),
    }
    if stage >= 7:
        sh["w_eg"] = f(inp["w_expert_gate"][0]).reshape(64 * D, 1024)
        sh["w_eu"] = f(inp["w_expert_up"][0]).reshape(64 * D, 1024)
        sh["w_ed"] = f(inp["w_expert_down"][0]).reshape(64 * 1024, D)
    for k, v in host_consts().items():
        sh["c_" + k] = v
    return sh


def make_core_map(inp, shared, core):
    b, half = core // 2, core % 2
    xb = np.asarray(inp["x"][b], np.float32)
    m = dict(shared)
    m["x_all"] = np.ascontiguousarray(xb)
    m["x_own"] = np.ascontiguousarray(xb[own_token_index(half)])
    m["c_col"] = col_layout(inp["c"][b])
    m["halfv"] = np.full((128, 1), float(half), np.float32)
    return m


def kernel(**inputs):
    nc, _ = build(stage=99, dbg=False)
    shared = make_shared(inputs, 99)
    in_maps = [make_core_map(inputs, shared, c) for c in range(8)]
    res = run_bass_kernel_spmd(nc, in_maps, core_ids=list(range(8)))
    out = np.zeros((4, SEQ, D), np.float32)
    for c in range(8):
        out[c // 2, own_token_index(c % 2)] = res.results[c]["out_own"]
    return out
```
